# Optimizing a Trainium2 kernel written in Bass

```python
import jax, jax.numpy as jnp
from jax import lax
import numpy as np

D_MODEL = 1024
BATCH = 8
SEQ = 4096
DEPTH = 1

HEAD_DIM = 64
RWKV_WIDTH = D_MODEL // 2
RWKV_HEADS = RWKV_WIDTH // HEAD_DIM
ATTN_WIDTH = D_MODEL - RWKV_WIDTH
ATTN_Q_HEADS = ATTN_WIDTH // HEAD_DIM
ATTN_KV_HEADS = max(1, ATTN_Q_HEADS // 4)
ATTN_GROUP = ATTN_Q_HEADS // ATTN_KV_HEADS
WINDOW = 128
ATTN_BLOCK = WINDOW
DECAY_LORA = 64
ICLR_LORA = 64
GATE_LORA = 128
RWKV_COLS = 3 * RWKV_WIDTH + DECAY_LORA + ICLR_LORA + GATE_LORA
ATTN_COLS = ATTN_WIDTH + 2 * ATTN_KV_HEADS * HEAD_DIM
IN_COLS = RWKV_COLS + ATTN_COLS
RWKV_SPLITS = [RWKV_WIDTH, 2 * RWKV_WIDTH, 3 * RWKV_WIDTH,
               3 * RWKV_WIDTH + DECAY_LORA, 3 * RWKV_WIDTH + DECAY_LORA + ICLR_LORA]
ATTN_SPLITS = [ATTN_WIDTH, ATTN_WIDTH + ATTN_KV_HEADS * HEAD_DIM]
N_EXPERTS = 32
TOP_K = 4
D_EXPERT = D_MODEL
SWIGLU_ALPHA = 1.702
SWIGLU_LIMIT = 7.0
MOE_BLOCK = 128
NORM_EPS = 1e-6
GN_EPS = 64e-5

kernel_name = 'hybrid_rwkv7_swa_sink_moe_adaln'


def rms_norm(x, g):
    xf = x.astype(jnp.float32)
    y = xf * lax.rsqrt(jnp.mean(xf * xf, axis=-1, keepdims=True) + NORM_EPS)
    return (y * g.astype(jnp.float32)).astype(x.dtype)


def modulate(h, shift, scale):
    return h * (1 + scale[:, None, :]) + shift[:, None, :]


def token_shift(p):
    return jnp.pad(p[:, :-1], ((0, 0), (1, 0), (0, 0)))


def rwkv7_scan(r, decay, k, v, kk, b):
    Bsz, _, H, N = r.shape

    def step(S, inp):
        r_t, w_t, k_t, v_t, kk_t, b_t = inp
        sa = jnp.einsum('bhvk,bhk->bhv', S, -kk_t)
        S = (S * w_t[:, :, None, :] + sa[..., None] * b_t[:, :, None, :]
             + v_t[..., None] * k_t[:, :, None, :])
        y = jnp.einsum('bhvk,bhk->bhv', S, r_t)
        return S, y

    xs = tuple(jnp.moveaxis(t, 1, 0) for t in (r, decay, k, v, kk, b))
    S0 = jnp.zeros((Bsz, H, N, N), jnp.float32)
    _, ys = lax.scan(step, S0, xs)
    return jnp.moveaxis(ys, 0, 1)


def rwkv7_time_mix(p, mu, w0, w_up, a0, a_up, g_up, k_k, k_a, r_k, lnx_g, lnx_b):
    Bsz, T, _ = p.shape
    f32 = jnp.float32
    p = p + (token_shift(p) - p) * mu
    r, k, v, wd, ad, gd = jnp.split(p, RWKV_SPLITS, axis=-1)
    w_log = -jax.nn.softplus(-(w0 + jnp.tanh(wd) @ w_up).astype(f32)) - 0.5
    decay = jnp.exp(-jnp.exp(w_log))
    a = jax.nn.sigmoid(a0 + ad @ a_up)
    g = jax.nn.sigmoid(gd) @ g_up
    kk = k * k_k
    k = k * (1 + (a - 1) * k_a)
    heads = lambda t: t.reshape(Bsz, T, RWKV_HEADS, HEAD_DIM).astype(f32)
    r_h, k_h, v_h, a_h, w_h, kk_h = map(heads, (r, k, v, a, decay, kk))
    kk_h = kk_h / jnp.maximum(jnp.sqrt(jnp.sum(kk_h * kk_h, -1, keepdims=True)), 1e-12)
    y = rwkv7_scan(r_h, w_h, k_h, v_h, kk_h, kk_h * a_h)
    mean = jnp.mean(y, -1, keepdims=True)
    var = jnp.mean(jnp.square(y - mean), -1, keepdims=True)
    y = ((y - mean) * lax.rsqrt(var + GN_EPS)).reshape(Bsz, T, RWKV_WIDTH)
    y = y * lnx_g.astype(f32) + lnx_b.astype(f32)
    bonus = jnp.sum(r_h * k_h * r_k.astype(f32), -1, keepdims=True) * v_h
    y = y + bonus.reshape(Bsz, T, RWKV_WIDTH)
    return (y * g.astype(f32)).astype(p.dtype)


def swa_sink_attention(p, q_norm_g, k_norm_g, sinks):
    Bsz, T, _ = p.shape
    nb = T // ATTN_BLOCK
    q, k, v = jnp.split(p, ATTN_SPLITS, axis=-1)
    q = rms_norm(q.reshape(Bsz, T, ATTN_Q_HEADS, HEAD_DIM), q_norm_g)
    k = rms_norm(k.reshape(Bsz, T, ATTN_KV_HEADS, HEAD_DIM), k_norm_g)
    v = v.reshape(Bsz, T, ATTN_KV_HEADS, HEAD_DIM)
    qb = q.reshape(Bsz, nb, ATTN_BLOCK, ATTN_KV_HEADS, ATTN_GROUP, HEAD_DIM)

    def with_prev(t):
        tb = t.reshape(Bsz, nb, ATTN_BLOCK, ATTN_KV_HEADS, HEAD_DIM)
        prev = jnp.pad(tb[:, :-1], ((0, 0), (1, 0), (0, 0), (0, 0), (0, 0)))
        return jnp.concatenate([prev, tb], axis=2)

    kw, vw = with_prev(k), with_prev(v)
    scale = HEAD_DIM ** -0.5
    s = jnp.einsum('bnqkgd,bnskd->bnkgqs', qb, kw).astype(jnp.float32) * scale
    blk = jnp.arange(nb)[:, None] * ATTN_BLOCK
    qpos = blk + jnp.arange(ATTN_BLOCK)[None, :]
    kpos = blk - ATTN_BLOCK + jnp.arange(2 * ATTN_BLOCK)[None, :]
    diff = qpos[:, :, None] - kpos[:, None, :]
    mask = (diff >= 0) & (diff < WINDOW) & (kpos[:, None, :] >= 0)
    s = jnp.where(mask[None, :, None, None], s, -jnp.inf)
    sink = jnp.broadcast_to(
        sinks.astype(jnp.float32).reshape(ATTN_KV_HEADS, ATTN_GROUP)[None, None, :, :, None, None],
        s.shape[:-1] + (1,))
    prob = jax.nn.softmax(jnp.concatenate([s, sink], axis=-1), axis=-1)[..., :-1]
    o = jnp.einsum('bnkgqs,bnskd->bnqkgd', prob.astype(v.dtype), vw)
    return o.reshape(Bsz, T, ATTN_WIDTH)


def moe_ffn(h, w_router, b_router, w1, b1, w2, b2):
    Bsz, T, D = h.shape
    n_tok = Bsz * T
    hf = h.reshape(n_tok, D)
    logits = (hf @ w_router + b_router).astype(jnp.float32)
    top_val, top_idx = lax.top_k(logits, TOP_K)
    gate = jax.nn.softmax(top_val, axis=-1)
    n_asg = n_tok * TOP_K
    e_flat = top_idx.reshape(-1)
    tok_flat = jnp.arange(n_asg, dtype=jnp.int32) // TOP_K
    g_flat = gate.reshape(-1)
    order = jnp.argsort(e_flat)
    e_sorted = e_flat[order]
    counts = jnp.bincount(e_flat, length=N_EXPERTS)
    starts = jnp.cumsum(counts) - counts
    padded = (counts + MOE_BLOCK - 1) // MOE_BLOCK * MOE_BLOCK
    pad_ends = jnp.cumsum(padded)
    pad_starts = pad_ends - padded
    dest = pad_starts[e_sorted] + (jnp.arange(n_asg) - starts[e_sorted])
    n_blocks = -(-n_asg // MOE_BLOCK) + N_EXPERTS
    n_slots = n_blocks * MOE_BLOCK
    slot_tok = jnp.zeros((n_slots,), jnp.int32).at[dest].set(tok_flat[order])
    slot_gate = jnp.zeros((n_slots,), jnp.float32).at[dest].set(g_flat[order])
    block_expert = jnp.minimum(
        jnp.searchsorted(pad_ends, jnp.arange(n_blocks) * MOE_BLOCK, side='right'),
        N_EXPERTS - 1)
    xs = hf[slot_tok].reshape(n_blocks, MOE_BLOCK, D)

    def expert_block(args):
        xb, e = args
        hu = xb @ w1[e] + b1[e]
        glu = jnp.minimum(hu[:, :D_EXPERT], SWIGLU_LIMIT)
        lin = jnp.clip(hu[:, D_EXPERT:], -SWIGLU_LIMIT, SWIGLU_LIMIT)
        act = glu * jax.nn.sigmoid(SWIGLU_ALPHA * glu) * (lin + 1)
        return act @ w2[e] + b2[e]

    ys = lax.map(expert_block, (xs, block_expert)).reshape(n_slots, D)
    ys = ys * slot_gate[:, None].astype(ys.dtype)
    out = jax.ops.segment_sum(ys, slot_tok, num_segments=n_tok)
    return out.reshape(Bsz, T, D)


def setup_inputs(seed: int = 0) -> dict:
    key = jax.random.key(seed)
    ks = jax.random.split(key, 32)
    L, D, W = DEPTH, D_MODEL, RWKV_WIDTH
    nrm = lambda k, shape, s: jax.random.normal(k, shape, jnp.float32) * s
    return {
        'x': nrm(ks[0], (BATCH, SEQ, D), 1.0),
        'c': nrm(ks[1], (BATCH, D), 1.0),
        'w_ada': nrm(ks[2], (L, D, 6 * D), D ** -0.5),
        'b_ada': nrm(ks[3], (L, 6 * D), 0.02),
        'norm1_g': 1.0 + nrm(ks[4], (L, D), 0.02),
        'w_in': nrm(ks[5], (L, D, IN_COLS), D ** -0.5),
        'mu_shift': jax.random.uniform(ks[6], (L, RWKV_COLS), jnp.float32),
        'w0': jax.random.uniform(ks[7], (L, W), jnp.float32, -6.0, 1.0),
        'w_up': nrm(ks[8], (L, DECAY_LORA, W), 0.1 * DECAY_LORA ** -0.5),
        'a0': nrm(ks[9], (L, W), 0.1),
        'a_up': nrm(ks[10], (L, ICLR_LORA, W), 0.1 * ICLR_LORA ** -0.5),
        'g_up': nrm(ks[11], (L, GATE_LORA, W), GATE_LORA ** -0.5),
        'k_k': 0.85 + nrm(ks[12], (L, W), 0.02),
        'k_a': 1.0 + nrm(ks[13], (L, W), 0.02),
        'r_k': nrm(ks[14], (L, RWKV_HEADS, HEAD_DIM), 0.1),
        'lnx_g': 1.0 + nrm(ks[15], (L, W), 0.02),
        'lnx_b': nrm(ks[16], (L, W), 0.02),
        'q_norm_g': 1.0 + nrm(ks[17], (L, HEAD_DIM), 0.02),
        'k_norm_g': 1.0 + nrm(ks[18], (L, HEAD_DIM), 0.02),
        'sinks': nrm(ks[19], (L, ATTN_Q_HEADS), 0.5),
        'w_out': nrm(ks[20], (L, D, D), D ** -0.5),
        'norm2_g': 1.0 + nrm(ks[21], (L, D), 0.02),
        'w_router': nrm(ks[22], (L, D, N_EXPERTS), D ** -0.5),
        'b_router': nrm(ks[23], (L, N_EXPERTS), 0.01),
        'w1': nrm(ks[24], (L, N_EXPERTS, D, 2 * D_EXPERT), D ** -0.5),
        'b1': nrm(ks[25], (L, N_EXPERTS, 2 * D_EXPERT), 0.01),
        'w2': nrm(ks[26], (L, N_EXPERTS, D_EXPERT, D), D_EXPERT ** -0.5),
        'b2': nrm(ks[27], (L, N_EXPERTS, D), 0.01),
    }


def reference(x, c, w_ada, b_ada, norm1_g, w_in, mu_shift, w0, w_up, a0, a_up, g_up,
              k_k, k_a, r_k, lnx_g, lnx_b, q_norm_g, k_norm_g, sinks, w_out, norm2_g,
              w_router, b_router, w1, b1, w2, b2):
    for l in range(DEPTH):
        mod = jax.nn.silu(c) @ w_ada[l] + b_ada[l]
        sh1, sc1, gt1, sh2, sc2, gt2 = jnp.split(mod, 6, axis=-1)
        h = modulate(rms_norm(x, norm1_g[l]), sh1, sc1)
        p = h @ w_in[l]
        o_rwkv = rwkv7_time_mix(p[..., :RWKV_COLS], mu_shift[l], w0[l], w_up[l], a0[l],
                                a_up[l], g_up[l], k_k[l], k_a[l], r_k[l], lnx_g[l], lnx_b[l])
        o_attn = swa_sink_attention(p[..., RWKV_COLS:], q_norm_g[l], k_norm_g[l], sinks[l])
        mix = jnp.concatenate([o_rwkv, o_attn], axis=-1) @ w_out[l]
        x = x + gt1[:, None, :] * mix
        h = modulate(rms_norm(x, norm2_g[l]), sh2, sc2)
        x = x + gt2[:, None, :] * moe_ffn(h, w_router[l], b_router[l], w1[l], b1[l], w2[l], b2[l])
    return x
```

```python
import os
import threading
from contextlib import ExitStack
import numpy as np
import ml_dtypes
import concourse.bass as bass
import concourse.mybir as mybir
from concourse.bass_utils import run_bass_kernel_spmd

F32 = mybir.dt.float32
BF16 = mybir.dt.bfloat16
I32 = mybir.dt.int32
ALU = mybir.AluOpType
AF = mybir.ActivationFunctionType
AX = mybir.AxisListType

T = 4096
D = 1024
NCH = T // 128
BLK = int(os.environ.get("KBLK", "384"))
JB = BLK // 128
MB = -(-T // BLK)
NB = T * 4 // BLK + 32
NSLOT = NB * BLK
DECAY_K = 0.6065306597126334
NORM_EPS = 1e-6
GN_EPS = 64e-5


KLIMIT = int(os.environ.get("KLIMIT", "100000000"))
NOPOOL = not os.environ.get("KPOOL")


class Buf:
    __slots__ = ("name", "w", "r", "excl")

    def __init__(self, name):
        self.name = name
        self.w = None
        self.r = []
        self.excl = False


class Prog:
    ENGS = ("pe", "act", "dve", "pool", "sp")

    def __init__(self, nc, stack):
        self.nc = nc
        self.stack = stack
        self.sem = {e: stack.enter_context(nc.semaphore("sem_" + e)) for e in self.ENGS}
        self.cnt = {e: 0 for e in self.ENGS}
        self.streams = {e: [] for e in self.ENGS}
        self.waited = {}
        self.dsem = {}
        self.ninstr = 0
        self.hook = None
        self.hold = False

    def _wait(self, eng, ev):
        if ev is None:
            return
        key, val = ev
        if key == "pe" and eng == "pe":
            return
        k = (eng, key)
        if self.waited.get(k, 0) >= val:
            return
        self.waited[k] = val
        sem = self.sem[key] if key in self.sem else self.dsem[key][0]
        self.streams[eng].append(lambda e, sem=sem, val=val: e.wait_ge(sem, val))
        self.ninstr += 1

    def _deps(self, eng, reads, writes):
        mx = {}
        for b in reads:
            if b.w is not None:
                mx[b.w[0]] = max(mx.get(b.w[0], 0), b.w[1])
            if b.excl:
                for k, v in b.r:
                    if k != eng:
                        mx[k] = max(mx.get(k, 0), v)
        for b in writes:
            if b.w is not None:
                mx[b.w[0]] = max(mx.get(b.w[0], 0), b.w[1])
            for k, v in b.r:
                mx[k] = max(mx.get(k, 0), v)
        for k, v in mx.items():
            self._wait(eng, (k, v))

    def _commit(self, ev, reads, writes):
        for b in writes:
            b.w = ev
            b.r = []
        for b in reads:
            if b not in writes:
                b.r.append(ev)
                if len(b.r) > 48:
                    mx = {}
                    for k, v in b.r:
                        mx[k] = max(mx.get(k, 0), v)
                    b.r = list(mx.items())

    def op(self, eng, fn, reads=(), writes=(), inc=True):
        if self.ninstr > KLIMIT:
            return None
        if self.hook is not None and not self.hold:
            self.hook()
        self.hold = not inc
        if eng == "pool" and NOPOOL:
            eng = "dve"
        self._deps(eng, reads, writes)
        if os.environ.get("KLIST"):
            import sys
            f = sys._getframe(1)
            while f.f_code.co_name in ("tt", "ts", "stt", "cp", "act", "mm", "red", "dma", "dbg", "op", "<lambda>"):
                f = f.f_back
            print("INSTR", self.ninstr, eng, "line", f.f_lineno)
        if inc:
            self.cnt[eng] += 1
            ev = (eng, self.cnt[eng])
            sem = self.sem[eng]
            self.streams[eng].append(lambda e, fn=fn, sem=sem: fn(e).then_inc(sem, 1))
        else:
            ev = (eng, self.cnt[eng] + 1)
            self.streams[eng].append(lambda e, fn=fn: fn(e))
        self.ninstr += 1
        self._commit(ev, reads, writes)
        return ev

    def dma(self, eng, fn, reads=(), writes=(), key="d"):
        if self.ninstr > KLIMIT:
            return None
        if self.hook is not None and not self.hold:
            self.hook()
        if key not in self.dsem:
            self.dsem[key] = [self.stack.enter_context(self.nc.semaphore("dsem_" + key)), 0]
        self._deps(eng, reads, writes)
        self.dsem[key][1] += 16
        ev = (key, self.dsem[key][1])
        sem = self.dsem[key][0]
        self.streams[eng].append(lambda e, fn=fn, sem=sem: fn(e).then_inc(sem, 16))
        self.ninstr += 1
        self._commit(ev, reads, writes)
        return ev

    def barrier(self, exclude=()):
        evs = [(e, self.cnt[e]) for e in self.ENGS if self.cnt[e] > 0]
        evs += [(k, v[1]) for k, v in self.dsem.items() if v[1] > 0 and k not in exclude]
        for e in self.ENGS:
            for ev in evs:
                self._wait(e, ev)

    def emit(self):
        with self.nc.Block() as blk:
            @blk.tensor
            def _(e):
                for f in self.streams["pe"]:
                    f(e)

            @blk.scalar
            def _(e):
                for f in self.streams["act"]:
                    f(e)

            @blk.vector
            def _(e):
                for f in self.streams["dve"]:
                    f(e)

            @blk.gpsimd
            def _(e):
                for f in self.streams["pool"]:
                    f(e)

            @blk.sync
            def _(e):
                for f in self.streams["sp"]:
                    f(e)


def interleave(P, fa, fb):
    cv = threading.Condition()
    st = {"turn": 0, "alive": [True, True], "exc": None}

    def hook():
        me = int(threading.current_thread().name[-1])
        with cv:
            if st["alive"][1 - me]:
                st["turn"] = 1 - me
                cv.notify_all()
                while st["turn"] != me:
                    cv.wait()

    def run(i, f):
        with cv:
            while st["turn"] != i:
                cv.wait()
        try:
            f()
        except BaseException as ex:
            st["exc"] = ex
        finally:
            with cv:
                st["alive"][i] = False
                st["turn"] = 1 - i
                cv.notify_all()

    P.hook = hook
    P.hold = False
    ts_ = [threading.Thread(target=run, args=(i, f), name="il%d" % i) for i, f in enumerate((fa, fb))]
    for t in ts_:
        t.start()
    for t in ts_:
        t.join()
    P.hook = None
    if st["exc"] is not None:
        raise st["exc"]


class Tl:
    def __init__(self, t, name):
        self.t = t
        self.b = Buf(name)

    def __getitem__(self, k):
        return self.t[k]


def _bl(xs):
    return [x.b if isinstance(x, Tl) else x for x in xs]


def build_program(dbg_chunk=None, nch=NCH):
    nc = bass.Bass("TRN2", target_bir_lowering=False)
    dram_in = {}

    def DI(name, shape, dt=F32):
        dram_in[name] = nc.dram_tensor(name, list(shape), dt, kind="ExternalInput").ap()
        return dram_in[name]

    x_d = DI("x", [T, D]); cfm_d = DI("c_fm", [128, 8])
    wada_d = DI("w_ada", [D, 6 * D]); bada_fm_d = DI("b_ada_fm", [128, 48]); bada_row_d = DI("b_ada_row", [1, 6 * D])
    g1_d = DI("g1_fm", [128, 8]); g2_d = DI("g2_row", [1, D])
    win_d = DI("w_in", [D, 2560]); mu_d = DI("mu_fm", [128, 14])
    ch_d = DI("chp", [128, 5, 4])
    lnx_d = DI("lnx_row", [1, 1024])
    lora_d = DI("lora_up", [128, 512]); gup_d = DI("g_up", [128, 512])
    qk_d = DI("qkg", [128, 2]); sink_d = DI("sink_fm", [128, 4])
    wout_d = DI("w_out", [D, D]); wr_d = DI("w_router", [D, 32]); br_d = DI("b_router", [1, 32])
    nexp = 32 if dbg_chunk is None else 1
    w1_d = DI("w1p", [nexp * 128, 8 * 2048]); w2_d = DI("w2p", [nexp * 128, 8 * 1024])
    b1_d = DI("b12p", [32 * 128, 16 + 1024])
    cst_d = DI("consts", [128, 14, 128]); cst2_d = DI("consts2", [128, MB + 3]); bst_d = DI("bstart", [128, NB * 32]); zer_d = DI("zeros", [1024, D], BF16)
    out_d = nc.dram_tensor("out", [T, D], F32, kind="ExternalOutput").ap()
    hbuf_d = nc.dram_tensor("hbuf", [T, D], BF16).ap()
    nsl = NSLOT if dbg_chunk is None else 128
    xs_d = nc.dram_tensor("xs", [nsl, D], BF16).ap()
    ys_d = nc.dram_tensor("ys", [nsl, D], F32).ap()
    dbg_out = {}

    with ExitStack() as st0:
        P = Prog(nc, st0)

        def sbuf(st, name, shape, dt=F32):
            return Tl(st.enter_context(nc.sbuf_tensor("s_" + name, list(shape), dt)), name)

        def E(eng):
            return eng

        def tt(eng, out, in0, in1, op, R, W):
            return P.op(eng, lambda e: e.tensor_tensor(out=out, in0=in0, in1=in1, op=op), _bl(R), _bl(W))

        def ts(eng, out, in0, s1, s2, op0, op1, R, W):
            if s2 is None:
                return P.op(eng, lambda e: e.tensor_scalar(out=out, in0=in0, scalar1=s1, scalar2=None, op0=op0), _bl(R), _bl(W))
            return P.op(eng, lambda e: e.tensor_scalar(out=out, in0=in0, scalar1=s1, scalar2=s2, op0=op0, op1=op1), _bl(R), _bl(W))

        def stt(eng, out, in0, sc, in1, op0, op1, R, W):
            return P.op(eng, lambda e: e.scalar_tensor_tensor(out=out, in0=in0, scalar=sc, in1=in1, op0=op0, op1=op1), _bl(R), _bl(W))

        def cp(eng, out, in_, R, W):
            if eng == "act":
                return P.op(eng, lambda e: e.copy(out=out, in_=in_), _bl(R), _bl(W))
            return P.op(eng, lambda e: e.tensor_copy(out=out, in_=in_), _bl(R), _bl(W))

        def act(out, in_, func, R, W, bias=None, scale=None, accum=None):
            kw = {}
            if bias is not None:
                kw["bias"] = bias
            if scale is not None:
                kw["scale"] = scale
            if accum is not None:
                kw["accum_out"] = accum
            return P.op("act", lambda e: e.activation(out=out, in_=in_, func=func, **kw), _bl(R), _bl(W))

        def mm(out, lhsT, rhs, R, W, start=True, stop=True, inc=True):
            return P.op("pe", lambda e: e.matmul(out, lhsT=lhsT, rhs=rhs, start=start, stop=stop), _bl(R), _bl(W), inc=inc)

        def red(eng, out, in_, R, W, op=ALU.add):
            return P.op(eng, lambda e: e.tensor_reduce(out=out, in_=in_, axis=AX.X, op=op), _bl(R), _bl(W))

        def dma(eng, out, in_, R, W, key):
            return P.dma(eng, lambda e: e.dma_start(out=out, in_=in_), _bl(R), _bl(W), key=key)

        regcache = {}

        def bc(e, v):
            if v not in regcache:
                regcache[v] = e.to_reg(v)
            return regcache[v]

        def mark(label):
            if os.environ.get("KMARK"):
                print("MARK", label, P.ninstr)

        def dbg(name, tile_ap, shape, dt, R):
            if dbg_chunk is None:
                return
            o = nc.dram_tensor("dbg_" + name, list(shape), dt, kind="ExternalOutput").ap()
            dbg_out[name] = o
            dma("sp", o, tile_ap, R, [], "dbg_" + name)

        psum_t = st0.enter_context(nc.psum_tensor("psum", [128, 8, 512], F32))
        banks = [Tl(psum_t, "bank%d" % i) for i in range(8)]
        for bk_ in banks:
            bk_.b.excl = True
        rr = [0, 0, 0]

        def _pool():
            if P.hook is None:
                return 0, 0, 8
            t = int(threading.current_thread().name[-1])
            return 1 + t, 4 * t, 4

        def bank():
            k, base, n = _pool()
            i = base + rr[k] % n
            rr[k] += 1
            return i, banks[i]

        def bank2():
            k, base, n = _pool()
            while rr[k] % 2:
                rr[k] += 1
            i = base + rr[k] % n
            rr[k] += 2
            return i, [banks[i], banks[i + 1]]

        def pv(i, n=1):
            return psum_t[:, i:i + n, :].rearrange("p a n -> p (a n)")

        def pvb(i):
            return psum_t[:, i, :].bitcast(BF16)

        cst = sbuf(st0, "cst", [128, 14, 128], BF16)
        cstf = sbuf(st0, "cstf", [128, 2, 128], F32)
        cst2 = sbuf(st0, "cst2", [128, MB + 3], F32)
        dma("pool", cst[:], cst_d, [], [cst], "c0")
        dma("sp", cstf[:, 0, :], cst_d[:, 0, :], [], [cstf], "c1")
        dma("sp", cstf[:, 1, :], cst_d[:, 5, :], [], [cstf], "c1")
        dma("sp", cst2[:], cst2_d, [], [cst2], "c2")
        ident = cst[:, 0, :]; tri_lt = cst[:, 1, :]; tri_le = cst[:, 2, :]; tri_gt = cst[:, 3, :]
        bdiag = cst[:, 4, :]; ones_bf = cst[:, 5, :]
        ident_f = cstf[:, 0, :]; ones_f = cstf[:, 1, :]
        thr8 = cst2[:, 0:MB]
        bsel_f = cst2[:, MB:MB + 2]
        iota_p = cst2[:, MB + 2:MB + 3]
        bsel = sbuf(st0, "bsel", [128, 2], BF16)
        cp("dve", bsel[:], bsel_f, [cst2], [bsel])

        lg_all = sbuf(st0, "lg_all", [128, NCH, 32])
        top_all = sbuf(st0, "top_all", [128, NCH, 8])
        gate_all = sbuf(st0, "gate_all", [128, NCH, 4])
        pos_all = sbuf(st0, "pos_all", [128, NCH, 32])
        carry = sbuf(st0, "carry", [128, 32])
        gt2_b = sbuf(st0, "gt2_b", [128, D])
        woff = sbuf(st0, "woff", [128, NB], I32)
        dest = sbuf(st0, "dest", [128, NCH, 4], I32)
        wofc = [sbuf(st0, "wofc%d" % i, [128, 1], I32) for i in range(2)]
        dcur = [sbuf(st0, "dcur%d" % i, [128, 4], I32) for i in range(2)]
        P.op("dve", lambda e: e.memset(carry[:], 0.0), [], _bl([carry]))

        with ExitStack() as st1:
            modfm = sbuf(st1, "modfm", [128, 32])
            s1 = sbuf(st1, "s1", [128, 8])
            s2_b = sbuf(st1, "s2_b", [128, D]); sh2_b = sbuf(st1, "sh2_b", [128, D])
            lnx_b = sbuf(st1, "lnx_b", [128, 1024])
            chp = sbuf(st1, "chp", [128, 5, 4]); mu = sbuf(st1, "mu", [128, 14]); g1 = sbuf(st1, "g1", [128, 8])
            qkg = sbuf(st1, "qkg", [128, 2]); esink = sbuf(st1, "esink", [128, 4])
            brt = sbuf(st1, "brt", [128, 32])
            omka = sbuf(st1, "omka", [128, 4])
            w_in = sbuf(st1, "w_in", [128, 8, 2560], BF16)
            w_out = sbuf(st1, "w_out", [128, 8, D], BF16)
            lora_up = sbuf(st1, "lora_up", [128, 512], BF16); g_up = sbuf(st1, "g_up", [128, 512], BF16)
            wr = sbuf(st1, "wr", [128, 8, 32], BF16)

            dma("sp", chp[:], ch_d, [], [chp], "p0"); dma("sp", mu[:], mu_d, [], [mu], "p1")
            dma("sp", g1[:], g1_d, [], [g1], "p2"); dma("sp", qkg[:], qk_d, [], [qkg], "p3")
            dma("sp", esink[:], sink_d, [], [esink], "p4")
            dma("sp", brt[:], br_d.partition_broadcast(128), [], [brt], "p5")
            dma("sp", lnx_b[:], lnx_d.partition_broadcast(128), [], [lnx_b], "p6")
            dma("pool", lora_up[:], lora_d, [], [lora_up], "p7"); dma("pool", g_up[:], gup_d, [], [g_up], "p8")
            dma("pool", wr[:], wr_d.rearrange("(k p) e -> p k e", p=128), [], [wr], "p9")
            for kc in range(8):
                dma("pool", w_in[:, kc, :], win_d[kc * 128:(kc + 1) * 128, :], [], [w_in], "wi")
                dma("pool", w_out[:, kc, :], wout_d[kc * 128:(kc + 1) * 128, :], [], [w_out], "wo")
            act(esink[:], esink[:], AF.Exp, [esink], [esink])
            ts("dve", omka[:], chp[:, 3, :], -1.0, 1.0, ALU.mult, ALU.add, [chp], [omka])
            ts("dve", qkg[:, 0:1], qkg[:, 0:1], 0.125, None, ALU.mult, None, [qkg], [qkg])

            with ExitStack() as st2:
                cfm = sbuf(st2, "cfm", [128, 8]); sc32 = sbuf(st2, "sc32", [128, 8])
                screp = sbuf(st2, "screp", [128, 8, 128])
                badafm = sbuf(st2, "badafm", [128, 48])
                wblk = [sbuf(st2, "wblk%d" % i, [128, 8, 512]) for i in range(2)]
                brow = [sbuf(st2, "brow%d" % i, [128, 512]) for i in range(2)]
                g2b = sbuf(st2, "g2b", [128, D]); gt1_b = sbuf(st2, "gt1_b", [128, D])
                dma("sp", cfm[:], cfm_d, [], [cfm], "a0"); dma("sp", badafm[:], bada_fm_d, [], [badafm], "a1")
                dma("sp", g2b[:], g2_d.partition_broadcast(128), [], [g2b], "a2")
                act(sc32[:], cfm[:], AF.Silu, [cfm], [sc32])
                for kc in range(8):
                    cp("dve", screp[:, kc, :], sc32[:, kc:kc + 1].to_broadcast([128, 128]), [sc32], [screp])
                wv = wada_d.rearrange("(k p) n -> p k n", p=128)
                bdst = {4: gt1_b[:, 0:512], 5: gt1_b[:, 512:1024], 6: sh2_b[:, 0:512], 7: sh2_b[:, 512:1024],
                        8: s2_b[:, 0:512], 9: s2_b[:, 512:1024], 10: gt2_b[:, 0:512], 11: gt2_b[:, 512:1024]}
                btl = {4: gt1_b, 5: gt1_b, 6: sh2_b, 7: sh2_b, 8: s2_b, 9: s2_b, 10: gt2_b, 11: gt2_b}
                bi_fm, bk_fm = bank()
                for blk in range(12):
                    wb = wblk[blk % 2]
                    dma("sp", wb[:], wv[:, :, blk * 512:(blk + 1) * 512], [], [wb], "wa%d" % (blk % 2))
                    if blk < 4:
                        for j in range(4):
                            col = blk * 4 + j
                            for kc in range(8):
                                mm(pv(bi_fm)[:, col:col + 1], wb[:, kc, j * 128:(j + 1) * 128], sc32[:, kc:kc + 1],
                                   [wb, sc32], [bk_fm], start=(kc == 0), stop=(kc == 7), inc=(kc == 7))
                    else:
                        br_ = brow[blk % 2]
                        dma("sp", br_[:], bada_row_d[:, blk * 512:(blk + 1) * 512].partition_broadcast(128), [], [br_], "wr%d" % (blk % 2))
                        bi, bk = bank()
                        for kc in range(8):
                            mm(pv(bi), screp[:, kc, :], wb[:, kc, :], [wb, screp], [bk], start=(kc == 0), stop=(kc == 7), inc=(kc == 7))
                        tt("dve", bdst[blk], pv(bi), br_[:], ALU.add, [bk, br_], [btl[blk]])
                    if blk == 3:
                        tt("dve", modfm[:, 0:16], pv(bi_fm)[:, 0:16], badafm[:, 0:16], ALU.add, [bk_fm, badafm], [modfm])
                stt("dve", s1[:], modfm[:, 8:16], 1.0, g1[:], ALU.add, ALU.mult, [modfm, g1], [s1])
                stt("dve", s2_b[:], s2_b[:], 1.0, g2b[:], ALU.add, ALU.mult, [s2_b, g2b], [s2_b])
                for kc in range(8):
                    tt("dve", w_out[:, kc, :], w_out[:, kc, :], gt1_b[:], ALU.mult, [w_out, gt1_b], [w_out])
                if dbg_chunk is None:
                    for q in range(0, NSLOT, 1024):
                        r_ = min(1024, NSLOT - q)
                        dma("sp", xs_d[q:q + r_, :], zer_d[0:r_, :], [], [], "zf")
                P.barrier(exclude=("zf",))
            sh1 = modfm[:, 0:8]

            xt = [sbuf(st1, "xt%d" % i, [128, D]) for i in range(2)]
            xn = sbuf(st1, "xn", [128, D], BF16)
            sm = sbuf(st1, "sm", [128, 16]); smB = sbuf(st1, "smB", [128, 16])
            hT = sbuf(st1, "hT", [128, 8, 128], BF16)
            pbuf = sbuf(st1, "pbuf", [128, 14, 129])
            psh = sbuf(st1, "psh", [128, 14, 128])
            lo_bf = sbuf(st1, "lo_bf", [128, 128], BF16); sg_bf = sbuf(st1, "sg_bf", [128, 128], BF16)
            sw = sbuf(st1, "sw", [128, 4, 128]); aa = sbuf(st1, "aa", [128, 4, 128])
            cs = sbuf(st1, "cs", [128, 4, 128])
            csC = sbuf(st1, "csC", [128, 4])
            gC = [sbuf(st1, "gC%d" % i, [128, 4]) for i in range(2)]
            kk = sbuf(st1, "kk", [128, 4, 128])
            k2 = sbuf(st1, "k2", [128, 4, 128]); beta = sbuf(st1, "beta", [128, 4, 128])
            e1 = sbuf(st1, "e1", [128, 4, 128]); e2 = sbuf(st1, "e2", [128, 4, 128])
            abp = sbuf(st1, "abp", [128, 4, 2, 128], BF16)
            kbar = sbuf(st1, "kbar", [128, 4, 128], BF16); bbar = sbuf(st1, "bbar", [128, 4, 128], BF16)
            fm3 = sbuf(st1, "fm3", [128, 2, 4, 128], BF16)
            wrk = sbuf(st1, "wrk", [128, 4, 128], BF16)
            tm3 = sbuf(st1, "tm3", [128, 3, 512], BF16)
            v32 = sbuf(st1, "v32", [128, 512]); vtm = sbuf(st1, "vtm", [128, 512], BF16)
            gtm = sbuf(st1, "gtm", [128, 512])
            atb = sbuf(st1, "atb", [128, 8, 256], BF16); atk = sbuf(st1, "atk", [128, 8, 256], BF16)
            Lf = [sbuf(st1, "Lf%d" % g, [128, 4, 2, 128], BF16) for g in range(2)]
            Pq = [[sbuf(st1, "Pq%d_%d" % (g, i), [128, 4, 2, 128], BF16) for i in range(2)] for g in range(2)]
            Mq = [[sbuf(st1, "Mq%d_%d" % (g, i), [128, 4, 2, 128], BF16) for i in range(2)] for g in range(2)]
            Xb0 = sbuf(st1, "Xb0", [128, 8, 128], BF16); Xbf = sbuf(st1, "Xbf", [128, 8, 128], BF16)
            RpT = sbuf(st1, "RpT", [128, 4, 128], BF16)
            Y0 = sbuf(st1, "Y0", [128, 512]); TpT = sbuf(st1, "TpT", [128, 4, 64], BF16); D32 = sbuf(st1, "D32", [128, 4, 64])
            H32 = sbuf(st1, "H32", [128, 4, 64]); Hbf = [sbuf(st1, "Hbf%d" % i, [128, 4, 64], BF16) for i in range(2)]
            yy = sbuf(st1, "yy", [128, 8, 64]); ysq = sbuf(st1, "ysq", [128, 8, 64])
            gn = sbuf(st1, "gn", [128, 6, 8]); bon = sbuf(st1, "bon", [128, 8])
            obf = sbuf(st1, "obf", [128, 512], BF16)
            mixR = sbuf(st1, "mixR", [128, 4, 128], BF16); matt = [sbuf(st1, "matt%d" % i, [128, 4, 128], BF16) for i in range(2)]
            qsq = sbuf(st1, "qsq", [128, 5, 128], BF16); qrs = sbuf(st1, "qrs", [128, 5, 128])
            qhat = sbuf(st1, "qhat", [128, 4, 128], BF16)
            khat = [sbuf(st1, "khat%d" % i, [128, 128], BF16) for i in range(2)]
            vat = [sbuf(st1, "vat%d" % i, [128, 128], BF16) for i in range(2)]
            ptm = [sbuf(st1, "ptm%d" % i, [128, 4, 128], BF16) for i in range(4)]
            h2 = [sbuf(st1, "h2_0", [128, D], BF16)] * 2
            msk = sbuf(st1, "msk", [128, 32], BF16); ex4 = sbuf(st1, "ex4", [128, 4])

            P.op("dve", lambda e: e.memset(pbuf[:], 0.0), [], _bl([pbuf]))
            if os.environ.get("KMARK"):
                print("SBUF remaining after mixer alloc", nc.sbuf_bytes_remaining)
            P.op("dve", lambda e: e.memset(H32[:], 0.0), [], _bl([H32]))
            P.op("dve", lambda e: e.memset(Hbf[0][:], 0.0), [], _bl([Hbf[0]]))

            nchunks = nch if dbg_chunk is None else dbg_chunk + 1
            def Fe(n):
                dg = (n == dbg_chunk)
                xc = xt[n % 2]
                dma("sp", xc[:], x_d[n * 128:(n + 1) * 128, :], [], [xc], "x%d" % (n % 2))
                mark("norm1")
                act(xn[:], xc[:], AF.Square, [xc], [xn, sm], accum=sm[:, 0:1])
                ts("dve", sm[:, 1:2], sm[:, 0:1], 1.0 / D, NORM_EPS, ALU.mult, ALU.add, [sm], [sm])
                act(sm[:, 1:2], sm[:, 1:2], AF.Sqrt, [sm], [sm])
                P.op("dve", lambda e: e.reciprocal(out=sm[:, 2:3], in_=sm[:, 1:2]), _bl([sm]), _bl([sm]))
                act(xn[:], xc[:], AF.Copy, [xc, sm], [xn], scale=sm[:, 2:3])
                bi, bk = bank()
                for kc in range(8):
                    P.op("pe", lambda e, kc=kc, bi=bi: e.transpose(pvb(bi)[:, kc * 128:(kc + 1) * 128], xn[:, kc * 128:(kc + 1) * 128], ident),
                         _bl([xn, cst]), _bl([bk]), inc=(kc == 7))
                for kc in range(8):
                    act(hT[:, kc, :], pvb(bi)[:, kc * 128:(kc + 1) * 128], AF.Identity, [bk, s1, modfm], [hT],
                        bias=sh1[:, kc:kc + 1], scale=s1[:, kc:kc + 1])
                if dg:
                    dbg("hT", hT[:].rearrange("p a n -> p (a n)"), [128, 1024], BF16, [hT])
                mark("inproj")
                pb = []
                for grp in range(5):
                    bi, bk = bank()
                    pb.append((bi, bk))
                    for j in range(4):
                        fc = grp * 4 + j
                        if fc == 19:
                            for kc in range(8):
                                mm(pv(bi)[:, j * 128:(j + 1) * 128], hT[:, kc, :], w_in[:, kc, 2432:2560], [hT, w_in], [bk],
                                   start=(kc == 0), stop=(kc == 7), inc=(kc == 7))
                            continue
                        col = fc * 128
                        for kc in range(8):
                            mm(pv(bi)[:, j * 128:(j + 1) * 128], w_in[:, kc, col:col + 128], hT[:, kc, :], [hT, w_in], [bk],
                               start=(kc == 0), stop=(kc == 7), inc=(kc == 7))
                    if grp < 4:
                        nj = 4 if grp < 3 else 2
                        cp("act" if grp % 2 == 0 else "dve", pbuf[:, grp * 4:grp * 4 + nj, 1:129], pv(bi)[:, 0:nj * 128].rearrange("p (a n) -> p a n", a=nj), [bk], [pbuf])
                mark("tokshift")
                tt("dve", psh[:], pbuf[:, :, 0:128], pbuf[:, :, 1:129], ALU.subtract, [pbuf], [psh])
                tt("pool", psh[:], psh[:], mu[:].unsqueeze(2).to_broadcast([128, 14, 128]), ALU.mult, [psh, mu], [psh])
                tt("dve", psh[:], psh[:], pbuf[:, :, 1:129], ALU.add, [psh, pbuf], [psh])
                cp("pool", pbuf[:, :, 0:1], pbuf[:, :, 128:129], [pbuf], [pbuf])
                if dg:
                    dbg("psh", psh[:].rearrange("p a n -> p (a n)"), [128, 14 * 128], F32, [psh])
                rr_ = psh[:, 0:4, :]; kr_ = psh[:, 4:8, :]; vr_ = psh[:, 8:12, :]
                mark("qknorm")
                qbi, qbk = pb[3][0], pb[3][1]
                q2bi, q2bk = pb[4]
                qv = [pv(qbi)[:, 256:384], pv(qbi)[:, 384:512], pv(q2bi)[:, 0:128], pv(q2bi)[:, 128:256], pv(q2bi)[:, 256:384]]
                qb_ = [qbk, qbk, q2bk, q2bk, q2bk]
                for j in range(5):
                    act(qsq[:, j, :], qv[j], AF.Square, [qb_[j]], [qsq])
                bi, bk = bank(); bi2, bk2 = bank()
                for j in range(5):
                    bb_i, bb_k = (bi, bk) if j < 4 else (bi2, bk2)
                    mm(pv(bb_i)[:, (j % 4) * 128:(j % 4 + 1) * 128], bdiag, qsq[:, j, :], [qsq, cst], [bb_k], inc=(j >= 3))
                ts("dve", qrs[:, 0:4, :], pv(bi).rearrange("p (a n) -> p a n", a=4), 1.0 / 64, NORM_EPS, ALU.mult, ALU.add, [bk], [qrs])
                ts("dve", qrs[:, 4, :], pv(bi2)[:, 0:128], 1.0 / 64, NORM_EPS, ALU.mult, ALU.add, [bk2], [qrs])
                act(qrs[:], qrs[:], AF.Sqrt, [qrs], [qrs])
                P.op("dve", lambda e: e.reciprocal(out=qrs[:], in_=qrs[:]), _bl([qrs]), _bl([qrs]))
                for j in range(4):
                    stt("dve", qhat[:, j, :], qv[j], qkg[:, 0:1], qrs[:, j, :], ALU.mult, ALU.mult, [qb_[j], qkg, qrs], [qhat])
                kc_ = khat[n % 2]; kp_ = khat[(n + 1) % 2]
                vc_ = vat[n % 2]; vp_ = vat[(n + 1) % 2]
                stt("dve", kc_[:], qv[4], qkg[:, 1:2], qrs[:, 4, :], ALU.mult, ALU.mult, [q2bk, qkg, qrs], [kc_])
                cp("act", vc_[:], pv(q2bi)[:, 384:512], [q2bk], [vc_])
                if dg:
                    dbg("qhat", qhat[:].rearrange("p a n -> p (a n)"), [128, 512], BF16, [qhat])
                    dbg("khat", kc_[:], [128, 128], BF16, [kc_])
                kcur[0] = (kc_, kp_, vc_, vp_)
            def Fl(n):
                dg = (n == dbg_chunk)
                xc = xt[n % 2]
                kc_, kp_, vc_, vp_ = khat[n % 2], khat[(n + 1) % 2], vat[n % 2], vat[(n + 1) % 2]
                rr_ = psh[:, 0:4, :]; kr_ = psh[:, 4:8, :]; vr_ = psh[:, 8:12, :]
                mark("attn")
                kbs = ([(kp_, vp_, tri_gt)] if n > 0 else []) + [(kc_, vc_, tri_le)]
                pts = []
                for kv in range(2):
                    Pp = slice(64 * kv, 64 * kv + 64)
                    for ib, (kt_, vt_, mk_) in enumerate(kbs):
                        bi, bk = bank()
                        mm(pv(bi).rearrange("p (a n) -> p a n", a=4), kt_[Pp, :], qhat[Pp, :, :], [kt_, qhat], [bk])
                        pm_ = ptm[kv * 2 + ib]
                        act(pm_[:], pv(bi).rearrange("p (a n) -> p a n", a=4), AF.Exp, [bk], [pm_])
                        tt("pool", pm_[:], pm_[:], mk_.unsqueeze(1).to_broadcast([128, 4, 128]), ALU.mult, [pm_, cst], [pm_])
                        pts.append((kv, pm_, vt_))
                obi, obk = bank(); dbi, dbk = bank()
                for kv in range(2):
                    Pp = slice(64 * kv, 64 * kv + 64)
                    lst = [p_ for p_ in pts if p_[0] == kv]
                    for ii, (_, pm_, vt_) in enumerate(lst):
                        mm(pv(obi)[Pp, :], vt_[:, Pp], pm_[:].rearrange("p a n -> p (a n)"), [vt_, pm_], [obk],
                           start=(ii == 0), stop=(ii == len(lst) - 1), inc=False)
                    for ii, (_, pm_, vt_) in enumerate(lst):
                        mm(pv(dbi)[Pp, :], ones_bf[:, 0:64], pm_[:].rearrange("p a n -> p (a n)"), [pm_, cst], [dbk],
                           start=(ii == 0), stop=(ii == len(lst) - 1), inc=(kv == 1 and ii == len(lst) - 1))
                den = e1[:]
                tt("dve", den, pv(dbi).rearrange("p (a n) -> p a n", a=4), esink[:].unsqueeze(2).to_broadcast([128, 4, 128]), ALU.add, [dbk, esink], [e1])
                P.op("dve", lambda e: e.reciprocal(out=den, in_=den), _bl([e1]), _bl([e1]))
                tt("dve", matt[n % 2][:], pv(obi).rearrange("p (a n) -> p a n", a=4), den, ALU.mult, [obk, e1], [matt[n % 2]])

                mark("rwkvprep")
                act(lo_bf[0:64, :], psh[0:64, 12, :], AF.Tanh, [psh], [lo_bf])
                cp("pool", lo_bf[64:128, :], psh[64:128, 12, :], [psh], [lo_bf])
                act(sg_bf[:], psh[:, 13, :], AF.Sigmoid, [psh], [sg_bf])
                zbi, zbk = bank(); abi, abk = bank(); gbi, gbk = bank()
                for c in range(4):
                    mm(pv(zbi)[:, c * 128:(c + 1) * 128], lora_up[0:64, c * 128:(c + 1) * 128], lo_bf[0:64, :], [lora_up, lo_bf], [zbk], inc=(c == 3))
                for c in range(4):
                    mm(pv(abi)[:, c * 128:(c + 1) * 128], lora_up[64:128, c * 128:(c + 1) * 128], lo_bf[64:128, :], [lora_up, lo_bf], [abk], inc=(c == 3))
                mm(pv(gbi), sg_bf[:], g_up[:], [sg_bf, g_up], [gbk])
                for c in range(4):
                    act(sw[:, c, :], pv(zbi)[:, c * 128:(c + 1) * 128], AF.Sigmoid, [zbk, chp], [sw], bias=chp[:, 0, c:c + 1])
                    act(aa[:, c, :], pv(abi)[:, c * 128:(c + 1) * 128], AF.Sigmoid, [abk, chp], [aa], bias=chp[:, 1, c:c + 1])
                cp("act", gtm[:], pv(gbi), [gbk], [gtm])
                for c in range(4):
                    P.op("dve", lambda e, c=c: e.tensor_tensor_scan(out=cs[:, c, :], data0=ones_f, data1=sw[:, c, :], initial=0.0,
                                                                     op0=ALU.mult, op1=ALU.add), _bl([sw, cstf]), _bl([cs]))
                ts("dve", csC[:], cs[:, :, 127], -DECAY_K, None, ALU.mult, None, [cs], [csC])
                gCn = gC[n % 2]
                act(gCn[:], csC[:], AF.Exp, [csC], [gCn])
                tt("dve", kk[:], kr_, chp[:, 2, :].unsqueeze(2).to_broadcast([128, 4, 128]), ALU.mult, [psh, chp], [kk])
                tt("pool", wrk[:], kk[:], kk[:], ALU.mult, [kk], [wrk])
                sbi, sbk = bank()
                for c in range(4):
                    mm(pv(sbi)[:, c * 128:(c + 1) * 128], bdiag, wrk[:, c, :], [wrk, cst], [sbk], inc=(c == 3))
                act(e2[:], pv(sbi).rearrange("p (a n) -> p a n", a=4), AF.Sqrt, [sbk], [e2])
                ts("dve", e2[:], e2[:], 1e-12, None, ALU.max, None, [e2], [e2])
                P.op("dve", lambda e: e.reciprocal(out=e2[:], in_=e2[:]), _bl([e2]), _bl([e2]))
                tt("dve", kk[:], kk[:], e2[:], ALU.mult, [kk, e2], [kk])
                tt("pool", k2[:], aa[:], chp[:, 3, :].unsqueeze(2).to_broadcast([128, 4, 128]), ALU.mult, [aa, chp], [k2])
                tt("pool", k2[:], k2[:], omka[:].unsqueeze(2).to_broadcast([128, 4, 128]), ALU.add, [k2, omka], [k2])
                tt("dve", k2[:], k2[:], kr_, ALU.mult, [k2, psh], [k2])
                tt("pool", beta[:], kk[:], aa[:], ALU.mult, [kk, aa], [beta])
                if dg:
                    dbg("sw", sw[:].rearrange("p a n -> p (a n)"), [128, 512], F32, [sw])
                    dbg("aa", aa[:].rearrange("p a n -> p (a n)"), [128, 512], F32, [aa])
                    dbg("kkn", kk[:].rearrange("p a n -> p (a n)"), [128, 512], F32, [kk])
                    dbg("k2", k2[:].rearrange("p a n -> p (a n)"), [128, 512], F32, [k2])
                mark("scaled")
                act(e1[:], cs[:], AF.Exp, [cs], [e1], scale=-DECAY_K)
                tt("dve", abp[:, :, 1, :], rr_, e1[:], ALU.mult, [psh, e1], [abp])
                act(e2[:], cs[:], AF.Exp, [cs], [e2], scale=DECAY_K)
                tt("pool", kbar[:], k2[:], e2[:], ALU.mult, [k2, e2], [kbar])
                tt("pool", bbar[:], beta[:], e2[:], ALU.mult, [beta, e2], [bbar])
                tt("pool", e1[:], cs[:], sw[:], ALU.subtract, [cs, sw], [e1])
                act(e1[:], e1[:], AF.Exp, [e1], [e1], scale=-DECAY_K)
                stt("dve", abp[:, :, 0, :], kk[:], -1.0, e1[:], ALU.mult, ALU.mult, [kk, e1], [abp])
                for c in range(4):
                    act(e2[:, c, :], cs[:, c, :], AF.Exp, [cs, csC], [e2], bias=csC[:, c:c + 1], scale=DECAY_K)
                tt("dve", fm3[:, 0, :, :], k2[:], e2[:], ALU.mult, [k2, e2], [fm3])
                tt("pool", fm3[:, 1, :, :], beta[:], e2[:], ALU.mult, [beta, e2], [fm3])
                ysq4 = ysq[:].rearrange("p h n -> p (h n)").rearrange("p (a n) -> p a n", a=4)
                tt("pool", ysq4, rr_, k2[:], ALU.mult, [psh, k2], [ysq])
                tt("pool", wrk[:], ysq4, chp[:, 4, :].unsqueeze(2).to_broadcast([128, 4, 128]), ALU.mult, [ysq, chp], [wrk])
                mark("transp")
                tb0, tk0 = bank(); tb1, tk1 = bank()
                for a3 in range(3):
                    for c in range(4):
                        idx = a3 * 4 + c
                        tb_, tk_ = (tb0, tk0) if idx < 8 else (tb1, tk1)
                        off = (idx % 8) * 128
                        src_ = abp[:, c, 0, :] if a3 == 0 else fm3[:, a3 - 1, c, :]
                        P.op("pe", lambda e, src_=src_, tb_=tb_, off=off: e.transpose(pvb(tb_)[:, off:off + 128], src_, ident),
                             _bl([fm3, abp, cst]), _bl([tk_]), inc=(idx == 7 or idx == 11))
                cp("act", tm3[:, 0:2, :], pvb(tb0).rearrange("p (a n) -> p a n", a=2), [tk0], [tm3])
                cp("dve", tm3[:, 2, :], pvb(tb1)[:, 0:512], [tk1], [tm3])
                vb_, vk_ = bank()
                for c in range(4):
                    P.op("pe", lambda e, c=c, vb_=vb_: e.transpose(pv(vb_)[:, c * 128:(c + 1) * 128], psh[:, 8 + c, :], ident_f),
                         _bl([psh, cstf]), _bl([vk_]), inc=(c == 3))
                cp("act", v32[:], pv(vb_), [vk_], [v32])
                cp("dve", vtm[:], pv(vb_), [vk_], [vtm])
                A_TM = tm3[:, 0, :]; Kt_TM = tm3[:, 1, :]; Bt_TM = tm3[:, 2, :]
            def B1(n):
                dg = (n == dbg_chunk)
                xc = xt[n % 2]
                gCn = gC[n % 2]
                A_TM = tm3[:, 0, :]; Kt_TM = tm3[:, 1, :]; Bt_TM = tm3[:, 2, :]
                mark("chunkmat")
                mkAT = cst[:, 1:3, :].unsqueeze(1).to_broadcast([128, 2, 2, 128])
                for c in range(4):
                    g, cc = c // 2, c % 2
                    ai2, ak2 = bank2(); li2, lk2 = bank2()
                    for hh in range(2):
                        Pp = slice(64 * hh, 64 * hh + 64)
                        mm(pv(ai2 + hh)[:, 0:256].rearrange("p (a n) -> p a n", a=2), bbar[Pp, c, :], abp[Pp, c, :, :], [bbar, abp], [ak2[hh]], inc=False)
                        mm(pv(ai2 + hh)[:, 256:512].rearrange("p (a n) -> p a n", a=2), kbar[Pp, c, :], abp[Pp, c, :, :], [kbar, abp], [ak2[hh]])
                        mm(pv(li2 + hh)[:, 0:128], abp[Pp, c, 0, :], bbar[Pp, c, :], [bbar, abp], [lk2[hh]])
                    vat_ = psum_t[:, ai2:ai2 + 2, :].rearrange("p h (s n) -> p h s n", s=4)
                    vl_ = psum_t[:, li2:li2 + 2, 0:128]
                    tt("dve", atb[:, 2 * c:2 * c + 2, :].rearrange("p h (a n) -> p h a n", a=2), vat_[:, :, 0:2, :], mkAT, ALU.mult, ak2 + [cst], [atb])
                    tt("dve", atk[:, 2 * c:2 * c + 2, :].rearrange("p h (a n) -> p h a n", a=2), vat_[:, :, 2:4, :], mkAT, ALU.mult, ak2 + [cst], [atk])
                    tt("dve", Pq[g][0][:, 2 * cc:2 * cc + 2, 1, :], vat_[:, :, 0, :], cst[:, 7, :].unsqueeze(1).to_broadcast([128, 2, 128]), ALU.mult, ak2 + [cst], [Pq[g][0]])
                    tt("dve", Lf[g][:, 2 * cc:2 * cc + 2, 0, :], vl_, tri_gt.unsqueeze(1).to_broadcast([128, 2, 128]), ALU.mult, lk2 + [cst], [Lf[g]])
                    tt("dve", Pq[g][0][:, 2 * cc:2 * cc + 2, 0, :], vl_, cst[:, 6, :].unsqueeze(1).to_broadcast([128, 2, 128]), ALU.mult, lk2 + [cst], [Pq[g][0]])
                idb = ident.unsqueeze(1).unsqueeze(1).to_broadcast([128, 4, 2, 128])
                for g in range(2):
                    cp("pool", Lf[g][:, :, 1, :], atb[:, 4 * g:4 * g + 4, 0:128], [atb], [Lf[g]])
                    xbi, xbk = bank()
                    for h4 in range(4):
                        h = 4 * g + h4
                        mm(pv(xbi)[:, h4 * 64:(h4 + 1) * 64], atk[:, h, 0:128], vtm[:, h * 64:(h + 1) * 64], [atk, vtm], [xbk], inc=(h4 == 3))
                    cp("act", Xb0[:, 4 * g:4 * g + 4, 64:128], pv(xbi)[:, 0:256].rearrange("p (h n) -> p h n", h=4), [xbk], [Xb0])
                    cp("pool", Xb0[:, 4 * g:4 * g + 4, 0:64], A_TM[:, 256 * g:256 * g + 256].rearrange("p (h n) -> p h n", h=4), [tm3], [Xb0])
                    tt("pool", Mq[g][0][:], Pq[g][0][:], idb, ALU.add, [Pq[g][0], cst], [Mq[g][0]])
                pc, mc = 0, 0
                for it in range(3):
                    for g in range(2):
                        Pc = Pq[g][pc]; Pn = Pq[g][1 - pc]; Mc = Mq[g][mc]; Mn = Mq[g][1 - mc]
                        si, sk = bank2()
                        for h4 in range(4):
                            mm(pv(si, 2)[:, h4 * 256:h4 * 256 + 128], Pc[:, h4, 1, :], Pc[:, h4, 0, :], [Pc], sk, inc=False)
                            mm(pv(si, 2)[:, h4 * 256 + 128:h4 * 256 + 256], Pc[:, h4, 0, :], Pc[:, h4, 1, :], [Pc], sk, inc=(h4 == 3))
                        cp("act", Pn[:], pv(si, 2).rearrange("p (h a n) -> p h a n", h=4, a=2), sk, [Pn])
                        gi_, gk_ = bank2()
                        for h4 in range(4):
                            mm(pv(gi_, 2)[:, h4 * 256:h4 * 256 + 128], Pn[:, h4, 1, :], Mc[:, h4, 0, :], [Pn, Mc], gk_, start=True, stop=False, inc=False)
                            mm(pv(gi_, 2)[:, h4 * 256:h4 * 256 + 128], ident, Mc[:, h4, 0, :], [Mc, cst], gk_, start=False, stop=True, inc=False)
                            mm(pv(gi_, 2)[:, h4 * 256 + 128:h4 * 256 + 256], Mc[:, h4, 0, :], Pn[:, h4, 1, :], [Pn, Mc], gk_, start=True, stop=False, inc=False)
                            mm(pv(gi_, 2)[:, h4 * 256 + 128:h4 * 256 + 256], ident, Mc[:, h4, 1, :], [Mc, cst], gk_, start=False, stop=True, inc=(h4 == 3))
                        cp("act", Mn[:], pv(gi_, 2).rearrange("p (h a n) -> p h a n", h=4, a=2), gk_, [Mn])
                    pc, mc = 1 - pc, 1 - mc
                for mi in (8, 10, 12):
                    for g in range(2):
                        Mc = Mq[g][mc]; Mn = Mq[g][1 - mc]; Zq = Pq[g][pc]
                        zi, zk = bank2()
                        for h4 in range(4):
                            mm(pv(zi, 2)[:, h4 * 256:h4 * 256 + 128], Lf[g][:, h4, 1, :], Mc[:, h4, 0, :], [Lf[g], Mc], zk, inc=False)
                            mm(pv(zi, 2)[:, h4 * 256 + 128:h4 * 256 + 256], Lf[g][:, h4, 0, :], Mc[:, h4, 1, :], [Lf[g], Mc], zk, inc=(h4 == 3))
                        tt("dve", Zq[:], pv(zi, 2).rearrange("p (h a n) -> p h a n", h=4, a=2), cst[:, mi:mi + 2, :].unsqueeze(1).to_broadcast([128, 4, 2, 128]), ALU.mult, zk + [cst], [Zq])
                        gi_, gk_ = bank2()
                        for h4 in range(4):
                            mm(pv(gi_, 2)[:, h4 * 256:h4 * 256 + 128], Mc[:, h4, 1, :], Zq[:, h4, 0, :], [Zq, Mc], gk_, start=True, stop=False, inc=False)
                            mm(pv(gi_, 2)[:, h4 * 256:h4 * 256 + 128], ident, Mc[:, h4, 0, :], [Mc, cst], gk_, start=False, stop=True, inc=False)
                            mm(pv(gi_, 2)[:, h4 * 256 + 128:h4 * 256 + 256], Mc[:, h4, 0, :], Zq[:, h4, 1, :], [Zq, Mc], gk_, start=True, stop=False, inc=False)
                            mm(pv(gi_, 2)[:, h4 * 256 + 128:h4 * 256 + 256], ident, Mc[:, h4, 1, :], [Mc, cst], gk_, start=False, stop=True, inc=(h4 == 3))
                        cp("act", Mn[:], pv(gi_, 2).rearrange("p (h a n) -> p h a n", h=4, a=2), gk_, [Mn])
                    mc = 1 - mc
                for g in range(2):
                    Mc = Mq[g][mc]
                    fi, fk = bank()
                    for h4 in range(4):
                        mm(pv(fi)[:, h4 * 128:(h4 + 1) * 128], Mc[:, h4, 1, :], Xb0[:, 4 * g + h4, :], [Mc, Xb0], [fk], inc=(h4 == 3))
                    cp("act", Xbf[:, 4 * g:4 * g + 4, :], pv(fi).rearrange("p (h n) -> p h n", h=4), [fk], [Xbf])
                if dg:
                    dbg("atb", atb[:].rearrange("p a n -> p (a n)"), [128, 2048], BF16, [atb])
                    dbg("atk", atk[:].rearrange("p a n -> p (a n)"), [128, 2048], BF16, [atk])
                    dbg("abp", abp[:].rearrange("p c a n -> p (c a n)"), [128, 1024], BF16, [abp])
                    dbg("bbar", bbar[:].rearrange("p c n -> p (c n)"), [128, 512], BF16, [bbar])
                    dbg("kbar", kbar[:].rearrange("p c n -> p (c n)"), [128, 512], BF16, [kbar])
                    dbg("tm3", tm3[:].rearrange("p c n -> p (c n)"), [128, 1536], BF16, [tm3])
                    dbg("vtm", vtm[:], [128, 512], BF16, [vtm])
                    dbg("x0", Xb0[:].rearrange("p h n -> p (h n)"), [128, 1024], BF16, [Xb0])
                    dbg("xf", Xbf[:].rearrange("p h n -> p (h n)"), [128, 1024], BF16, [Xbf])
                mark("rpt")
                rbi, rbk = bank(); ybi, ybk = bank(); tbi, tbk = bank()
                for h in range(8):
                    c, hh = h // 2, h % 2
                    Pp = slice(64 * hh, 64 * hh + 64)
                    mm(pv(rbi)[Pp, c * 128:(c + 1) * 128], Xbf[:, h, 0:64], atb[:, h, 128:256], [Xbf, atb], [rbk], inc=(h == 7))
                tt("dve", RpT[:], pv(rbi).rearrange("p (a n) -> p a n", a=4), abp[:, :, 1, :], ALU.add, [rbk, abp], [RpT])
                for h in range(8):
                    mm(pv(ybi)[:, h * 64:(h + 1) * 64], atb[:, h, 128:256], Xbf[:, h, 64:128], [Xbf, atb], [ybk], start=True, stop=False, inc=False)
                    mm(pv(ybi)[:, h * 64:(h + 1) * 64], atk[:, h, 128:256], vtm[:, h * 64:(h + 1) * 64], [atk, vtm], [ybk], start=False, stop=True, inc=(h == 7))
                cp("act", Y0[:], pv(ybi), [ybk], [Y0])
                if dg:
                    dbg("y0", Y0[:], [128, 512], F32, [Y0])
                for h in range(8):
                    c, hh = h // 2, h % 2
                    Pp = slice(64 * hh, 64 * hh + 64)
                    mm(pv(tbi)[Pp, c * 64:(c + 1) * 64], Xbf[:, h, 0:64], Bt_TM[:, h * 64:(h + 1) * 64], [Xbf, tm3], [tbk], inc=False)
                    mm(pv(tbi)[Pp, 256 + c * 64:256 + (c + 1) * 64], Kt_TM[:, h * 64:(h + 1) * 64], vtm[:, h * 64:(h + 1) * 64], [tm3, vtm], [tbk], start=True, stop=False, inc=False)
                    mm(pv(tbi)[Pp, 256 + c * 64:256 + (c + 1) * 64], Bt_TM[:, h * 64:(h + 1) * 64], Xbf[:, h, 64:128], [tm3, Xbf], [tbk], start=False, stop=True, inc=(h == 7))
                cp("act", TpT[:], pv(tbi)[:, 0:256].rearrange("p (a n) -> p a n", a=4), [tbk], [TpT])
                cp("dve", D32[:], pv(tbi)[:, 256:512].rearrange("p (a n) -> p a n", a=4), [tbk], [D32])
                mark("serial")
                Hc = Hbf[n % 2]; Hn = Hbf[(n + 1) % 2]
                ob2 = [bank(), bank()]
                for h in range(8):
                    c, hh = h // 2, h % 2
                    Pp = slice(64 * hh, 64 * hh + 64)
                    mm(pv(ob2[hh][0])[:, c * 64:(c + 1) * 64], RpT[Pp, c, :], Hc[Pp, c, :], [RpT, Hc], [ob2[hh][1]], inc=(h >= 6))
                yyv = yy[:].rearrange("p (c q) n -> p c q n", q=2); y0v = Y0[:].rearrange("p (c q n) -> p c q n", q=2, n=64)
                for hh in range(2):
                    tt("dve", yyv[:, :, hh, :], pv(ob2[hh][0])[:, 0:256].rearrange("p (a n) -> p a n", a=4), y0v[:, :, hh, :], ALU.add, [ob2[hh][1], Y0], [yy])
                hb2 = [bank(), bank()]
                for h in range(8):
                    c, hh = h // 2, h % 2
                    Pp = slice(64 * hh, 64 * hh + 64)
                    mm(pv(hb2[hh][0])[Pp, c * 64:(c + 1) * 64], TpT[Pp, c, :], Hc[Pp, c, :], [TpT, Hc], [hb2[hh][1]], inc=(h >= 6))
                for hh in range(2):
                    Pp = slice(64 * hh, 64 * hh + 64)
                    tt("dve", D32[Pp, :, :], D32[Pp, :, :], pv(hb2[hh][0])[Pp, 0:256].rearrange("p (a n) -> p a n", a=4), ALU.add, [D32, hb2[hh][1]], [D32])
                for c in range(4):
                    stt("dve", H32[:, c, :], H32[:, c, :], gCn[:, c:c + 1], D32[:, c, :], ALU.mult, ALU.add, [H32, gCn, D32], [H32])
                cp("dve", Hn[:], H32[:], [H32], [Hn])
                if dg:
                    dbg("yy", yy[:].rearrange("p a n -> p (a n)"), [128, 512], F32, [yy])
                mark("gnorm")
                red("dve", gn[:, 0, :], yy[:], [yy], [gn])
                tt("pool", ysq[:], yy[:], yy[:], ALU.mult, [yy], [ysq])
                red("dve", gn[:, 1, :], ysq[:], [ysq], [gn])
                ts("dve", gn[:, 2, :], gn[:, 0, :], 1.0 / 64, None, ALU.mult, None, [gn], [gn])
                tt("dve", gn[:, 3, :], gn[:, 2, :], gn[:, 2, :], ALU.mult, [gn], [gn])
                stt("dve", gn[:, 4, :], gn[:, 1, :], 1.0 / 64, gn[:, 3, :], ALU.mult, ALU.subtract, [gn], [gn])
                ts("dve", gn[:, 4, :], gn[:, 4, :], GN_EPS, None, ALU.add, None, [gn], [gn])
                act(gn[:, 4, :], gn[:, 4, :], AF.Sqrt, [gn], [gn])
                P.op("dve", lambda e: e.reciprocal(out=gn[:, 5, :], in_=gn[:, 4, :]), _bl([gn]), _bl([gn]))
                tt("dve", yy[:], yy[:], gn[:, 2, :].unsqueeze(2).to_broadcast([128, 8, 64]), ALU.subtract, [yy, gn], [yy])
                tt("dve", yy[:], yy[:], gn[:, 5, :].unsqueeze(2).to_broadcast([128, 8, 64]), ALU.mult, [yy, gn], [yy])
                yf = yy[:].rearrange("p h n -> p (h n)")
                tt("pool", yf, yf, lnx_b[:, 0:512], ALU.mult, [yy, lnx_b], [yy])
                tt("pool", yf, yf, lnx_b[:, 512:1024], ALU.add, [yy, lnx_b], [yy])
                bbi, bbk = bank()
                for c in range(4):
                    mm(pv(bbi)[:, c * 2:(c + 1) * 2], wrk[:, c, :], bsel[:], [wrk, bsel], [bbk], inc=(c == 3))
                cp("act", bon[:], pv(bbi)[:, 0:8], [bbk], [bon])
                tt("dve", ysq[:], v32[:].rearrange("p (h n) -> p h n", h=8), bon[:].unsqueeze(2).to_broadcast([128, 8, 64]), ALU.mult, [v32, bon], [ysq])
                tt("dve", yy[:], yy[:], ysq[:], ALU.add, [yy, ysq], [yy])
                tt("dve", obf[:], yf, gtm[:], ALU.mult, [yy, gtm], [obf])
                if dg:
                    dbg("orwkv", obf[:], [128, 512], BF16, [obf])
            def B2(n):
                dg = (n == dbg_chunk)
                xc = xt[n % 2]
                tbi2, tbk2 = bank()
                for c in range(4):
                    P.op("pe", lambda e, c=c, tbi2=tbi2: e.transpose(pvb(tbi2)[:, c * 128:(c + 1) * 128], obf[:, c * 128:(c + 1) * 128], ident),
                         _bl([obf, cst]), _bl([tbk2]), inc=(c == 3))
                cp("act", mixR[:], pvb(tbi2)[:, 0:512].rearrange("p (a n) -> p a n", a=4), [tbk2], [mixR])
                if dg:
                    dbg("mixR", mixR[:].rearrange("p a n -> p (a n)"), [128, 512], BF16, [mixR])
                    dbg("mixA", matt[n % 2][:].rearrange("p a n -> p (a n)"), [128, 512], BF16, [matt[n % 2]])
                mark("outproj")
                x1c = xc; h2c = h2[n % 2]
                oi, ok = bank2()
                for nh in range(2):
                    for kc in range(8):
                        mt_ = mixR[:, kc, :] if kc < 4 else matt[n % 2][:, kc - 4, :]
                        mm(pv(oi, 2)[:, nh * 512:(nh + 1) * 512], mt_, w_out[:, kc, nh * 512:(nh + 1) * 512], [mixR, matt[n % 2], w_out], ok,
                           start=(kc == 0), stop=(kc == 7), inc=(kc == 7 and nh == 1))
                tt("dve", xc[:], pv(oi, 2), xc[:], ALU.add, ok + [xc], [xc])
                dma("sp", out_d[n * 128:(n + 1) * 128, :], x1c[:], [x1c], [], "x1o")
                if dg:
                    dbg("x1", x1c[:], [128, 1024], F32, [x1c])
                mark("norm2")
                act(h2c[:], x1c[:], AF.Square, [x1c], [h2c, smB], accum=smB[:, 4:5])
                ts("dve", smB[:, 5:6], smB[:, 4:5], 1.0 / D, NORM_EPS, ALU.mult, ALU.add, [smB], [smB])
                act(smB[:, 5:6], smB[:, 5:6], AF.Sqrt, [smB], [smB])
                P.op("dve", lambda e: e.reciprocal(out=smB[:, 6:7], in_=smB[:, 5:6]), _bl([smB]), _bl([smB]))
                h2f = atb[:].rearrange("p a n -> p (a n)").bitcast(F32)[:, 0:D]
                stt("dve", h2f, x1c[:], smB[:, 6:7], s2_b[:], ALU.mult, ALU.mult, [x1c, smB, s2_b], [atb])
                tt("pool", h2c[:], h2f, sh2_b[:], ALU.add, [atb, sh2_b], [h2c])
                dma("sp", hbuf_d[n * 128:(n + 1) * 128, :], h2c[:], [h2c], [], "h2o")
                mark("router")
                ti, tk_ = bank()
                for kc in range(8):
                    P.op("pe", lambda e, kc=kc, ti=ti: e.transpose(pvb(ti)[:, kc * 128:(kc + 1) * 128], h2c[:, kc * 128:(kc + 1) * 128], ident),
                         _bl([h2c, cst]), _bl([tk_]), inc=(kc == 7))
                h2T = atk[:].rearrange("p a n -> p (a n)")[:, 0:1024].rearrange("p (a n) -> p a n", a=8)
                cp("act", h2T, pvb(ti).rearrange("p (a n) -> p a n", a=8), [tk_], [atk])
                li, lk = bank()
                for kc in range(8):
                    mm(pv(li)[:, 0:32], h2T[:, kc, :], wr[:, kc, :], [atk, wr], [lk], start=(kc == 0), stop=(kc == 7), inc=(kc == 7))
                tt("dve", lg_all[:, n, :], pv(li)[:, 0:32], brt[:], ALU.add, [lk, brt], [lg_all])
                P.op("dve", lambda e, n=n: e.max(out=top_all[:, n, :], in_=lg_all[:, n, :]), _bl([lg_all]), _bl([top_all]))
                ts("dve", smB[:, 8:9], top_all[:, n, 0:1], -1.0, None, ALU.mult, None, [top_all], [smB])
                act(ex4[:], top_all[:, n, 0:4], AF.Exp, [top_all, smB], [ex4, smB], bias=smB[:, 8:9], accum=smB[:, 9:10])
                P.op("dve", lambda e: e.reciprocal(out=smB[:, 10:11], in_=smB[:, 9:10]), _bl([smB]), _bl([smB]))
                ts("dve", gate_all[:, n, :], ex4[:], smB[:, 10:11], None, ALU.mult, None, [ex4, smB], [gate_all])
                ts("dve", msk[:], lg_all[:, n, :], top_all[:, n, 3:4], None, ALU.is_ge, None, [lg_all, top_all], [msk])
                ci, ck = bank()
                mm(pv(ci)[:, 0:32], tri_lt, msk[:], [msk, cst], [ck], inc=False)
                mm(pv(ci)[:, 32:64], ones_bf, msk[:], [msk, cst], [ck])
                tt("dve", pos_all[:, n, :], pv(ci)[:, 0:32], carry[:], ALU.add, [ck, carry], [pos_all])
                tt("dve", carry[:], carry[:], pv(ci)[:, 32:64], ALU.add, [ck, carry], [carry])
            kcur = [None]
            Fe(0); Fl(0)
            for n in range(nchunks):
                if n + 1 < nchunks and not os.environ.get("KNOIL"):
                    interleave(P, lambda n=n: B1(n), lambda n=n: Fe(n + 1))
                    interleave(P, lambda n=n: B2(n), lambda n=n: Fl(n + 1))
                else:
                    B1(n); B2(n)
                    if n + 1 < nchunks:
                        Fe(n + 1); Fl(n + 1)
            if dbg_chunk is not None:
                dbg("lg", lg_all[:, 0:nchunks, :].rearrange("p a n -> p (a n)"), [128, nchunks * 32], F32, [lg_all])
            P.barrier()

        if dbg_chunk is None and not os.environ.get("KSKIPMOE"):
            with ExitStack() as st3:
                bst = sbuf(st3, "bst", [128, NB * 32])
                dma("sp", bst[:], bst_d, [], [bst], "bst")
                bstart = bst[:].rearrange("p (b e) -> p b e", b=NB)
                cmp8 = sbuf(st3, "cmp8", [128, 32, MB]); nblk = sbuf(st3, "nblk", [128, 32])
                pend = sbuf(st3, "pend", [128, 32]); pstart = sbuf(st3, "pstart", [128, 32])
                cmpb = sbuf(st3, "cmpb", [128, NB, 32]); eb = sbuf(st3, "eb", [128, NB])
                woff_f = sbuf(st3, "woff_f", [128, NB]); same = sbuf(st3, "same", [128, NB])
                vala = sbuf(st3, "vala", [128, NCH, 32]); oha = sbuf(st3, "oha", [128, NCH, 32])
                dest_f = sbuf(st3, "dest_f", [128, NCH, 4])
                hs = [sbuf(st3, "hs%d" % i, [128, D], BF16) for i in range(4)]
                tt("dve", cmp8[:], carry[:].unsqueeze(2).to_broadcast([128, 32, MB]), thr8.unsqueeze(1).to_broadcast([128, 32, MB]), ALU.is_gt, [carry, cst2], [cmp8])
                red("dve", nblk[:], cmp8[:], [cmp8], [nblk])
                ts("dve", nblk[:], nblk[:], float(BLK), None, ALU.mult, None, [nblk], [nblk])
                P.op("dve", lambda e: e.tensor_tensor_scan(out=pend[:], data0=ones_f[:, 0:32], data1=nblk[:], initial=0.0, op0=ALU.mult, op1=ALU.add),
                     _bl([nblk, cstf]), _bl([pend]))
                tt("dve", pstart[:], pend[:], nblk[:], ALU.subtract, [pend, nblk], [pstart])
                tt("dve", cmpb[:], bstart, pend[:].unsqueeze(1).to_broadcast([128, NB, 32]), ALU.is_ge, [pend, bst], [cmpb])
                red("dve", eb[:], cmpb[:], [cmpb], [eb])
                ts("dve", eb[:], eb[:], 31.0, None, ALU.min, None, [eb], [eb])
                ts("dve", woff_f[:], eb[:], 128.0, iota_p, ALU.mult, ALU.add, [eb, cst2], [woff_f])
                P.op("dve", lambda e: e.memset(same[:], 0.0), [], _bl([same]))
                tt("dve", same[:, 2:NB], eb[:, 2:NB], eb[:, 0:NB - 2], ALU.is_equal, [eb, same], [same])
                stt("dve", woff_f[:], same[:], 8192.0, woff_f[:], ALU.mult, ALU.add, [same, woff_f], [woff_f])
                cp("dve", woff[:], woff_f[:], [woff_f], [woff])
                tt("dve", vala[:, 0:nch, :], pos_all[:, 0:nch, :], pstart[:].unsqueeze(1).to_broadcast([128, nch, 32]), ALU.add, [pos_all, pstart], [vala])
                for k in range(4):
                    tt("dve", oha[:, 0:nch, :], lg_all[:, 0:nch, :], top_all[:, 0:nch, k:k + 1].to_broadcast([128, nch, 32]), ALU.is_equal, [lg_all, top_all], [oha])
                    tt("dve", oha[:, 0:nch, :], oha[:, 0:nch, :], vala[:, 0:nch, :], ALU.mult, [oha, vala], [oha])
                    red("dve", dest_f[:, 0:nch, k], oha[:, 0:nch, :], [oha], [dest_f])
                cp("dve", dest[:, 0:nch, :], dest_f[:, 0:nch, :], [dest_f], [dest])
                P._wait("pool", ("zf", P.dsem["zf"][1]))
                for n in range(nch):
                    hc = hs[n % 4]; dc = dcur[n % 2]
                    dma("sp", hc[:], hbuf_d[n * 128:(n + 1) * 128, :], [], [hc], "hs%d" % (n % 4))
                    cp("dve", dc[:], dest[:, n, :], [dest], [dc])
                    for k in range(4):
                        P.dma("pool", lambda e, k=k, hc=hc, dc=dc: e.indirect_dma_start(
                            out=xs_d, out_offset=bass.IndirectOffsetOnAxis(ap=dc[:, k:k + 1], axis=0),
                            in_=hc[:, :], in_offset=None, bounds_check=bc(e, NSLOT - 1), oob_is_err=False),
                            _bl([hc, dc]), [], key="sc%d" % (n % 4))
                P.barrier()

            with ExitStack() as st4:
                w1s = [sbuf(st4, "w1s%d" % i, [128, 8, 2048], BF16) for i in range(2)]
                w2s = [sbuf(st4, "w2s%d" % i, [128, 8, 1024], BF16) for i in range(2)]
                b1s = [sbuf(st4, "b1s%d" % i, [128, 16 + 1024]) for i in range(2)]
                b1l1 = [sbuf(st4, "b1l1_%d" % i, [128, 8]) for i in range(2)]
                xg = [sbuf(st4, "xg%d" % i, [128, JB, D], BF16) for i in range(2)]
                xT = sbuf(st4, "xT", [128, 8, BLK], BF16)
                aT = sbuf(st4, "aT", [128, 8, BLK], BF16)
                t1 = [sbuf(st4, "t1_%d" % i, [128, BLK]) for i in range(2)]
                t2 = [sbuf(st4, "t2_%d" % i, [128, BLK]) for i in range(2)]
                sgm = [sbuf(st4, "sgm%d" % i, [128, BLK]) for i in range(2)]
                yo = [sbuf(st4, "yo%d" % i, [128, D]) for i in range(2)]
                nblocks = min(NB, nch * 128 * 4 // BLK + 32)

                def load_w(b):
                    i = b % 2
                    cp("dve", wofc[i][:], woff[:, b:b + 1], [woff], [wofc[i]])
                    P.dma("pool", lambda e: e.indirect_dma_start(out=w1s[i][:].rearrange("p a n -> p (a n)"), out_offset=None, in_=w1_d,
                          in_offset=bass.IndirectOffsetOnAxis(ap=wofc[i][:, 0:1], axis=0), bounds_check=bc(e, 32 * 128 - 1), oob_is_err=False),
                          _bl([wofc[i]]), _bl([w1s[i]]), key="w1_%d" % i)
                    P.dma("pool", lambda e: e.indirect_dma_start(out=w2s[i][:].rearrange("p a n -> p (a n)"), out_offset=None, in_=w2_d,
                          in_offset=bass.IndirectOffsetOnAxis(ap=wofc[i][:, 0:1], axis=0), bounds_check=bc(e, 32 * 128 - 1), oob_is_err=False),
                          _bl([wofc[i]]), _bl([w2s[i]]), key="w2_%d" % i)
                    P.dma("pool", lambda e: e.indirect_dma_start(out=b1s[i][:, :], out_offset=None, in_=b1_d,
                          in_offset=bass.IndirectOffsetOnAxis(ap=wofc[i][:, 0:1], axis=0), bounds_check=bc(e, 32 * 128 - 1), oob_is_err=False),
                          _bl([wofc[i]]), _bl([b1s[i]]), key="b1_%d" % i)

                def load_x(b):
                    i = b % 2
                    dma("sp", xg[i][:], xs_d[b * BLK:(b + 1) * BLK, :].rearrange("(j p) d -> p j d", p=128), [], [xg[i]], "xg%d" % i)

                load_w(0); load_x(0)
                for b in range(nblocks):
                    i = b % 2
                    if b + 1 < nblocks:
                        load_w(b + 1); load_x(b + 1)
                    for kc in range(8):
                        ti, tk_ = bank()
                        for j in range(JB):
                            P.op("pe", lambda e, kc=kc, j=j, ti=ti, i=i: e.transpose(pvb(ti)[:, j * 128:(j + 1) * 128], xg[i][:, j, kc * 128:(kc + 1) * 128], ident),
                                 _bl([xg[i], cst]), _bl([tk_]), inc=(j == JB - 1))
                        cp("act", xT[:, kc, :], pvb(ti)[:, 0:BLK], [tk_], [xT])
                    ts("dve", b1l1[i][:], b1s[i][:, 8:16], 1.0, None, ALU.add, None, [b1s[i]], [b1l1[i]])
                    for fc in range(8):
                        gi, gk = bank(); li, lk = bank()
                        for kc in range(8):
                            mm(pv(gi)[:, 0:BLK], w1s[i][:, kc, fc * 128:(fc + 1) * 128], xT[:, kc, :], [w1s[i], xT], [gk], start=(kc == 0), stop=(kc == 7), inc=(kc == 7))
                        for kc in range(8):
                            mm(pv(li)[:, 0:BLK], w1s[i][:, kc, 1024 + fc * 128:1024 + (fc + 1) * 128], xT[:, kc, :], [w1s[i], xT], [lk], start=(kc == 0), stop=(kc == 7), inc=(kc == 7))
                        a1 = t1[fc % 2]; a2 = t2[fc % 2]; sg_ = sgm[fc % 2]
                        ts("dve", a1[:], pv(gi)[:, 0:BLK], b1s[i][:, fc:fc + 1], 7.0, ALU.add, ALU.min, [gk, b1s[i]], [a1])
                        act(sg_[:], a1[:], AF.Sigmoid, [a1], [sg_], scale=1.702)
                        ts("dve", a2[:], pv(li)[:, 0:BLK], b1l1[i][:, fc:fc + 1], 8.0, ALU.add, ALU.min, [lk, b1l1[i]], [a2])
                        stt("dve", a2[:], a2[:], -6.0, a1[:], ALU.max, ALU.mult, [a2, a1], [a2])
                        tt("dve", aT[:, fc, :], a2[:], sg_[:], ALU.mult, [a2, sg_], [aT])
                    for j in range(JB):
                        oi, ok = bank2()
                        for nh in range(2):
                            for kc in range(8):
                                mm(pv(oi, 2)[:, nh * 512:(nh + 1) * 512], aT[:, kc, j * 128:(j + 1) * 128], w2s[i][:, kc, nh * 512:(nh + 1) * 512],
                                   [aT, w2s[i]], ok, start=(kc == 0), stop=(kc == 7), inc=(kc == 7 and nh == 1))
                        yb = yo[j % 2]
                        tt("dve", yb[:], pv(oi, 2), b1s[i][:, 16:1040], ALU.add, ok + [b1s[i]], [yb])
                        dma("sp", ys_d[b * BLK + j * 128:b * BLK + (j + 1) * 128, :], yb[:], [yb], [], "yo%d" % (j % 2))
                P.barrier()

            with ExitStack() as st5:
                yg = [[sbuf(st5, "yg%d_%d" % (i, k), [128, D]) for k in range(4)] for i in range(3)]
                xr = [sbuf(st5, "xr%d" % i, [128, D]) for i in range(3)]
                acc = [sbuf(st5, "acc%d" % i, [128, D]) for i in range(3)]
                nfin = nch
                for n in range(nfin):
                    i = n % 3
                    dma("sp", xr[i][:], out_d[n * 128:(n + 1) * 128, :], [], [xr[i]], "xr%d" % i)
                    dc = dcur[n % 2]
                    cp("dve", dc[:], dest[:, n, :], [dest], [dc])
                    for k in range(4):
                        P.dma("pool", lambda e, k=k, i=i, dc=dc: e.indirect_dma_start(out=yg[i][k][:, :], out_offset=None, in_=ys_d,
                              in_offset=bass.IndirectOffsetOnAxis(ap=dc[:, k:k + 1], axis=0), bounds_check=bc(e, NSLOT - 1), oob_is_err=False),
                              _bl([dc]), _bl([yg[i][k]]), key="yg%d_%d" % (i, k))
                    a_ = acc[i]
                    ts("dve", a_[:], yg[i][0][:], gate_all[:, n, 0:1], None, ALU.mult, None, [yg[i][0], gate_all], [a_])
                    for k in range(1, 4):
                        stt("dve", a_[:], yg[i][k][:], gate_all[:, n, k:k + 1], a_[:], ALU.mult, ALU.add, [yg[i][k], gate_all, a_], [a_])
                    tt("dve", a_[:], a_[:], gt2_b[:], ALU.mult, [a_, gt2_b], [a_])
                    tt("dve", a_[:], a_[:], xr[i][:], ALU.add, [a_, xr[i]], [a_])
                    dma("sp", out_d[n * 128:(n + 1) * 128, :], a_[:], [a_], [], "fo%d" % i)
        for k, v in P.dsem.items():
            P._wait("sp", (k, v[1]))
        P.emit()
    return nc, dbg_out, P


def _consts():
    p = np.arange(128)[:, None]; j = np.arange(128)[None, :]
    c = np.zeros((128, 14, 128), np.float32)
    c[:, 0] = (p == j); c[:, 1] = (p < j); c[:, 2] = (p <= j); c[:, 3] = (p > j)
    c[:, 4] = ((p // 64) == (j // 64)); c[:, 5] = 1.0
    c[:, 6] = (p > j) & ((p // 16) == (j // 16)); c[:, 7] = (p < j) & ((p // 16) == (j // 16))
    for ii, bsz in enumerate((32, 64, 128)):
        hb = bsz // 2
        c[:, 8 + 2 * ii] = ((p // bsz) == (j // bsz)) & ((p // hb) > (j // hb))
        c[:, 9 + 2 * ii] = ((p // bsz) == (j // bsz)) & ((p // hb) < (j // hb))
    c2 = np.zeros((128, MB + 3), np.float32)
    c2[:, 0:MB] = np.arange(MB)[None, :] * BLK
    c2[:, MB] = (np.arange(128) < 64); c2[:, MB + 1] = (np.arange(128) >= 64)
    c2[:, MB + 2] = np.arange(128)
    bst = np.ascontiguousarray(np.broadcast_to(np.repeat(np.arange(NB) * BLK, 32)[None, :], (128, NB * 32))).astype(np.float32)
    return c, c2, bst


def _prep_shared(inp):
    f = lambda a: np.ascontiguousarray(np.asarray(a, dtype=np.float32))
    L = 0
    fm = lambda v, k: f(np.asarray(v).reshape(k, 128).T)
    qperm = np.concatenate([np.concatenate([np.arange(64) + 64 * jj, np.arange(64) + 64 * (4 + jj)]) for jj in range(4)])
    w_in = np.asarray(inp["w_in"][L])
    cols = np.concatenate([np.arange(1792), 1792 + qperm, np.arange(2304, 2560)])
    w_out = np.asarray(inp["w_out"][L])
    rows = np.concatenate([np.arange(512), 512 + qperm])
    c, c2, bst = _consts()
    sh = {
        "w_ada": f(inp["w_ada"][L]), "b_ada_fm": fm(inp["b_ada"][L], 48), "b_ada_row": f(inp["b_ada"][L]).reshape(1, -1),
        "g1_fm": fm(inp["norm1_g"][L], 8), "g2_row": f(inp["norm2_g"][L]).reshape(1, -1),
        "w_in": f(w_in[:, cols]), "mu_fm": fm(inp["mu_shift"][L], 14),
        "chp": f(np.stack([np.asarray(inp[k][L]).reshape(-1).reshape(4, 128).T for k in ("w0", "a0", "k_k", "k_a", "r_k")], axis=1)),
        "lnx_row": f(np.concatenate([inp["lnx_g"][L], inp["lnx_b"][L]])).reshape(1, -1),
        "lora_up": f(np.concatenate([inp["w_up"][L], inp["a_up"][L]], axis=0)), "g_up": f(inp["g_up"][L]),
        "qkg": f(np.stack([np.tile(inp["q_norm_g"][L], 2), np.tile(inp["k_norm_g"][L], 2)], axis=1)),
        "sink_fm": f(np.concatenate([np.tile(np.asarray(inp["sinks"][L])[0:4][None, :], (64, 1)),
                                     np.tile(np.asarray(inp["sinks"][L])[4:8][None, :], (64, 1))], axis=0)),
        "w_out": f(w_out[rows, :]), "w_router": f(inp["w_router"][L]), "b_router": f(inp["b_router"][L]).reshape(1, -1),
        "w1p": f(np.asarray(inp["w1"][L]).reshape(32, 8, 128, 2048).transpose(0, 2, 1, 3).reshape(32 * 128, 8 * 2048)),
        "w2p": f(np.asarray(inp["w2"][L]).reshape(32, 8, 128, 1024).transpose(0, 2, 1, 3).reshape(32 * 128, 8 * 1024)),
        "b12p": f(np.concatenate([np.asarray(inp["b1"][L]).reshape(32, 16, 128).transpose(0, 2, 1).reshape(32 * 128, 16),
                                  np.repeat(np.asarray(inp["b2"][L]), 128, axis=0)], axis=1)),
        "consts": c, "consts2": c2, "bstart": bst, "zeros": np.zeros((1024, D), dtype=ml_dtypes.bfloat16),
    }
    return sh


def kernel(**inputs):
    sh = _prep_shared(inputs)
    x = np.asarray(inputs["x"], dtype=np.float32)
    cc = np.asarray(inputs["c"], dtype=np.float32)
    nc, _, _ = build_program()
    in_maps = []
    for b in range(8):
        m = dict(sh)
        m["x"] = np.ascontiguousarray(x[b])
        m["c_fm"] = np.ascontiguousarray(cc[b].reshape(8, 128).T)
        in_maps.append(m)
    res = run_bass_kernel_spmd(nc, in_maps, core_ids=list(range(8)))
    return np.stack([np.asarray(res.results[b]["out"], dtype=np.float32) for b in range(8)], axis=0)
```

```python
import os
import threading
from contextlib import ExitStack
import numpy as np
import ml_dtypes
import concourse.bass as bass
import concourse.mybir as mybir
from concourse.bass_utils import run_bass_kernel_spmd

F32 = mybir.dt.float32
BF16 = mybir.dt.bfloat16
I32 = mybir.dt.int32
ALU = mybir.AluOpType
AF = mybir.ActivationFunctionType
AX = mybir.AxisListType

T = 4096
D = 1024
NCH = T // 128
BLK = int(os.environ.get("KBLK", "384"))
JB = BLK // 128
MB = -(-T // BLK)
NB = T * 4 // BLK + 32
NSLOT = NB * BLK
DECAY_K = 0.6065306597126334
NORM_EPS = 1e-6
GN_EPS = 64e-5


KLIMIT = int(os.environ.get("KLIMIT", "100000000"))
NOPOOL = not os.environ.get("KPOOL")


class Buf:
    __slots__ = ("name", "w", "r", "excl")

    def __init__(self, name):
        self.name = name
        self.w = None
        self.r = []
        self.excl = False


class Prog:
    ENGS = ("pe", "act", "dve", "pool", "sp")

    def __init__(self, nc, stack):
        self.nc = nc
        self.stack = stack
        self.sem = {e: stack.enter_context(nc.semaphore("sem_" + e)) for e in self.ENGS}
        self.cnt = {e: 0 for e in self.ENGS}
        self.streams = {e: [] for e in self.ENGS}
        self.waited = {}
        self.dsem = {}
        self.ninstr = 0
        self.hook = None
        self.hold = False

    def _wait(self, eng, ev):
        if ev is None:
            return
        key, val = ev
        if key == "pe" and eng == "pe":
            return
        k = (eng, key)
        if self.waited.get(k, 0) >= val:
            return
        self.waited[k] = val
        sem = self.sem[key] if key in self.sem else self.dsem[key][0]
        self.streams[eng].append(lambda e, sem=sem, val=val: e.wait_ge(sem, val))
        self.ninstr += 1

    def _deps(self, eng, reads, writes):
        mx = {}
        for b in reads:
            if b.w is not None:
                mx[b.w[0]] = max(mx.get(b.w[0], 0), b.w[1])
            if b.excl:
                for k, v in b.r:
                    if k != eng:
                        mx[k] = max(mx.get(k, 0), v)
        for b in writes:
            if b.w is not None:
                mx[b.w[0]] = max(mx.get(b.w[0], 0), b.w[1])
            for k, v in b.r:
                mx[k] = max(mx.get(k, 0), v)
        for k, v in mx.items():
            self._wait(eng, (k, v))

    def _commit(self, ev, reads, writes):
        for b in writes:
            b.w = ev
            b.r = []
        for b in reads:
            if b not in writes:
                b.r.append(ev)
                if len(b.r) > 48:
                    mx = {}
                    for k, v in b.r:
                        mx[k] = max(mx.get(k, 0), v)
                    b.r = list(mx.items())

    def op(self, eng, fn, reads=(), writes=(), inc=True):
        if self.ninstr > KLIMIT:
            return None
        if self.hook is not None and not self.hold:
            self.hook()
        self.hold = not inc
        if eng == "pool" and NOPOOL:
            eng = "dve"
        self._deps(eng, reads, writes)
        if os.environ.get("KLIST"):
            import sys
            f = sys._getframe(1)
            while f.f_code.co_name in ("tt", "ts", "stt", "cp", "act", "mm", "red", "dma", "dbg", "op", "<lambda>"):
                f = f.f_back
            print("INSTR", self.ninstr, eng, "line", f.f_lineno)
        if inc:
            self.cnt[eng] += 1
            ev = (eng, self.cnt[eng])
            sem = self.sem[eng]
            self.streams[eng].append(lambda e, fn=fn, sem=sem: fn(e).then_inc(sem, 1))
        else:
            ev = (eng, self.cnt[eng] + 1)
            self.streams[eng].append(lambda e, fn=fn: fn(e))
        self.ninstr += 1
        self._commit(ev, reads, writes)
        return ev

    def dma(self, eng, fn, reads=(), writes=(), key="d"):
        if self.ninstr > KLIMIT:
            return None
        if self.hook is not None and not self.hold:
            self.hook()
        if key not in self.dsem:
            self.dsem[key] = [self.stack.enter_context(self.nc.semaphore("dsem_" + key)), 0]
        self._deps(eng, reads, writes)
        self.dsem[key][1] += 16
        ev = (key, self.dsem[key][1])
        sem = self.dsem[key][0]
        self.streams[eng].append(lambda e, fn=fn, sem=sem: fn(e).then_inc(sem, 16))
        self.ninstr += 1
        self._commit(ev, reads, writes)
        return ev

    def barrier(self, exclude=()):
        evs = [(e, self.cnt[e]) for e in self.ENGS if self.cnt[e] > 0]
        evs += [(k, v[1]) for k, v in self.dsem.items() if v[1] > 0 and k not in exclude]
        for e in self.ENGS:
            for ev in evs:
                self._wait(e, ev)

    def emit(self):
        with self.nc.Block() as blk:
            @blk.tensor
            def _(e):
                for f in self.streams["pe"]:
                    f(e)

            @blk.scalar
            def _(e):
                for f in self.streams["act"]:
                    f(e)

            @blk.vector
            def _(e):
                for f in self.streams["dve"]:
                    f(e)

            @blk.gpsimd
            def _(e):
                for f in self.streams["pool"]:
                    f(e)

            @blk.sync
            def _(e):
                for f in self.streams["sp"]:
                    f(e)


def interleave(P, fa, fb):
    cv = threading.Condition()
    st = {"turn": 0, "alive": [True, True], "exc": None}

    def hook():
        me = int(threading.current_thread().name[-1])
        with cv:
            if st["alive"][1 - me]:
                st["turn"] = 1 - me
                cv.notify_all()
                while st["turn"] != me:
                    cv.wait()

    def run(i, f):
        with cv:
            while st["turn"] != i:
                cv.wait()
        try:
            f()
        except BaseException as ex:
            st["exc"] = ex
        finally:
            with cv:
                st["alive"][i] = False
                st["turn"] = 1 - i
                cv.notify_all()

    P.hook = hook
    P.hold = False
    ts_ = [threading.Thread(target=run, args=(i, f), name="il%d" % i) for i, f in enumerate((fa, fb))]
    for t in ts_:
        t.start()
    for t in ts_:
        t.join()
    P.hook = None
    if st["exc"] is not None:
        raise st["exc"]


class Tl:
    def __init__(self, t, name):
        self.t = t
        self.b = Buf(name)

    def __getitem__(self, k):
        return self.t[k]


def _bl(xs):
    return [x.b if isinstance(x, Tl) else x for x in xs]


def build_program(dbg_chunk=None, nch=NCH):
    nc = bass.Bass("TRN2", target_bir_lowering=False)
    dram_in = {}

    def DI(name, shape, dt=F32):
        dram_in[name] = nc.dram_tensor(name, list(shape), dt, kind="ExternalInput").ap()
        return dram_in[name]

    x_d = DI("x", [T, D]); cfm_d = DI("c_fm", [128, 8])
    wada_d = DI("w_ada", [D, 6 * D]); bada_fm_d = DI("b_ada_fm", [128, 48]); bada_row_d = DI("b_ada_row", [1, 6 * D])
    g1_d = DI("g1_fm", [128, 8]); g2_d = DI("g2_row", [1, D])
    win_d = DI("w_in", [D, 2560]); mu_d = DI("mu_fm", [128, 14])
    ch_d = DI("chp", [128, 5, 4])
    lnx_d = DI("lnx_row", [1, 1024])
    lora_d = DI("lora_up", [128, 512]); gup_d = DI("g_up", [128, 512])
    qk_d = DI("qkg", [128, 2]); sink_d = DI("sink_fm", [128, 4])
    wout_d = DI("w_out", [D, D]); wr_d = DI("w_router", [D, 32]); br_d = DI("b_router", [1, 32])
    nexp = 32 if dbg_chunk is None else 1
    w1_d = DI("w1p", [nexp * 128, 8 * 2048]); w2_d = DI("w2p", [nexp * 128, 8 * 1024])
    b1_d = DI("b12p", [32 * 128, 16 + 1024])
    cst_d = DI("consts", [128, 14, 128]); cst2_d = DI("consts2", [128, MB + 3]); bst_d = DI("bstart", [128, NB * 32]); zer_d = DI("zeros", [1024, D], BF16)
    out_d = nc.dram_tensor("out", [T, D], F32, kind="ExternalOutput").ap()
    hbuf_d = nc.dram_tensor("hbuf", [T, D], BF16).ap()
    nsl = NSLOT if dbg_chunk is None else 128
    xs_d = nc.dram_tensor("xs", [nsl, D], BF16).ap()
    ys_d = nc.dram_tensor("ys", [nsl, D], F32).ap()
    dbg_out = {}

    with ExitStack() as st0:
        P = Prog(nc, st0)

        def sbuf(st, name, shape, dt=F32):
            return Tl(st.enter_context(nc.sbuf_tensor("s_" + name, list(shape), dt)), name)

        def E(eng):
            return eng

        def tt(eng, out, in0, in1, op, R, W):
            return P.op(eng, lambda e: e.tensor_tensor(out=out, in0=in0, in1=in1, op=op), _bl(R), _bl(W))

        def ts(eng, out, in0, s1, s2, op0, op1, R, W):
            if s2 is None:
                return P.op(eng, lambda e: e.tensor_scalar(out=out, in0=in0, scalar1=s1, scalar2=None, op0=op0), _bl(R), _bl(W))
            return P.op(eng, lambda e: e.tensor_scalar(out=out, in0=in0, scalar1=s1, scalar2=s2, op0=op0, op1=op1), _bl(R), _bl(W))

        def stt(eng, out, in0, sc, in1, op0, op1, R, W):
            return P.op(eng, lambda e: e.scalar_tensor_tensor(out=out, in0=in0, scalar=sc, in1=in1, op0=op0, op1=op1), _bl(R), _bl(W))

        def cp(eng, out, in_, R, W):
            if eng == "act":
                return P.op(eng, lambda e: e.copy(out=out, in_=in_), _bl(R), _bl(W))
            return P.op(eng, lambda e: e.tensor_copy(out=out, in_=in_), _bl(R), _bl(W))

        def act(out, in_, func, R, W, bias=None, scale=None, accum=None):
            kw = {}
            if bias is not None:
                kw["bias"] = bias
            if scale is not None:
                kw["scale"] = scale
            if accum is not None:
                kw["accum_out"] = accum
            return P.op("act", lambda e: e.activation(out=out, in_=in_, func=func, **kw), _bl(R), _bl(W))

        def mm(out, lhsT, rhs, R, W, start=True, stop=True, inc=True):
            return P.op("pe", lambda e: e.matmul(out, lhsT=lhsT, rhs=rhs, start=start, stop=stop), _bl(R), _bl(W), inc=inc)

        def red(eng, out, in_, R, W, op=ALU.add):
            return P.op(eng, lambda e: e.tensor_reduce(out=out, in_=in_, axis=AX.X, op=op), _bl(R), _bl(W))

        def dma(eng, out, in_, R, W, key):
            return P.dma(eng, lambda e: e.dma_start(out=out, in_=in_), _bl(R), _bl(W), key=key)

        regcache = {}

        def bc(e, v):
            if v not in regcache:
                regcache[v] = e.to_reg(v)
            return regcache[v]

        def mark(label):
            if os.environ.get("KMARK"):
                print("MARK", label, P.ninstr)

        def dbg(name, tile_ap, shape, dt, R):
            if dbg_chunk is None:
                return
            o = nc.dram_tensor("dbg_" + name, list(shape), dt, kind="ExternalOutput").ap()
            dbg_out[name] = o
            dma("sp", o, tile_ap, R, [], "dbg_" + name)

        psum_t = st0.enter_context(nc.psum_tensor("psum", [128, 8, 512], F32))
        banks = [Tl(psum_t, "bank%d" % i) for i in range(8)]
        for bk_ in banks:
            bk_.b.excl = True
        rr = [0, 0, 0]

        def _pool():
            if P.hook is None:
                return 0, 0, 8
            t = int(threading.current_thread().name[-1])
            return 1 + t, 4 * t, 4

        def bank():
            k, base, n = _pool()
            i = base + rr[k] % n
            rr[k] += 1
            return i, banks[i]

        def bank2():
            k, base, n = _pool()
            while rr[k] % 2:
                rr[k] += 1
            i = base + rr[k] % n
            rr[k] += 2
            return i, [banks[i], banks[i + 1]]

        def pv(i, n=1):
            return psum_t[:, i:i + n, :].rearrange("p a n -> p (a n)")

        def pvb(i):
            return psum_t[:, i, :].bitcast(BF16)

        cst = sbuf(st0, "cst", [128, 14, 128], BF16)
        cstf = sbuf(st0, "cstf", [128, 2, 128], F32)
        cst2 = sbuf(st0, "cst2", [128, MB + 3], F32)
        dma("pool", cst[:], cst_d, [], [cst], "c0")
        dma("sp", cstf[:, 0, :], cst_d[:, 0, :], [], [cstf], "c1")
        dma("sp", cstf[:, 1, :], cst_d[:, 5, :], [], [cstf], "c1")
        dma("sp", cst2[:], cst2_d, [], [cst2], "c2")
        ident = cst[:, 0, :]; tri_lt = cst[:, 1, :]; tri_le = cst[:, 2, :]; tri_gt = cst[:, 3, :]
        bdiag = cst[:, 4, :]; ones_bf = cst[:, 5, :]
        ident_f = cstf[:, 0, :]; ones_f = cstf[:, 1, :]
        thr8 = cst2[:, 0:MB]
        bsel_f = cst2[:, MB:MB + 2]
        iota_p = cst2[:, MB + 2:MB + 3]
        bsel = sbuf(st0, "bsel", [128, 2], BF16)
        cp("dve", bsel[:], bsel_f, [cst2], [bsel])

        lg_all = sbuf(st0, "lg_all", [128, NCH, 32])
        top_all = sbuf(st0, "top_all", [128, NCH, 8])
        gate_all = sbuf(st0, "gate_all", [128, NCH, 4])
        pos_all = sbuf(st0, "pos_all", [128, NCH, 32])
        carry = sbuf(st0, "carry", [128, 32])
        gt2_b = sbuf(st0, "gt2_b", [128, D])
        woff = sbuf(st0, "woff", [128, NB], I32)
        dest = sbuf(st0, "dest", [128, NCH, 4], I32)
        wofc = [sbuf(st0, "wofc%d" % i, [128, 1], I32) for i in range(2)]
        dcur = [sbuf(st0, "dcur%d" % i, [128, 4], I32) for i in range(2)]
        P.op("dve", lambda e: e.memset(carry[:], 0.0), [], _bl([carry]))

        with ExitStack() as st1:
            modfm = sbuf(st1, "modfm", [128, 32])
            s1 = sbuf(st1, "s1", [128, 8])
            s2_b = sbuf(st1, "s2_b", [128, D]); sh2_b = sbuf(st1, "sh2_b", [128, D])
            lnx_b = sbuf(st1, "lnx_b", [128, 1024])
            chp = sbuf(st1, "chp", [128, 5, 4]); mu = sbuf(st1, "mu", [128, 14]); g1 = sbuf(st1, "g1", [128, 8])
            qkg = sbuf(st1, "qkg", [128, 2]); esink = sbuf(st1, "esink", [128, 4])
            brt = sbuf(st1, "brt", [128, 32])
            omka = sbuf(st1, "omka", [128, 4])
            w_in = sbuf(st1, "w_in", [128, 8, 2560], BF16)
            w_out = sbuf(st1, "w_out", [128, 8, D], BF16)
            lora_up = sbuf(st1, "lora_up", [128, 512], BF16); g_up = sbuf(st1, "g_up", [128, 512], BF16)
            wr = sbuf(st1, "wr", [128, 8, 32], BF16)

            dma("sp", chp[:], ch_d, [], [chp], "p0"); dma("sp", mu[:], mu_d, [], [mu], "p1")
            dma("sp", g1[:], g1_d, [], [g1], "p2"); dma("sp", qkg[:], qk_d, [], [qkg], "p3")
            dma("sp", esink[:], sink_d, [], [esink], "p4")
            dma("sp", brt[:], br_d.partition_broadcast(128), [], [brt], "p5")
            dma("sp", lnx_b[:], lnx_d.partition_broadcast(128), [], [lnx_b], "p6")
            dma("pool", lora_up[:], lora_d, [], [lora_up], "p7"); dma("pool", g_up[:], gup_d, [], [g_up], "p8")
            dma("pool", wr[:], wr_d.rearrange("(k p) e -> p k e", p=128), [], [wr], "p9")
            for kc in range(8):
                dma("pool", w_in[:, kc, :], win_d[kc * 128:(kc + 1) * 128, :], [], [w_in], "wi")
                dma("pool", w_out[:, kc, :], wout_d[kc * 128:(kc + 1) * 128, :], [], [w_out], "wo")
            act(esink[:], esink[:], AF.Exp, [esink], [esink])
            ts("dve", omka[:], chp[:, 3, :], -1.0, 1.0, ALU.mult, ALU.add, [chp], [omka])
            ts("dve", qkg[:, 0:1], qkg[:, 0:1], 0.125, None, ALU.mult, None, [qkg], [qkg])

            with ExitStack() as st2:
                cfm = sbuf(st2, "cfm", [128, 8]); sc32 = sbuf(st2, "sc32", [128, 8])
                screp = sbuf(st2, "screp", [128, 8, 128])
                badafm = sbuf(st2, "badafm", [128, 48])
                wblk = [sbuf(st2, "wblk%d" % i, [128, 8, 512]) for i in range(2)]
                brow = [sbuf(st2, "brow%d" % i, [128, 512]) for i in range(2)]
                g2b = sbuf(st2, "g2b", [128, D]); gt1_b = sbuf(st2, "gt1_b", [128, D])
                dma("sp", cfm[:], cfm_d, [], [cfm], "a0"); dma("sp", badafm[:], bada_fm_d, [], [badafm], "a1")
                dma("sp", g2b[:], g2_d.partition_broadcast(128), [], [g2b], "a2")
                act(sc32[:], cfm[:], AF.Silu, [cfm], [sc32])
                for kc in range(8):
                    cp("dve", screp[:, kc, :], sc32[:, kc:kc + 1].to_broadcast([128, 128]), [sc32], [screp])
                wv = wada_d.rearrange("(k p) n -> p k n", p=128)
                bdst = {4: gt1_b[:, 0:512], 5: gt1_b[:, 512:1024], 6: sh2_b[:, 0:512], 7: sh2_b[:, 512:1024],
                        8: s2_b[:, 0:512], 9: s2_b[:, 512:1024], 10: gt2_b[:, 0:512], 11: gt2_b[:, 512:1024]}
                btl = {4: gt1_b, 5: gt1_b, 6: sh2_b, 7: sh2_b, 8: s2_b, 9: s2_b, 10: gt2_b, 11: gt2_b}
                bi_fm, bk_fm = bank()
                for blk in range(12):
                    wb = wblk[blk % 2]
                    dma("sp", wb[:], wv[:, :, blk * 512:(blk + 1) * 512], [], [wb], "wa%d" % (blk % 2))
                    if blk < 4:
                        for j in range(4):
                            col = blk * 4 + j
                            for kc in range(8):
                                mm(pv(bi_fm)[:, col:col + 1], wb[:, kc, j * 128:(j + 1) * 128], sc32[:, kc:kc + 1],
                                   [wb, sc32], [bk_fm], start=(kc == 0), stop=(kc == 7), inc=(kc == 7))
                    else:
                        br_ = brow[blk % 2]
                        dma("sp", br_[:], bada_row_d[:, blk * 512:(blk + 1) * 512].partition_broadcast(128), [], [br_], "wr%d" % (blk % 2))
                        bi, bk = bank()
                        for kc in range(8):
                            mm(pv(bi), screp[:, kc, :], wb[:, kc, :], [wb, screp], [bk], start=(kc == 0), stop=(kc == 7), inc=(kc == 7))
                        tt("dve", bdst[blk], pv(bi), br_[:], ALU.add, [bk, br_], [btl[blk]])
                    if blk == 3:
                        tt("dve", modfm[:, 0:16], pv(bi_fm)[:, 0:16], badafm[:, 0:16], ALU.add, [bk_fm, badafm], [modfm])
                stt("dve", s1[:], modfm[:, 8:16], 1.0, g1[:], ALU.add, ALU.mult, [modfm, g1], [s1])
                stt("dve", s2_b[:], s2_b[:], 1.0, g2b[:], ALU.add, ALU.mult, [s2_b, g2b], [s2_b])
                for kc in range(8):
                    tt("dve", w_out[:, kc, :], w_out[:, kc, :], gt1_b[:], ALU.mult, [w_out, gt1_b], [w_out])
                if dbg_chunk is None:
                    for q in range(0, NSLOT, 1024):
                        r_ = min(1024, NSLOT - q)
                        dma("act", xs_d[q:q + r_, :], zer_d[0:r_, :], [], [], "zf")
                P.barrier(exclude=("zf",))
            sh1 = modfm[:, 0:8]

            xt = [sbuf(st1, "xt%d" % i, [128, D]) for i in range(2)]
            xn = sbuf(st1, "xn", [128, D], BF16)
            sm = sbuf(st1, "sm", [128, 16]); smB = sbuf(st1, "smB", [128, 16])
            hT = sbuf(st1, "hT", [128, 8, 128], BF16)
            pbuf = sbuf(st1, "pbuf", [128, 14, 129])
            psh = sbuf(st1, "psh", [128, 14, 128])
            lo_bf = sbuf(st1, "lo_bf", [128, 128], BF16); sg_bf = sbuf(st1, "sg_bf", [128, 128], BF16)
            sw = sbuf(st1, "sw", [128, 4, 128]); aa = sbuf(st1, "aa", [128, 4, 128])
            cs = sbuf(st1, "cs", [128, 4, 128])
            csC = sbuf(st1, "csC", [128, 4])
            gC = [sbuf(st1, "gC%d" % i, [128, 4]) for i in range(2)]
            kk = sbuf(st1, "kk", [128, 4, 128])
            k2 = sbuf(st1, "k2", [128, 4, 128]); beta = sbuf(st1, "beta", [128, 4, 128])
            e1 = sbuf(st1, "e1", [128, 4, 128]); e2 = sbuf(st1, "e2", [128, 4, 128])
            abp = sbuf(st1, "abp", [128, 4, 2, 128], BF16)
            kbar = sbuf(st1, "kbar", [128, 4, 128], BF16); bbar = sbuf(st1, "bbar", [128, 4, 128], BF16)
            fm3 = sbuf(st1, "fm3", [128, 2, 4, 128], BF16)
            wrk = sbuf(st1, "wrk", [128, 4, 128], BF16)
            tm3 = sbuf(st1, "tm3", [128, 3, 512], BF16)
            v32 = sbuf(st1, "v32", [128, 512]); vtm = sbuf(st1, "vtm", [128, 512], BF16)
            gtm = sbuf(st1, "gtm", [128, 512])
            atb = sbuf(st1, "atb", [128, 8, 256], BF16); atk = sbuf(st1, "atk", [128, 8, 256], BF16)
            Lf = [sbuf(st1, "Lf%d" % g, [128, 4, 2, 128], BF16) for g in range(2)]
            Pq = [[sbuf(st1, "Pq%d_%d" % (g, i), [128, 4, 2, 128], BF16) for i in range(2)] for g in range(2)]
            Mq = [[sbuf(st1, "Mq%d_%d" % (g, i), [128, 4, 2, 128], BF16) for i in range(2)] for g in range(2)]
            Xb0 = sbuf(st1, "Xb0", [128, 8, 128], BF16); Xbf = sbuf(st1, "Xbf", [128, 8, 128], BF16)
            RpT = sbuf(st1, "RpT", [128, 4, 128], BF16)
            Y0 = sbuf(st1, "Y0", [128, 512]); TpT = sbuf(st1, "TpT", [128, 4, 64], BF16); D32 = sbuf(st1, "D32", [128, 4, 64])
            H32 = sbuf(st1, "H32", [128, 4, 64]); Hbf = [sbuf(st1, "Hbf%d" % i, [128, 4, 64], BF16) for i in range(2)]
            yy = sbuf(st1, "yy", [128, 8, 64]); ysq = sbuf(st1, "ysq", [128, 8, 64])
            gn = sbuf(st1, "gn", [128, 6, 8]); bon = sbuf(st1, "bon", [128, 8])
            obf = sbuf(st1, "obf", [128, 512], BF16)
            mixR = sbuf(st1, "mixR", [128, 4, 128], BF16); matt = [sbuf(st1, "matt%d" % i, [128, 4, 128], BF16) for i in range(2)]
            qsq = sbuf(st1, "qsq", [128, 5, 128], BF16); qrs = sbuf(st1, "qrs", [128, 5, 128])
            qhat = sbuf(st1, "qhat", [128, 4, 128], BF16)
            khat = [sbuf(st1, "khat%d" % i, [128, 128], BF16) for i in range(2)]
            vat = [sbuf(st1, "vat%d" % i, [128, 128], BF16) for i in range(2)]
            ptm = [sbuf(st1, "ptm%d" % i, [128, 4, 128], BF16) for i in range(4)]
            h2 = [sbuf(st1, "h2_0", [128, D], BF16)] * 2
            msk = sbuf(st1, "msk", [128, 32], BF16); ex4 = sbuf(st1, "ex4", [128, 4])

            P.op("dve", lambda e: e.memset(pbuf[:], 0.0), [], _bl([pbuf]))
            if os.environ.get("KMARK"):
                print("SBUF remaining after mixer alloc", nc.sbuf_bytes_remaining)
            P.op("dve", lambda e: e.memset(H32[:], 0.0), [], _bl([H32]))
            P.op("dve", lambda e: e.memset(Hbf[0][:], 0.0), [], _bl([Hbf[0]]))

            nchunks = nch if dbg_chunk is None else dbg_chunk + 1
            def Fe(n):
                dg = (n == dbg_chunk)
                xc = xt[n % 2]
                dma("sp", xc[:], x_d[n * 128:(n + 1) * 128, :], [], [xc], "x%d" % (n % 2))
                mark("norm1")
                act(xn[:], xc[:], AF.Square, [xc], [xn, sm], accum=sm[:, 0:1])
                ts("dve", sm[:, 1:2], sm[:, 0:1], 1.0 / D, NORM_EPS, ALU.mult, ALU.add, [sm], [sm])
                act(sm[:, 1:2], sm[:, 1:2], AF.Sqrt, [sm], [sm])
                P.op("dve", lambda e: e.reciprocal(out=sm[:, 2:3], in_=sm[:, 1:2]), _bl([sm]), _bl([sm]))
                act(xn[:], xc[:], AF.Copy, [xc, sm], [xn], scale=sm[:, 2:3])
                bi, bk = bank()
                for kc in range(8):
                    P.op("pe", lambda e, kc=kc, bi=bi: e.transpose(pvb(bi)[:, kc * 128:(kc + 1) * 128], xn[:, kc * 128:(kc + 1) * 128], ident),
                         _bl([xn, cst]), _bl([bk]), inc=(kc == 7))
                for kc in range(8):
                    act(hT[:, kc, :], pvb(bi)[:, kc * 128:(kc + 1) * 128], AF.Identity, [bk, s1, modfm], [hT],
                        bias=sh1[:, kc:kc + 1], scale=s1[:, kc:kc + 1])
                if dg:
                    dbg("hT", hT[:].rearrange("p a n -> p (a n)"), [128, 1024], BF16, [hT])
                mark("inproj")
                pb = []
                for grp in range(5):
                    bi, bk = bank()
                    pb.append((bi, bk))
                    for j in range(4):
                        fc = grp * 4 + j
                        if fc == 19:
                            for kc in range(8):
                                mm(pv(bi)[:, j * 128:(j + 1) * 128], hT[:, kc, :], w_in[:, kc, 2432:2560], [hT, w_in], [bk],
                                   start=(kc == 0), stop=(kc == 7), inc=(kc == 7))
                            continue
                        col = fc * 128
                        for kc in range(8):
                            mm(pv(bi)[:, j * 128:(j + 1) * 128], w_in[:, kc, col:col + 128], hT[:, kc, :], [hT, w_in], [bk],
                               start=(kc == 0), stop=(kc == 7), inc=(kc == 7))
                    if grp < 4:
                        nj = 4 if grp < 3 else 2
                        cp("act" if grp % 2 == 0 else "dve", pbuf[:, grp * 4:grp * 4 + nj, 1:129], pv(bi)[:, 0:nj * 128].rearrange("p (a n) -> p a n", a=nj), [bk], [pbuf])
                mark("tokshift")
                tt("dve", psh[:], pbuf[:, :, 0:128], pbuf[:, :, 1:129], ALU.subtract, [pbuf], [psh])
                tt("pool", psh[:], psh[:], mu[:].unsqueeze(2).to_broadcast([128, 14, 128]), ALU.mult, [psh, mu], [psh])
                tt("dve", psh[:], psh[:], pbuf[:, :, 1:129], ALU.add, [psh, pbuf], [psh])
                cp("pool", pbuf[:, :, 0:1], pbuf[:, :, 128:129], [pbuf], [pbuf])
                if dg:
                    dbg("psh", psh[:].rearrange("p a n -> p (a n)"), [128, 14 * 128], F32, [psh])
                rr_ = psh[:, 0:4, :]; kr_ = psh[:, 4:8, :]; vr_ = psh[:, 8:12, :]
                mark("qknorm")
                qbi, qbk = pb[3][0], pb[3][1]
                q2bi, q2bk = pb[4]
                qv = [pv(qbi)[:, 256:384], pv(qbi)[:, 384:512], pv(q2bi)[:, 0:128], pv(q2bi)[:, 128:256], pv(q2bi)[:, 256:384]]
                qb_ = [qbk, qbk, q2bk, q2bk, q2bk]
                for j in range(5):
                    act(qsq[:, j, :], qv[j], AF.Square, [qb_[j]], [qsq])
                bi, bk = bank(); bi2, bk2 = bank()
                for j in range(5):
                    bb_i, bb_k = (bi, bk) if j < 4 else (bi2, bk2)
                    mm(pv(bb_i)[:, (j % 4) * 128:(j % 4 + 1) * 128], bdiag, qsq[:, j, :], [qsq, cst], [bb_k], inc=(j >= 3))
                ts("dve", qrs[:, 0:4, :], pv(bi).rearrange("p (a n) -> p a n", a=4), 1.0 / 64, NORM_EPS, ALU.mult, ALU.add, [bk], [qrs])
                ts("dve", qrs[:, 4, :], pv(bi2)[:, 0:128], 1.0 / 64, NORM_EPS, ALU.mult, ALU.add, [bk2], [qrs])
                act(qrs[:], qrs[:], AF.Sqrt, [qrs], [qrs])
                P.op("dve", lambda e: e.reciprocal(out=qrs[:], in_=qrs[:]), _bl([qrs]), _bl([qrs]))
                for j in range(4):
                    stt("dve", qhat[:, j, :], qv[j], qkg[:, 0:1], qrs[:, j, :], ALU.mult, ALU.mult, [qb_[j], qkg, qrs], [qhat])
                kc_ = khat[n % 2]; kp_ = khat[(n + 1) % 2]
                vc_ = vat[n % 2]; vp_ = vat[(n + 1) % 2]
                stt("dve", kc_[:], qv[4], qkg[:, 1:2], qrs[:, 4, :], ALU.mult, ALU.mult, [q2bk, qkg, qrs], [kc_])
                cp("act", vc_[:], pv(q2bi)[:, 384:512], [q2bk], [vc_])
                if dg:
                    dbg("qhat", qhat[:].rearrange("p a n -> p (a n)"), [128, 512], BF16, [qhat])
                    dbg("khat", kc_[:], [128, 128], BF16, [kc_])
                kcur[0] = (kc_, kp_, vc_, vp_)
            def Fl(n):
                dg = (n == dbg_chunk)
                xc = xt[n % 2]
                kc_, kp_, vc_, vp_ = khat[n % 2], khat[(n + 1) % 2], vat[n % 2], vat[(n + 1) % 2]
                rr_ = psh[:, 0:4, :]; kr_ = psh[:, 4:8, :]; vr_ = psh[:, 8:12, :]
                mark("attn")
                kbs = ([(kp_, vp_, tri_gt)] if n > 0 else []) + [(kc_, vc_, tri_le)]
                pts = []
                for kv in range(2):
                    Pp = slice(64 * kv, 64 * kv + 64)
                    for ib, (kt_, vt_, mk_) in enumerate(kbs):
                        bi, bk = bank()
                        mm(pv(bi).rearrange("p (a n) -> p a n", a=4), kt_[Pp, :], qhat[Pp, :, :], [kt_, qhat], [bk])
                        pm_ = ptm[kv * 2 + ib]
                        act(pm_[:], pv(bi).rearrange("p (a n) -> p a n", a=4), AF.Exp, [bk], [pm_])
                        tt("pool", pm_[:], pm_[:], mk_.unsqueeze(1).to_broadcast([128, 4, 128]), ALU.mult, [pm_, cst], [pm_])
                        pts.append((kv, pm_, vt_))
                obi, obk = bank(); dbi, dbk = bank()
                for kv in range(2):
                    Pp = slice(64 * kv, 64 * kv + 64)
                    lst = [p_ for p_ in pts if p_[0] == kv]
                    for ii, (_, pm_, vt_) in enumerate(lst):
                        mm(pv(obi)[Pp, :], vt_[:, Pp], pm_[:].rearrange("p a n -> p (a n)"), [vt_, pm_], [obk],
                           start=(ii == 0), stop=(ii == len(lst) - 1), inc=False)
                    for ii, (_, pm_, vt_) in enumerate(lst):
                        mm(pv(dbi)[Pp, :], ones_bf[:, 0:64], pm_[:].rearrange("p a n -> p (a n)"), [pm_, cst], [dbk],
                           start=(ii == 0), stop=(ii == len(lst) - 1), inc=(kv == 1 and ii == len(lst) - 1))
                den = e1[:]
                tt("dve", den, pv(dbi).rearrange("p (a n) -> p a n", a=4), esink[:].unsqueeze(2).to_broadcast([128, 4, 128]), ALU.add, [dbk, esink], [e1])
                P.op("dve", lambda e: e.reciprocal(out=den, in_=den), _bl([e1]), _bl([e1]))
                tt("dve", matt[n % 2][:], pv(obi).rearrange("p (a n) -> p a n", a=4), den, ALU.mult, [obk, e1], [matt[n % 2]])

                mark("rwkvprep")
                act(lo_bf[0:64, :], psh[0:64, 12, :], AF.Tanh, [psh], [lo_bf])
                cp("pool", lo_bf[64:128, :], psh[64:128, 12, :], [psh], [lo_bf])
                act(sg_bf[:], psh[:, 13, :], AF.Sigmoid, [psh], [sg_bf])
                zbi, zbk = bank(); abi, abk = bank(); gbi, gbk = bank()
                for c in range(4):
                    mm(pv(zbi)[:, c * 128:(c + 1) * 128], lora_up[0:64, c * 128:(c + 1) * 128], lo_bf[0:64, :], [lora_up, lo_bf], [zbk], inc=(c == 3))
                for c in range(4):
                    mm(pv(abi)[:, c * 128:(c + 1) * 128], lora_up[64:128, c * 128:(c + 1) * 128], lo_bf[64:128, :], [lora_up, lo_bf], [abk], inc=(c == 3))
                mm(pv(gbi), sg_bf[:], g_up[:], [sg_bf, g_up], [gbk])
                for c in range(4):
                    act(sw[:, c, :], pv(zbi)[:, c * 128:(c + 1) * 128], AF.Sigmoid, [zbk, chp], [sw], bias=chp[:, 0, c:c + 1])
                    act(aa[:, c, :], pv(abi)[:, c * 128:(c + 1) * 128], AF.Sigmoid, [abk, chp], [aa], bias=chp[:, 1, c:c + 1])
                cp("act", gtm[:], pv(gbi), [gbk], [gtm])
                for c in range(4):
                    P.op("dve", lambda e, c=c: e.tensor_tensor_scan(out=cs[:, c, :], data0=ones_f, data1=sw[:, c, :], initial=0.0,
                                                                     op0=ALU.mult, op1=ALU.add), _bl([sw, cstf]), _bl([cs]))
                ts("dve", csC[:], cs[:, :, 127], -DECAY_K, None, ALU.mult, None, [cs], [csC])
                gCn = gC[n % 2]
                act(gCn[:], csC[:], AF.Exp, [csC], [gCn])
                tt("dve", kk[:], kr_, chp[:, 2, :].unsqueeze(2).to_broadcast([128, 4, 128]), ALU.mult, [psh, chp], [kk])
                tt("pool", wrk[:], kk[:], kk[:], ALU.mult, [kk], [wrk])
                sbi, sbk = bank()
                for c in range(4):
                    mm(pv(sbi)[:, c * 128:(c + 1) * 128], bdiag, wrk[:, c, :], [wrk, cst], [sbk], inc=(c == 3))
                act(e2[:], pv(sbi).rearrange("p (a n) -> p a n", a=4), AF.Sqrt, [sbk], [e2])
                ts("dve", e2[:], e2[:], 1e-12, None, ALU.max, None, [e2], [e2])
                P.op("dve", lambda e: e.reciprocal(out=e2[:], in_=e2[:]), _bl([e2]), _bl([e2]))
                tt("dve", kk[:], kk[:], e2[:], ALU.mult, [kk, e2], [kk])
                tt("pool", k2[:], aa[:], chp[:, 3, :].unsqueeze(2).to_broadcast([128, 4, 128]), ALU.mult, [aa, chp], [k2])
                tt("pool", k2[:], k2[:], omka[:].unsqueeze(2).to_broadcast([128, 4, 128]), ALU.add, [k2, omka], [k2])
                tt("dve", k2[:], k2[:], kr_, ALU.mult, [k2, psh], [k2])
                tt("pool", beta[:], kk[:], aa[:], ALU.mult, [kk, aa], [beta])
                if dg:
                    dbg("sw", sw[:].rearrange("p a n -> p (a n)"), [128, 512], F32, [sw])
                    dbg("aa", aa[:].rearrange("p a n -> p (a n)"), [128, 512], F32, [aa])
                    dbg("kkn", kk[:].rearrange("p a n -> p (a n)"), [128, 512], F32, [kk])
                    dbg("k2", k2[:].rearrange("p a n -> p (a n)"), [128, 512], F32, [k2])
                mark("scaled")
                act(e1[:], cs[:], AF.Exp, [cs], [e1], scale=-DECAY_K)
                tt("dve", abp[:, :, 1, :], rr_, e1[:], ALU.mult, [psh, e1], [abp])
                act(e2[:], cs[:], AF.Exp, [cs], [e2], scale=DECAY_K)
                tt("pool", kbar[:], k2[:], e2[:], ALU.mult, [k2, e2], [kbar])
                tt("pool", bbar[:], beta[:], e2[:], ALU.mult, [beta, e2], [bbar])
                tt("pool", e1[:], cs[:], sw[:], ALU.subtract, [cs, sw], [e1])
                act(e1[:], e1[:], AF.Exp, [e1], [e1], scale=-DECAY_K)
                stt("dve", abp[:, :, 0, :], kk[:], -1.0, e1[:], ALU.mult, ALU.mult, [kk, e1], [abp])
                for c in range(4):
                    act(e2[:, c, :], cs[:, c, :], AF.Exp, [cs, csC], [e2], bias=csC[:, c:c + 1], scale=DECAY_K)
                tt("dve", fm3[:, 0, :, :], k2[:], e2[:], ALU.mult, [k2, e2], [fm3])
                tt("pool", fm3[:, 1, :, :], beta[:], e2[:], ALU.mult, [beta, e2], [fm3])
                ysq4 = ysq[:].rearrange("p h n -> p (h n)").rearrange("p (a n) -> p a n", a=4)
                tt("pool", ysq4, rr_, k2[:], ALU.mult, [psh, k2], [ysq])
                tt("pool", wrk[:], ysq4, chp[:, 4, :].unsqueeze(2).to_broadcast([128, 4, 128]), ALU.mult, [ysq, chp], [wrk])
                mark("transp")
                tb0, tk0 = bank(); tb1, tk1 = bank()
                for a3 in range(3):
                    for c in range(4):
                        idx = a3 * 4 + c
                        tb_, tk_ = (tb0, tk0) if idx < 8 else (tb1, tk1)
                        off = (idx % 8) * 128
                        src_ = abp[:, c, 0, :] if a3 == 0 else fm3[:, a3 - 1, c, :]
                        P.op("pe", lambda e, src_=src_, tb_=tb_, off=off: e.transpose(pvb(tb_)[:, off:off + 128], src_, ident),
                             _bl([fm3, abp, cst]), _bl([tk_]), inc=(idx == 7 or idx == 11))
                cp("act", tm3[:, 0:2, :], pvb(tb0).rearrange("p (a n) -> p a n", a=2), [tk0], [tm3])
                cp("dve", tm3[:, 2, :], pvb(tb1)[:, 0:512], [tk1], [tm3])
                vb_, vk_ = bank()
                for c in range(4):
                    P.op("pe", lambda e, c=c, vb_=vb_: e.transpose(pv(vb_)[:, c * 128:(c + 1) * 128], psh[:, 8 + c, :], ident_f),
                         _bl([psh, cstf]), _bl([vk_]), inc=(c == 3))
                cp("act", v32[:], pv(vb_), [vk_], [v32])
                cp("dve", vtm[:], pv(vb_), [vk_], [vtm])
                A_TM = tm3[:, 0, :]; Kt_TM = tm3[:, 1, :]; Bt_TM = tm3[:, 2, :]
            def B1(n):
                dg = (n == dbg_chunk)
                xc = xt[n % 2]
                gCn = gC[n % 2]
                A_TM = tm3[:, 0, :]; Kt_TM = tm3[:, 1, :]; Bt_TM = tm3[:, 2, :]
                mark("chunkmat")
                mkAT = cst[:, 1:3, :].unsqueeze(1).to_broadcast([128, 2, 2, 128])
                for c in range(4):
                    g, cc = c // 2, c % 2
                    ai2, ak2 = bank2(); li2, lk2 = bank2()
                    for hh in range(2):
                        Pp = slice(64 * hh, 64 * hh + 64)
                        mm(pv(ai2 + hh)[:, 0:256].rearrange("p (a n) -> p a n", a=2), bbar[Pp, c, :], abp[Pp, c, :, :], [bbar, abp], [ak2[hh]], inc=False)
                        mm(pv(ai2 + hh)[:, 256:512].rearrange("p (a n) -> p a n", a=2), kbar[Pp, c, :], abp[Pp, c, :, :], [kbar, abp], [ak2[hh]])
                        mm(pv(li2 + hh)[:, 0:128], abp[Pp, c, 0, :], bbar[Pp, c, :], [bbar, abp], [lk2[hh]])
                    vat_ = psum_t[:, ai2:ai2 + 2, :].rearrange("p h (s n) -> p h s n", s=4)
                    vl_ = psum_t[:, li2:li2 + 2, 0:128]
                    tt("dve", atb[:, 2 * c:2 * c + 2, :].rearrange("p h (a n) -> p h a n", a=2), vat_[:, :, 0:2, :], mkAT, ALU.mult, ak2 + [cst], [atb])
                    tt("dve", atk[:, 2 * c:2 * c + 2, :].rearrange("p h (a n) -> p h a n", a=2), vat_[:, :, 2:4, :], mkAT, ALU.mult, ak2 + [cst], [atk])
                    tt("dve", Pq[g][0][:, 2 * cc:2 * cc + 2, 1, :], vat_[:, :, 0, :], cst[:, 7, :].unsqueeze(1).to_broadcast([128, 2, 128]), ALU.mult, ak2 + [cst], [Pq[g][0]])
                    tt("dve", Lf[g][:, 2 * cc:2 * cc + 2, 0, :], vl_, tri_gt.unsqueeze(1).to_broadcast([128, 2, 128]), ALU.mult, lk2 + [cst], [Lf[g]])
                    tt("dve", Pq[g][0][:, 2 * cc:2 * cc + 2, 0, :], vl_, cst[:, 6, :].unsqueeze(1).to_broadcast([128, 2, 128]), ALU.mult, lk2 + [cst], [Pq[g][0]])
                idb = ident.unsqueeze(1).unsqueeze(1).to_broadcast([128, 4, 2, 128])
                for g in range(2):
                    cp("pool", Lf[g][:, :, 1, :], atb[:, 4 * g:4 * g + 4, 0:128], [atb], [Lf[g]])
                    xbi, xbk = bank()
                    for h4 in range(4):
                        h = 4 * g + h4
                        mm(pv(xbi)[:, h4 * 64:(h4 + 1) * 64], atk[:, h, 0:128], vtm[:, h * 64:(h + 1) * 64], [atk, vtm], [xbk], inc=(h4 == 3))
                    cp("act", Xb0[:, 4 * g:4 * g + 4, 64:128], pv(xbi)[:, 0:256].rearrange("p (h n) -> p h n", h=4), [xbk], [Xb0])
                    cp("pool", Xb0[:, 4 * g:4 * g + 4, 0:64], A_TM[:, 256 * g:256 * g + 256].rearrange("p (h n) -> p h n", h=4), [tm3], [Xb0])
                    tt("pool", Mq[g][0][:], Pq[g][0][:], idb, ALU.add, [Pq[g][0], cst], [Mq[g][0]])
                pc, mc = 0, 0
                for it in range(3):
                    for g in range(2):
                        Pc = Pq[g][pc]; Pn = Pq[g][1 - pc]; Mc = Mq[g][mc]; Mn = Mq[g][1 - mc]
                        si, sk = bank2()
                        for h4 in range(4):
                            mm(pv(si, 2)[:, h4 * 256:h4 * 256 + 128], Pc[:, h4, 1, :], Pc[:, h4, 0, :], [Pc], sk, inc=False)
                            mm(pv(si, 2)[:, h4 * 256 + 128:h4 * 256 + 256], Pc[:, h4, 0, :], Pc[:, h4, 1, :], [Pc], sk, inc=(h4 == 3))
                        cp("act", Pn[:], pv(si, 2).rearrange("p (h a n) -> p h a n", h=4, a=2), sk, [Pn])
                        gi_, gk_ = bank2()
                        for h4 in range(4):
                            mm(pv(gi_, 2)[:, h4 * 256:h4 * 256 + 128], Pn[:, h4, 1, :], Mc[:, h4, 0, :], [Pn, Mc], gk_, start=True, stop=False, inc=False)
                            mm(pv(gi_, 2)[:, h4 * 256:h4 * 256 + 128], ident, Mc[:, h4, 0, :], [Mc, cst], gk_, start=False, stop=True, inc=False)
                            mm(pv(gi_, 2)[:, h4 * 256 + 128:h4 * 256 + 256], Mc[:, h4, 0, :], Pn[:, h4, 1, :], [Pn, Mc], gk_, start=True, stop=False, inc=False)
                            mm(pv(gi_, 2)[:, h4 * 256 + 128:h4 * 256 + 256], ident, Mc[:, h4, 1, :], [Mc, cst], gk_, start=False, stop=True, inc=(h4 == 3))
                        cp("act", Mn[:], pv(gi_, 2).rearrange("p (h a n) -> p h a n", h=4, a=2), gk_, [Mn])
                    pc, mc = 1 - pc, 1 - mc
                for mi in (8, 10, 12):
                    for g in range(2):
                        Mc = Mq[g][mc]; Mn = Mq[g][1 - mc]; Zq = Pq[g][pc]
                        zi, zk = bank2()
                        for h4 in range(4):
                            mm(pv(zi, 2)[:, h4 * 256:h4 * 256 + 128], Lf[g][:, h4, 1, :], Mc[:, h4, 0, :], [Lf[g], Mc], zk, inc=False)
                            mm(pv(zi, 2)[:, h4 * 256 + 128:h4 * 256 + 256], Lf[g][:, h4, 0, :], Mc[:, h4, 1, :], [Lf[g], Mc], zk, inc=(h4 == 3))
                        tt("dve", Zq[:], pv(zi, 2).rearrange("p (h a n) -> p h a n", h=4, a=2), cst[:, mi:mi + 2, :].unsqueeze(1).to_broadcast([128, 4, 2, 128]), ALU.mult, zk + [cst], [Zq])
                        gi_, gk_ = bank2()
                        for h4 in range(4):
                            mm(pv(gi_, 2)[:, h4 * 256:h4 * 256 + 128], Mc[:, h4, 1, :], Zq[:, h4, 0, :], [Zq, Mc], gk_, start=True, stop=False, inc=False)
                            mm(pv(gi_, 2)[:, h4 * 256:h4 * 256 + 128], ident, Mc[:, h4, 0, :], [Mc, cst], gk_, start=False, stop=True, inc=False)
                            mm(pv(gi_, 2)[:, h4 * 256 + 128:h4 * 256 + 256], Mc[:, h4, 0, :], Zq[:, h4, 1, :], [Zq, Mc], gk_, start=True, stop=False, inc=False)
                            mm(pv(gi_, 2)[:, h4 * 256 + 128:h4 * 256 + 256], ident, Mc[:, h4, 1, :], [Mc, cst], gk_, start=False, stop=True, inc=(h4 == 3))
                        cp("act", Mn[:], pv(gi_, 2).rearrange("p (h a n) -> p h a n", h=4, a=2), gk_, [Mn])
                    mc = 1 - mc
                for g in range(2):
                    Mc = Mq[g][mc]
                    fi, fk = bank()
                    for h4 in range(4):
                        mm(pv(fi)[:, h4 * 128:(h4 + 1) * 128], Mc[:, h4, 1, :], Xb0[:, 4 * g + h4, :], [Mc, Xb0], [fk], inc=(h4 == 3))
                    cp("act", Xbf[:, 4 * g:4 * g + 4, :], pv(fi).rearrange("p (h n) -> p h n", h=4), [fk], [Xbf])
                if dg:
                    dbg("atb", atb[:].rearrange("p a n -> p (a n)"), [128, 2048], BF16, [atb])
                    dbg("atk", atk[:].rearrange("p a n -> p (a n)"), [128, 2048], BF16, [atk])
                    dbg("abp", abp[:].rearrange("p c a n -> p (c a n)"), [128, 1024], BF16, [abp])
                    dbg("bbar", bbar[:].rearrange("p c n -> p (c n)"), [128, 512], BF16, [bbar])
                    dbg("kbar", kbar[:].rearrange("p c n -> p (c n)"), [128, 512], BF16, [kbar])
                    dbg("tm3", tm3[:].rearrange("p c n -> p (c n)"), [128, 1536], BF16, [tm3])
                    dbg("vtm", vtm[:], [128, 512], BF16, [vtm])
                    dbg("x0", Xb0[:].rearrange("p h n -> p (h n)"), [128, 1024], BF16, [Xb0])
                    dbg("xf", Xbf[:].rearrange("p h n -> p (h n)"), [128, 1024], BF16, [Xbf])
                mark("rpt")
                rbi, rbk = bank(); ybi, ybk = bank(); tbi, tbk = bank()
                for h in range(8):
                    c, hh = h // 2, h % 2
                    Pp = slice(64 * hh, 64 * hh + 64)
                    mm(pv(rbi)[Pp, c * 128:(c + 1) * 128], Xbf[:, h, 0:64], atb[:, h, 128:256], [Xbf, atb], [rbk], inc=(h == 7))
                tt("dve", RpT[:], pv(rbi).rearrange("p (a n) -> p a n", a=4), abp[:, :, 1, :], ALU.add, [rbk, abp], [RpT])
                for h in range(8):
                    mm(pv(ybi)[:, h * 64:(h + 1) * 64], atb[:, h, 128:256], Xbf[:, h, 64:128], [Xbf, atb], [ybk], start=True, stop=False, inc=False)
                    mm(pv(ybi)[:, h * 64:(h + 1) * 64], atk[:, h, 128:256], vtm[:, h * 64:(h + 1) * 64], [atk, vtm], [ybk], start=False, stop=True, inc=(h == 7))
                cp("act", Y0[:], pv(ybi), [ybk], [Y0])
                if dg:
                    dbg("y0", Y0[:], [128, 512], F32, [Y0])
                for h in range(8):
                    c, hh = h // 2, h % 2
                    Pp = slice(64 * hh, 64 * hh + 64)
                    mm(pv(tbi)[Pp, c * 64:(c + 1) * 64], Xbf[:, h, 0:64], Bt_TM[:, h * 64:(h + 1) * 64], [Xbf, tm3], [tbk], inc=False)
                    mm(pv(tbi)[Pp, 256 + c * 64:256 + (c + 1) * 64], Kt_TM[:, h * 64:(h + 1) * 64], vtm[:, h * 64:(h + 1) * 64], [tm3, vtm], [tbk], start=True, stop=False, inc=False)
                    mm(pv(tbi)[Pp, 256 + c * 64:256 + (c + 1) * 64], Bt_TM[:, h * 64:(h + 1) * 64], Xbf[:, h, 64:128], [tm3, Xbf], [tbk], start=False, stop=True, inc=(h == 7))
                cp("act", TpT[:], pv(tbi)[:, 0:256].rearrange("p (a n) -> p a n", a=4), [tbk], [TpT])
                cp("dve", D32[:], pv(tbi)[:, 256:512].rearrange("p (a n) -> p a n", a=4), [tbk], [D32])
                mark("serial")
                Hc = Hbf[n % 2]; Hn = Hbf[(n + 1) % 2]
                ob2 = [bank(), bank()]
                for h in range(8):
                    c, hh = h // 2, h % 2
                    Pp = slice(64 * hh, 64 * hh + 64)
                    mm(pv(ob2[hh][0])[:, c * 64:(c + 1) * 64], RpT[Pp, c, :], Hc[Pp, c, :], [RpT, Hc], [ob2[hh][1]], inc=(h >= 6))
                yyv = yy[:].rearrange("p (c q) n -> p c q n", q=2); y0v = Y0[:].rearrange("p (c q n) -> p c q n", q=2, n=64)
                for hh in range(2):
                    tt("dve", yyv[:, :, hh, :], pv(ob2[hh][0])[:, 0:256].rearrange("p (a n) -> p a n", a=4), y0v[:, :, hh, :], ALU.add, [ob2[hh][1], Y0], [yy])
                hb2 = [bank(), bank()]
                for h in range(8):
                    c, hh = h // 2, h % 2
                    Pp = slice(64 * hh, 64 * hh + 64)
                    mm(pv(hb2[hh][0])[Pp, c * 64:(c + 1) * 64], TpT[Pp, c, :], Hc[Pp, c, :], [TpT, Hc], [hb2[hh][1]], inc=(h >= 6))
                for hh in range(2):
                    Pp = slice(64 * hh, 64 * hh + 64)
                    tt("dve", D32[Pp, :, :], D32[Pp, :, :], pv(hb2[hh][0])[Pp, 0:256].rearrange("p (a n) -> p a n", a=4), ALU.add, [D32, hb2[hh][1]], [D32])
                for c in range(4):
                    stt("dve", H32[:, c, :], H32[:, c, :], gCn[:, c:c + 1], D32[:, c, :], ALU.mult, ALU.add, [H32, gCn, D32], [H32])
                cp("dve", Hn[:], H32[:], [H32], [Hn])
                if dg:
                    dbg("yy", yy[:].rearrange("p a n -> p (a n)"), [128, 512], F32, [yy])
                mark("gnorm")
                red("dve", gn[:, 0, :], yy[:], [yy], [gn])
                tt("pool", ysq[:], yy[:], yy[:], ALU.mult, [yy], [ysq])
                red("dve", gn[:, 1, :], ysq[:], [ysq], [gn])
                ts("dve", gn[:, 2, :], gn[:, 0, :], 1.0 / 64, None, ALU.mult, None, [gn], [gn])
                tt("dve", gn[:, 3, :], gn[:, 2, :], gn[:, 2, :], ALU.mult, [gn], [gn])
                stt("dve", gn[:, 4, :], gn[:, 1, :], 1.0 / 64, gn[:, 3, :], ALU.mult, ALU.subtract, [gn], [gn])
                ts("dve", gn[:, 4, :], gn[:, 4, :], GN_EPS, None, ALU.add, None, [gn], [gn])
                act(gn[:, 4, :], gn[:, 4, :], AF.Sqrt, [gn], [gn])
                P.op("dve", lambda e: e.reciprocal(out=gn[:, 5, :], in_=gn[:, 4, :]), _bl([gn]), _bl([gn]))
                tt("dve", yy[:], yy[:], gn[:, 2, :].unsqueeze(2).to_broadcast([128, 8, 64]), ALU.subtract, [yy, gn], [yy])
                tt("dve", yy[:], yy[:], gn[:, 5, :].unsqueeze(2).to_broadcast([128, 8, 64]), ALU.mult, [yy, gn], [yy])
                yf = yy[:].rearrange("p h n -> p (h n)")
                tt("pool", yf, yf, lnx_b[:, 0:512], ALU.mult, [yy, lnx_b], [yy])
                tt("pool", yf, yf, lnx_b[:, 512:1024], ALU.add, [yy, lnx_b], [yy])
                bbi, bbk = bank()
                for c in range(4):
                    mm(pv(bbi)[:, c * 2:(c + 1) * 2], wrk[:, c, :], bsel[:], [wrk, bsel], [bbk], inc=(c == 3))
                cp("act", bon[:], pv(bbi)[:, 0:8], [bbk], [bon])
                tt("dve", ysq[:], v32[:].rearrange("p (h n) -> p h n", h=8), bon[:].unsqueeze(2).to_broadcast([128, 8, 64]), ALU.mult, [v32, bon], [ysq])
                tt("dve", yy[:], yy[:], ysq[:], ALU.add, [yy, ysq], [yy])
                tt("dve", obf[:], yf, gtm[:], ALU.mult, [yy, gtm], [obf])
                if dg:
                    dbg("orwkv", obf[:], [128, 512], BF16, [obf])
            def B2(n):
                dg = (n == dbg_chunk)
                xc = xt[n % 2]
                tbi2, tbk2 = bank()
                for c in range(4):
                    P.op("pe", lambda e, c=c, tbi2=tbi2: e.transpose(pvb(tbi2)[:, c * 128:(c + 1) * 128], obf[:, c * 128:(c + 1) * 128], ident),
                         _bl([obf, cst]), _bl([tbk2]), inc=(c == 3))
                cp("act", mixR[:], pvb(tbi2)[:, 0:512].rearrange("p (a n) -> p a n", a=4), [tbk2], [mixR])
                if dg:
                    dbg("mixR", mixR[:].rearrange("p a n -> p (a n)"), [128, 512], BF16, [mixR])
                    dbg("mixA", matt[n % 2][:].rearrange("p a n -> p (a n)"), [128, 512], BF16, [matt[n % 2]])
                mark("outproj")
                x1c = xc; h2c = h2[n % 2]
                oi, ok = bank2()
                for nh in range(2):
                    for kc in range(8):
                        mt_ = mixR[:, kc, :] if kc < 4 else matt[n % 2][:, kc - 4, :]
                        mm(pv(oi, 2)[:, nh * 512:(nh + 1) * 512], mt_, w_out[:, kc, nh * 512:(nh + 1) * 512], [mixR, matt[n % 2], w_out], ok,
                           start=(kc == 0), stop=(kc == 7), inc=(kc == 7 and nh == 1))
                tt("dve", xc[:], pv(oi, 2), xc[:], ALU.add, ok + [xc], [xc])
                dma("sp", out_d[n * 128:(n + 1) * 128, :], x1c[:], [x1c], [], "x1o")
                if dg:
                    dbg("x1", x1c[:], [128, 1024], F32, [x1c])
                mark("norm2")
                act(h2c[:], x1c[:], AF.Square, [x1c], [h2c, smB], accum=smB[:, 4:5])
                ts("dve", smB[:, 5:6], smB[:, 4:5], 1.0 / D, NORM_EPS, ALU.mult, ALU.add, [smB], [smB])
                act(smB[:, 5:6], smB[:, 5:6], AF.Sqrt, [smB], [smB])
                P.op("dve", lambda e: e.reciprocal(out=smB[:, 6:7], in_=smB[:, 5:6]), _bl([smB]), _bl([smB]))
                h2f = atb[:].rearrange("p a n -> p (a n)").bitcast(F32)[:, 0:D]
                stt("dve", h2f, x1c[:], smB[:, 6:7], s2_b[:], ALU.mult, ALU.mult, [x1c, smB, s2_b], [atb])
                tt("pool", h2c[:], h2f, sh2_b[:], ALU.add, [atb, sh2_b], [h2c])
                dma("sp", hbuf_d[n * 128:(n + 1) * 128, :], h2c[:], [h2c], [], "h2o")
                mark("router")
                ti, tk_ = bank()
                for kc in range(8):
                    P.op("pe", lambda e, kc=kc, ti=ti: e.transpose(pvb(ti)[:, kc * 128:(kc + 1) * 128], h2c[:, kc * 128:(kc + 1) * 128], ident),
                         _bl([h2c, cst]), _bl([tk_]), inc=(kc == 7))
                h2T = atk[:].rearrange("p a n -> p (a n)")[:, 0:1024].rearrange("p (a n) -> p a n", a=8)
                cp("act", h2T, pvb(ti).rearrange("p (a n) -> p a n", a=8), [tk_], [atk])
                li, lk = bank()
                for kc in range(8):
                    mm(pv(li)[:, 0:32], h2T[:, kc, :], wr[:, kc, :], [atk, wr], [lk], start=(kc == 0), stop=(kc == 7), inc=(kc == 7))
                tt("dve", lg_all[:, n, :], pv(li)[:, 0:32], brt[:], ALU.add, [lk, brt], [lg_all])
                P.op("dve", lambda e, n=n: e.max(out=top_all[:, n, :], in_=lg_all[:, n, :]), _bl([lg_all]), _bl([top_all]))
                ts("dve", smB[:, 8:9], top_all[:, n, 0:1], -1.0, None, ALU.mult, None, [top_all], [smB])
                act(ex4[:], top_all[:, n, 0:4], AF.Exp, [top_all, smB], [ex4, smB], bias=smB[:, 8:9], accum=smB[:, 9:10])
                P.op("dve", lambda e: e.reciprocal(out=smB[:, 10:11], in_=smB[:, 9:10]), _bl([smB]), _bl([smB]))
                ts("dve", gate_all[:, n, :], ex4[:], smB[:, 10:11], None, ALU.mult, None, [ex4, smB], [gate_all])
                ts("dve", msk[:], lg_all[:, n, :], top_all[:, n, 3:4], None, ALU.is_ge, None, [lg_all, top_all], [msk])
                ci, ck = bank()
                mm(pv(ci)[:, 0:32], tri_lt, msk[:], [msk, cst], [ck], inc=False)
                mm(pv(ci)[:, 32:64], ones_bf, msk[:], [msk, cst], [ck])
                tt("dve", pos_all[:, n, :], pv(ci)[:, 0:32], carry[:], ALU.add, [ck, carry], [pos_all])
                tt("dve", carry[:], carry[:], pv(ci)[:, 32:64], ALU.add, [ck, carry], [carry])
            kcur = [None]
            Fe(0); Fl(0)
            for n in range(nchunks):
                if n + 1 < nchunks and not os.environ.get("KNOIL"):
                    interleave(P, lambda n=n: B1(n), lambda n=n: Fe(n + 1))
                    interleave(P, lambda n=n: B2(n), lambda n=n: Fl(n + 1))
                else:
                    B1(n); B2(n)
                    if n + 1 < nchunks:
                        Fe(n + 1); Fl(n + 1)
            if dbg_chunk is not None:
                dbg("lg", lg_all[:, 0:nchunks, :].rearrange("p a n -> p (a n)"), [128, nchunks * 32], F32, [lg_all])
            P.barrier()

        if dbg_chunk is None and not os.environ.get("KSKIPMOE"):
            with ExitStack() as st3:
                bst = sbuf(st3, "bst", [128, NB * 32])
                dma("sp", bst[:], bst_d, [], [bst], "bst")
                bstart = bst[:].rearrange("p (b e) -> p b e", b=NB)
                cmp8 = sbuf(st3, "cmp8", [128, 32, MB]); nblk = sbuf(st3, "nblk", [128, 32])
                pend = sbuf(st3, "pend", [128, 32]); pstart = sbuf(st3, "pstart", [128, 32])
                cmpb = sbuf(st3, "cmpb", [128, NB, 32]); eb = sbuf(st3, "eb", [128, NB])
                woff_f = sbuf(st3, "woff_f", [128, NB]); same = sbuf(st3, "same", [128, NB])
                vala = sbuf(st3, "vala", [128, NCH, 32]); oha = sbuf(st3, "oha", [128, NCH, 32])
                dest_f = sbuf(st3, "dest_f", [128, NCH, 4])
                hs = [sbuf(st3, "hs%d" % i, [128, D], BF16) for i in range(4)]
                tt("dve", cmp8[:], carry[:].unsqueeze(2).to_broadcast([128, 32, MB]), thr8.unsqueeze(1).to_broadcast([128, 32, MB]), ALU.is_gt, [carry, cst2], [cmp8])
                red("dve", nblk[:], cmp8[:], [cmp8], [nblk])
                ts("dve", nblk[:], nblk[:], float(BLK), None, ALU.mult, None, [nblk], [nblk])
                P.op("dve", lambda e: e.tensor_tensor_scan(out=pend[:], data0=ones_f[:, 0:32], data1=nblk[:], initial=0.0, op0=ALU.mult, op1=ALU.add),
                     _bl([nblk, cstf]), _bl([pend]))
                tt("dve", pstart[:], pend[:], nblk[:], ALU.subtract, [pend, nblk], [pstart])
                tt("dve", cmpb[:], bstart, pend[:].unsqueeze(1).to_broadcast([128, NB, 32]), ALU.is_ge, [pend, bst], [cmpb])
                red("dve", eb[:], cmpb[:], [cmpb], [eb])
                ts("dve", eb[:], eb[:], 31.0, None, ALU.min, None, [eb], [eb])
                ts("dve", woff_f[:], eb[:], 128.0, iota_p, ALU.mult, ALU.add, [eb, cst2], [woff_f])
                P.op("dve", lambda e: e.memset(same[:], 0.0), [], _bl([same]))
                tt("dve", same[:, 2:NB], eb[:, 2:NB], eb[:, 0:NB - 2], ALU.is_equal, [eb, same], [same])
                stt("dve", woff_f[:], same[:], 8192.0, woff_f[:], ALU.mult, ALU.add, [same, woff_f], [woff_f])
                cp("dve", woff[:], woff_f[:], [woff_f], [woff])
                tt("dve", vala[:, 0:nch, :], pos_all[:, 0:nch, :], pstart[:].unsqueeze(1).to_broadcast([128, nch, 32]), ALU.add, [pos_all, pstart], [vala])
                for k in range(4):
                    tt("dve", oha[:, 0:nch, :], lg_all[:, 0:nch, :], top_all[:, 0:nch, k:k + 1].to_broadcast([128, nch, 32]), ALU.is_equal, [lg_all, top_all], [oha])
                    tt("dve", oha[:, 0:nch, :], oha[:, 0:nch, :], vala[:, 0:nch, :], ALU.mult, [oha, vala], [oha])
                    red("dve", dest_f[:, 0:nch, k], oha[:, 0:nch, :], [oha], [dest_f])
                cp("dve", dest[:, 0:nch, :], dest_f[:, 0:nch, :], [dest_f], [dest])
                P._wait("pool", ("zf", P.dsem["zf"][1]))
                for n in range(nch):
                    hc = hs[n % 4]; dc = dcur[n % 2]
                    dma("sp", hc[:], hbuf_d[n * 128:(n + 1) * 128, :], [], [hc], "hs%d" % (n % 4))
                    cp("dve", dc[:], dest[:, n, :], [dest], [dc])
                    for k in range(4):
                        P.dma("pool", lambda e, k=k, hc=hc, dc=dc: e.indirect_dma_start(
                            out=xs_d, out_offset=bass.IndirectOffsetOnAxis(ap=dc[:, k:k + 1], axis=0),
                            in_=hc[:, :], in_offset=None, bounds_check=bc(e, NSLOT - 1), oob_is_err=False),
                            _bl([hc, dc]), [], key="sc%d" % (n % 4))
                P.barrier()

            with ExitStack() as st4:
                w1s = [sbuf(st4, "w1s%d" % i, [128, 8, 2048], BF16) for i in range(2)]
                w2s = [sbuf(st4, "w2s%d" % i, [128, 8, 1024], BF16) for i in range(2)]
                b1s = [sbuf(st4, "b1s%d" % i, [128, 16 + 1024]) for i in range(2)]
                b1l1 = [sbuf(st4, "b1l1_%d" % i, [128, 8]) for i in range(2)]
                xg = [sbuf(st4, "xg%d" % i, [128, JB, D], BF16) for i in range(2)]
                xT = sbuf(st4, "xT", [128, 8, BLK], BF16)
                aT = sbuf(st4, "aT", [128, 8, BLK], BF16)
                t1 = [sbuf(st4, "t1_%d" % i, [128, BLK]) for i in range(2)]
                t2 = [sbuf(st4, "t2_%d" % i, [128, BLK]) for i in range(2)]
                sgm = [sbuf(st4, "sgm%d" % i, [128, BLK]) for i in range(2)]
                yo = [sbuf(st4, "yo%d" % i, [128, D]) for i in range(2)]
                nblocks = min(NB, nch * 128 * 4 // BLK + 32)

                def load_w(b):
                    i = b % 2
                    cp("dve", wofc[i][:], woff[:, b:b + 1], [woff], [wofc[i]])
                    P.dma("pool", lambda e: e.indirect_dma_start(out=w1s[i][:].rearrange("p a n -> p (a n)"), out_offset=None, in_=w1_d,
                          in_offset=bass.IndirectOffsetOnAxis(ap=wofc[i][:, 0:1], axis=0), bounds_check=bc(e, 32 * 128 - 1), oob_is_err=False),
                          _bl([wofc[i]]), _bl([w1s[i]]), key="w1_%d" % i)
                    P.dma("pool", lambda e: e.indirect_dma_start(out=w2s[i][:].rearrange("p a n -> p (a n)"), out_offset=None, in_=w2_d,
                          in_offset=bass.IndirectOffsetOnAxis(ap=wofc[i][:, 0:1], axis=0), bounds_check=bc(e, 32 * 128 - 1), oob_is_err=False),
                          _bl([wofc[i]]), _bl([w2s[i]]), key="w2_%d" % i)
                    P.dma("pool", lambda e: e.indirect_dma_start(out=b1s[i][:, :], out_offset=None, in_=b1_d,
                          in_offset=bass.IndirectOffsetOnAxis(ap=wofc[i][:, 0:1], axis=0), bounds_check=bc(e, 32 * 128 - 1), oob_is_err=False),
                          _bl([wofc[i]]), _bl([b1s[i]]), key="b1_%d" % i)

                def load_x(b):
                    i = b % 2
                    dma("sp", xg[i][:], xs_d[b * BLK:(b + 1) * BLK, :].rearrange("(j p) d -> p j d", p=128), [], [xg[i]], "xg%d" % i)

                load_w(0); load_x(0)
                for b in range(nblocks):
                    i = b % 2
                    if b + 1 < nblocks:
                        load_w(b + 1); load_x(b + 1)
                    for kc in range(8):
                        ti, tk_ = bank()
                        for j in range(JB):
                            P.op("pe", lambda e, kc=kc, j=j, ti=ti, i=i: e.transpose(pvb(ti)[:, j * 128:(j + 1) * 128], xg[i][:, j, kc * 128:(kc + 1) * 128], ident),
                                 _bl([xg[i], cst]), _bl([tk_]), inc=(j == JB - 1))
                        cp("act", xT[:, kc, :], pvb(ti)[:, 0:BLK], [tk_], [xT])
                    ts("dve", b1l1[i][:], b1s[i][:, 8:16], 1.0, None, ALU.add, None, [b1s[i]], [b1l1[i]])
                    for fc in range(8):
                        gi, gk = bank(); li, lk = bank()
                        for kc in range(8):
                            mm(pv(gi)[:, 0:BLK], w1s[i][:, kc, fc * 128:(fc + 1) * 128], xT[:, kc, :], [w1s[i], xT], [gk], start=(kc == 0), stop=(kc == 7), inc=(kc == 7))
                        for kc in range(8):
                            mm(pv(li)[:, 0:BLK], w1s[i][:, kc, 1024 + fc * 128:1024 + (fc + 1) * 128], xT[:, kc, :], [w1s[i], xT], [lk], start=(kc == 0), stop=(kc == 7), inc=(kc == 7))
                        a1 = t1[fc % 2]; a2 = t2[fc % 2]; sg_ = sgm[fc % 2]
                        ts("dve", a1[:], pv(gi)[:, 0:BLK], b1s[i][:, fc:fc + 1], 7.0, ALU.add, ALU.min, [gk, b1s[i]], [a1])
                        act(sg_[:], a1[:], AF.Sigmoid, [a1], [sg_], scale=1.702)
                        ts("dve", a2[:], pv(li)[:, 0:BLK], b1l1[i][:, fc:fc + 1], 8.0, ALU.add, ALU.min, [lk, b1l1[i]], [a2])
                        stt("dve", a2[:], a2[:], -6.0, a1[:], ALU.max, ALU.mult, [a2, a1], [a2])
                        tt("dve", aT[:, fc, :], a2[:], sg_[:], ALU.mult, [a2, sg_], [aT])
                    for j in range(JB):
                        oi, ok = bank2()
                        for nh in range(2):
                            for kc in range(8):
                                mm(pv(oi, 2)[:, nh * 512:(nh + 1) * 512], aT[:, kc, j * 128:(j + 1) * 128], w2s[i][:, kc, nh * 512:(nh + 1) * 512],
                                   [aT, w2s[i]], ok, start=(kc == 0), stop=(kc == 7), inc=(kc == 7 and nh == 1))
                        yb = yo[j % 2]
                        tt("dve", yb[:], pv(oi, 2), b1s[i][:, 16:1040], ALU.add, ok + [b1s[i]], [yb])
                        dma("sp", ys_d[b * BLK + j * 128:b * BLK + (j + 1) * 128, :], yb[:], [yb], [], "yo%d" % (j % 2))
                P.barrier()

            with ExitStack() as st5:
                yg = [[sbuf(st5, "yg%d_%d" % (i, k), [128, D]) for k in range(4)] for i in range(3)]
                xr = [sbuf(st5, "xr%d" % i, [128, D]) for i in range(3)]
                acc = [sbuf(st5, "acc%d" % i, [128, D]) for i in range(3)]
                nfin = nch
                for n in range(nfin):
                    i = n % 3
                    dma("sp", xr[i][:], out_d[n * 128:(n + 1) * 128, :], [], [xr[i]], "xr%d" % i)
                    dc = dcur[n % 2]
                    cp("dve", dc[:], dest[:, n, :], [dest], [dc])
                    for k in range(4):
                        P.dma("pool", lambda e, k=k, i=i, dc=dc: e.indirect_dma_start(out=yg[i][k][:, :], out_offset=None, in_=ys_d,
                              in_offset=bass.IndirectOffsetOnAxis(ap=dc[:, k:k + 1], axis=0), bounds_check=bc(e, NSLOT - 1), oob_is_err=False),
                              _bl([dc]), _bl([yg[i][k]]), key="yg%d_%d" % (i, k))
                    a_ = acc[i]
                    ts("dve", a_[:], yg[i][0][:], gate_all[:, n, 0:1], None, ALU.mult, None, [yg[i][0], gate_all], [a_])
                    for k in range(1, 4):
                        stt("dve", a_[:], yg[i][k][:], gate_all[:, n, k:k + 1], a_[:], ALU.mult, ALU.add, [yg[i][k], gate_all, a_], [a_])
                    tt("dve", a_[:], a_[:], gt2_b[:], ALU.mult, [a_, gt2_b], [a_])
                    tt("dve", a_[:], a_[:], xr[i][:], ALU.add, [a_, xr[i]], [a_])
                    dma("sp", out_d[n * 128:(n + 1) * 128, :], a_[:], [a_], [], "fo%d" % i)
        for k, v in P.dsem.items():
            P._wait("sp", (k, v[1]))
        P.emit()
    return nc, dbg_out, P


def _consts():
    p = np.arange(128)[:, None]; j = np.arange(128)[None, :]
    c = np.zeros((128, 14, 128), np.float32)
    c[:, 0] = (p == j); c[:, 1] = (p < j); c[:, 2] = (p <= j); c[:, 3] = (p > j)
    c[:, 4] = ((p // 64) == (j // 64)); c[:, 5] = 1.0
    c[:, 6] = (p > j) & ((p // 16) == (j // 16)); c[:, 7] = (p < j) & ((p // 16) == (j // 16))
    for ii, bsz in enumerate((32, 64, 128)):
        hb = bsz // 2
        c[:, 8 + 2 * ii] = ((p // bsz) == (j // bsz)) & ((p // hb) > (j // hb))
        c[:, 9 + 2 * ii] = ((p // bsz) == (j // bsz)) & ((p // hb) < (j // hb))
    c2 = np.zeros((128, MB + 3), np.float32)
    c2[:, 0:MB] = np.arange(MB)[None, :] * BLK
    c2[:, MB] = (np.arange(128) < 64); c2[:, MB + 1] = (np.arange(128) >= 64)
    c2[:, MB + 2] = np.arange(128)
    bst = np.ascontiguousarray(np.broadcast_to(np.repeat(np.arange(NB) * BLK, 32)[None, :], (128, NB * 32))).astype(np.float32)
    return c, c2, bst


def _prep_shared(inp):
    f = lambda a: np.ascontiguousarray(np.asarray(a, dtype=np.float32))
    L = 0
    fm = lambda v, k: f(np.asarray(v).reshape(k, 128).T)
    qperm = np.concatenate([np.concatenate([np.arange(64) + 64 * jj, np.arange(64) + 64 * (4 + jj)]) for jj in range(4)])
    w_in = np.asarray(inp["w_in"][L])
    cols = np.concatenate([np.arange(1792), 1792 + qperm, np.arange(2304, 2560)])
    w_out = np.asarray(inp["w_out"][L])
    rows = np.concatenate([np.arange(512), 512 + qperm])
    c, c2, bst = _consts()
    sh = {
        "w_ada": f(inp["w_ada"][L]), "b_ada_fm": fm(inp["b_ada"][L], 48), "b_ada_row": f(inp["b_ada"][L]).reshape(1, -1),
        "g1_fm": fm(inp["norm1_g"][L], 8), "g2_row": f(inp["norm2_g"][L]).reshape(1, -1),
        "w_in": f(w_in[:, cols]), "mu_fm": fm(inp["mu_shift"][L], 14),
        "chp": f(np.stack([np.asarray(inp[k][L]).reshape(-1).reshape(4, 128).T for k in ("w0", "a0", "k_k", "k_a", "r_k")], axis=1)),
        "lnx_row": f(np.concatenate([inp["lnx_g"][L], inp["lnx_b"][L]])).reshape(1, -1),
        "lora_up": f(np.concatenate([inp["w_up"][L], inp["a_up"][L]], axis=0)), "g_up": f(inp["g_up"][L]),
        "qkg": f(np.stack([np.tile(inp["q_norm_g"][L], 2), np.tile(inp["k_norm_g"][L], 2)], axis=1)),
        "sink_fm": f(np.concatenate([np.tile(np.asarray(inp["sinks"][L])[0:4][None, :], (64, 1)),
                                     np.tile(np.asarray(inp["sinks"][L])[4:8][None, :], (64, 1))], axis=0)),
        "w_out": f(w_out[rows, :]), "w_router": f(inp["w_router"][L]), "b_router": f(inp["b_router"][L]).reshape(1, -1),
        "w1p": f(np.asarray(inp["w1"][L]).reshape(32, 8, 128, 2048).transpose(0, 2, 1, 3).reshape(32 * 128, 8 * 2048)),
        "w2p": f(np.asarray(inp["w2"][L]).reshape(32, 8, 128, 1024).transpose(0, 2, 1, 3).reshape(32 * 128, 8 * 1024)),
        "b12p": f(np.concatenate([np.asarray(inp["b1"][L]).reshape(32, 16, 128).transpose(0, 2, 1).reshape(32 * 128, 16),
                                  np.repeat(np.asarray(inp["b2"][L]), 128, axis=0)], axis=1)),
        "consts": c, "consts2": c2, "bstart": bst, "zeros": np.zeros((1024, D), dtype=ml_dtypes.bfloat16),
    }
    return sh


def kernel(**inputs):
    sh = _prep_shared(inputs)
    x = np.asarray(inputs["x"], dtype=np.float32)
    cc = np.asarray(inputs["c"], dtype=np.float32)
    nc, _, _ = build_program()
    in_maps = []
    for b in range(8):
        m = dict(sh)
        m["x"] = np.ascontiguousarray(x[b])
        m["c_fm"] = np.ascontiguousarray(cc[b].reshape(8, 128).T)
        in_maps.append(m)
    res = run_bass_kernel_spmd(nc, in_maps, core_ids=list(range(8)))
    return np.stack([np.asarray(res.results[b]["out"], dtype=np.float32) for b in range(8)], axis=0)
```

```python
import os
import threading
from contextlib import ExitStack
import numpy as np
import ml_dtypes
import concourse.bass as bass
import concourse.mybir as mybir
from concourse.bass_utils import run_bass_kernel_spmd

F32 = mybir.dt.float32
BF16 = mybir.dt.bfloat16
I32 = mybir.dt.int32
ALU = mybir.AluOpType
AF = mybir.ActivationFunctionType
AX = mybir.AxisListType

T = 4096
D = 1024
NCH = T // 128
BLK = int(os.environ.get("KBLK", "384"))
JB = BLK // 128
MB = -(-T // BLK)
NB = T * 4 // BLK + 32
NSLOT = NB * BLK
DECAY_K = 0.6065306597126334
NORM_EPS = 1e-6
GN_EPS = 64e-5


KLIMIT = int(os.environ.get("KLIMIT", "100000000"))
NOPOOL = not os.environ.get("KPOOL")


class Buf:
    __slots__ = ("name", "w", "r", "excl")

    def __init__(self, name):
        self.name = name
        self.w = None
        self.r = []
        self.excl = False


class Prog:
    ENGS = ("pe", "act", "dve", "pool", "sp")

    def __init__(self, nc, stack):
        self.nc = nc
        self.stack = stack
        self.sem = {e: stack.enter_context(nc.semaphore("sem_" + e)) for e in self.ENGS}
        self.cnt = {e: 0 for e in self.ENGS}
        self.streams = {e: [] for e in self.ENGS}
        self.waited = {}
        self.dsem = {}
        self.ninstr = 0
        self.hook = None
        self.hold = False

    def _wait(self, eng, ev):
        if ev is None:
            return
        key, val = ev
        if key == "pe" and eng == "pe":
            return
        k = (eng, key)
        if self.waited.get(k, 0) >= val:
            return
        self.waited[k] = val
        sem = self.sem[key] if key in self.sem else self.dsem[key][0]
        self.streams[eng].append(lambda e, sem=sem, val=val: e.wait_ge(sem, val))
        self.ninstr += 1

    def _deps(self, eng, reads, writes):
        mx = {}
        for b in reads:
            if b.w is not None:
                mx[b.w[0]] = max(mx.get(b.w[0], 0), b.w[1])
            if b.excl:
                for k, v in b.r:
                    if k != eng:
                        mx[k] = max(mx.get(k, 0), v)
        for b in writes:
            if b.w is not None:
                mx[b.w[0]] = max(mx.get(b.w[0], 0), b.w[1])
            for k, v in b.r:
                mx[k] = max(mx.get(k, 0), v)
        for k, v in mx.items():
            self._wait(eng, (k, v))

    def _commit(self, ev, reads, writes):
        for b in writes:
            b.w = ev
            b.r = []
        for b in reads:
            if b not in writes:
                b.r.append(ev)
                if len(b.r) > 48:
                    mx = {}
                    for k, v in b.r:
                        mx[k] = max(mx.get(k, 0), v)
                    b.r = list(mx.items())

    def op(self, eng, fn, reads=(), writes=(), inc=True):
        if self.ninstr > KLIMIT:
            return None
        if self.hook is not None and not self.hold:
            self.hook()
        self.hold = not inc
        if eng == "pool" and NOPOOL:
            eng = "dve"
        self._deps(eng, reads, writes)
        if os.environ.get("KLIST"):
            import sys
            f = sys._getframe(1)
            while f.f_code.co_name in ("tt", "ts", "stt", "cp", "act", "mm", "red", "dma", "dbg", "op", "<lambda>"):
                f = f.f_back
            print("INSTR", self.ninstr, eng, "line", f.f_lineno)
        if inc:
            self.cnt[eng] += 1
            ev = (eng, self.cnt[eng])
            sem = self.sem[eng]
            self.streams[eng].append(lambda e, fn=fn, sem=sem: fn(e).then_inc(sem, 1))
        else:
            ev = (eng, self.cnt[eng] + 1)
            self.streams[eng].append(lambda e, fn=fn: fn(e))
        self.ninstr += 1
        self._commit(ev, reads, writes)
        return ev

    def dma(self, eng, fn, reads=(), writes=(), key="d"):
        if self.ninstr > KLIMIT:
            return None
        if self.hook is not None and not self.hold:
            self.hook()
        if key not in self.dsem:
            self.dsem[key] = [self.stack.enter_context(self.nc.semaphore("dsem_" + key)), 0]
        self._deps(eng, reads, writes)
        self.dsem[key][1] += 16
        ev = (key, self.dsem[key][1])
        sem = self.dsem[key][0]
        self.streams[eng].append(lambda e, fn=fn, sem=sem: fn(e).then_inc(sem, 16))
        self.ninstr += 1
        self._commit(ev, reads, writes)
        return ev

    def barrier(self, exclude=()):
        evs = [(e, self.cnt[e]) for e in self.ENGS if self.cnt[e] > 0]
        evs += [(k, v[1]) for k, v in self.dsem.items() if v[1] > 0 and k not in exclude]
        for e in self.ENGS:
            for ev in evs:
                self._wait(e, ev)

    def emit(self):
        with self.nc.Block() as blk:
            @blk.tensor
            def _(e):
                for f in self.streams["pe"]:
                    f(e)

            @blk.scalar
            def _(e):
                for f in self.streams["act"]:
                    f(e)

            @blk.vector
            def _(e):
                for f in self.streams["dve"]:
                    f(e)

            @blk.gpsimd
            def _(e):
                for f in self.streams["pool"]:
                    f(e)

            @blk.sync
            def _(e):
                for f in self.streams["sp"]:
                    f(e)


def interleave(P, fa, fb):
    cv = threading.Condition()
    st = {"turn": 0, "alive": [True, True], "exc": None}

    def hook():
        me = int(threading.current_thread().name[-1])
        with cv:
            if st["alive"][1 - me]:
                st["turn"] = 1 - me
                cv.notify_all()
                while st["turn"] != me:
                    cv.wait()

    def run(i, f):
        with cv:
            while st["turn"] != i:
                cv.wait()
        try:
            f()
        except BaseException as ex:
            st["exc"] = ex
        finally:
            with cv:
                st["alive"][i] = False
                st["turn"] = 1 - i
                cv.notify_all()

    P.hook = hook
    P.hold = False
    ts_ = [threading.Thread(target=run, args=(i, f), name="il%d" % i) for i, f in enumerate((fa, fb))]
    for t in ts_:
        t.start()
    for t in ts_:
        t.join()
    P.hook = None
    if st["exc"] is not None:
        raise st["exc"]


class Tl:
    def __init__(self, t, name):
        self.t = t
        self.b = Buf(name)

    def __getitem__(self, k):
        return self.t[k]


def _bl(xs):
    return [x.b if isinstance(x, Tl) else x for x in xs]


def build_program(dbg_chunk=None, nch=NCH):
    nc = bass.Bass("TRN2", target_bir_lowering=False)
    dram_in = {}

    def DI(name, shape, dt=F32):
        dram_in[name] = nc.dram_tensor(name, list(shape), dt, kind="ExternalInput").ap()
        return dram_in[name]

    x_d = DI("x", [T, D]); cfm_d = DI("c_fm", [128, 8])
    wada_d = DI("w_ada", [D, 6 * D]); bada_fm_d = DI("b_ada_fm", [128, 48]); bada_row_d = DI("b_ada_row", [1, 6 * D])
    g1_d = DI("g1_fm", [128, 8]); g2_d = DI("g2_row", [1, D])
    win_d = DI("w_in", [D, 2560]); mu_d = DI("mu_fm", [128, 14])
    ch_d = DI("chp", [128, 5, 4])
    lnx_d = DI("lnx_row", [1, 1024])
    lora_d = DI("lora_up", [128, 512]); gup_d = DI("g_up", [128, 512])
    qk_d = DI("qkg", [128, 2]); sink_d = DI("sink_fm", [128, 4])
    wout_d = DI("w_out", [D, D]); wr_d = DI("w_router", [D, 32]); br_d = DI("b_router", [1, 32])
    nexp = 32 if dbg_chunk is None else 1
    w1_d = DI("w1p", [nexp * 128, 8 * 2048]); w2_d = DI("w2p", [nexp * 128, 8 * 1024])
    b1_d = DI("b12p", [32 * 128, 16 + 1024])
    cst_d = DI("consts", [128, 14, 128]); cst2_d = DI("consts2", [128, MB + 3]); bst_d = DI("bstart", [128, NB * 32]); zer_d = DI("zeros", [1024, D], BF16)
    out_d = nc.dram_tensor("out", [T, D], F32, kind="ExternalOutput").ap()
    hbuf_d = nc.dram_tensor("hbuf", [T, D], BF16).ap()
    nsl = NSLOT if dbg_chunk is None else 128
    xs_d = nc.dram_tensor("xs", [nsl, D], BF16).ap()
    ys_d = nc.dram_tensor("ys", [nsl, D], F32).ap()
    dbg_out = {}

    with ExitStack() as st0:
        P = Prog(nc, st0)

        def sbuf(st, name, shape, dt=F32):
            return Tl(st.enter_context(nc.sbuf_tensor("s_" + name, list(shape), dt)), name)

        def E(eng):
            return eng

        def tt(eng, out, in0, in1, op, R, W):
            return P.op(eng, lambda e: e.tensor_tensor(out=out, in0=in0, in1=in1, op=op), _bl(R), _bl(W))

        def ts(eng, out, in0, s1, s2, op0, op1, R, W):
            if s2 is None:
                return P.op(eng, lambda e: e.tensor_scalar(out=out, in0=in0, scalar1=s1, scalar2=None, op0=op0), _bl(R), _bl(W))
            return P.op(eng, lambda e: e.tensor_scalar(out=out, in0=in0, scalar1=s1, scalar2=s2, op0=op0, op1=op1), _bl(R), _bl(W))

        def stt(eng, out, in0, sc, in1, op0, op1, R, W):
            return P.op(eng, lambda e: e.scalar_tensor_tensor(out=out, in0=in0, scalar=sc, in1=in1, op0=op0, op1=op1), _bl(R), _bl(W))

        def cp(eng, out, in_, R, W):
            if eng == "act":
                return P.op(eng, lambda e: e.copy(out=out, in_=in_), _bl(R), _bl(W))
            return P.op(eng, lambda e: e.tensor_copy(out=out, in_=in_), _bl(R), _bl(W))

        def act(out, in_, func, R, W, bias=None, scale=None, accum=None):
            kw = {}
            if bias is not None:
                kw["bias"] = bias
            if scale is not None:
                kw["scale"] = scale
            if accum is not None:
                kw["accum_out"] = accum
            return P.op("act", lambda e: e.activation(out=out, in_=in_, func=func, **kw), _bl(R), _bl(W))

        def mm(out, lhsT, rhs, R, W, start=True, stop=True, inc=True):
            return P.op("pe", lambda e: e.matmul(out, lhsT=lhsT, rhs=rhs, start=start, stop=stop), _bl(R), _bl(W), inc=inc)

        def red(eng, out, in_, R, W, op=ALU.add):
            return P.op(eng, lambda e: e.tensor_reduce(out=out, in_=in_, axis=AX.X, op=op), _bl(R), _bl(W))

        def dma(eng, out, in_, R, W, key):
            return P.dma(eng, lambda e: e.dma_start(out=out, in_=in_), _bl(R), _bl(W), key=key)

        regcache = {}

        def bc(e, v):
            if v not in regcache:
                regcache[v] = e.to_reg(v)
            return regcache[v]

        def mark(label):
            if os.environ.get("KMARK"):
                print("MARK", label, P.ninstr)

        def dbg(name, tile_ap, shape, dt, R):
            if dbg_chunk is None:
                return
            o = nc.dram_tensor("dbg_" + name, list(shape), dt, kind="ExternalOutput").ap()
            dbg_out[name] = o
            dma("sp", o, tile_ap, R, [], "dbg_" + name)

        psum_t = st0.enter_context(nc.psum_tensor("psum", [128, 8, 512], F32))
        banks = [Tl(psum_t, "bank%d" % i) for i in range(8)]
        for bk_ in banks:
            bk_.b.excl = True
        rr = [0, 0, 0]

        def _pool():
            if P.hook is None:
                return 0, 0, 8
            t = int(threading.current_thread().name[-1])
            return 1 + t, 4 * t, 4

        def bank():
            k, base, n = _pool()
            i = base + rr[k] % n
            rr[k] += 1
            return i, banks[i]

        def bank2():
            k, base, n = _pool()
            while rr[k] % 2:
                rr[k] += 1
            i = base + rr[k] % n
            rr[k] += 2
            return i, [banks[i], banks[i + 1]]

        def pv(i, n=1):
            return psum_t[:, i:i + n, :].rearrange("p a n -> p (a n)")

        def pvb(i):
            return psum_t[:, i, :].bitcast(BF16)

        cst = sbuf(st0, "cst", [128, 14, 128], BF16)
        cstf = sbuf(st0, "cstf", [128, 2, 128], F32)
        cst2 = sbuf(st0, "cst2", [128, MB + 3], F32)
        dma("pool", cst[:], cst_d, [], [cst], "c0")
        dma("sp", cstf[:, 0, :], cst_d[:, 0, :], [], [cstf], "c1")
        dma("sp", cstf[:, 1, :], cst_d[:, 5, :], [], [cstf], "c1")
        dma("sp", cst2[:], cst2_d, [], [cst2], "c2")
        ident = cst[:, 0, :]; tri_lt = cst[:, 1, :]; tri_le = cst[:, 2, :]; tri_gt = cst[:, 3, :]
        bdiag = cst[:, 4, :]; ones_bf = cst[:, 5, :]
        ident_f = cstf[:, 0, :]; ones_f = cstf[:, 1, :]
        thr8 = cst2[:, 0:MB]
        bsel_f = cst2[:, MB:MB + 2]
        iota_p = cst2[:, MB + 2:MB + 3]
        bsel = sbuf(st0, "bsel", [128, 2], BF16)
        cp("dve", bsel[:], bsel_f, [cst2], [bsel])

        lg_all = sbuf(st0, "lg_all", [128, NCH, 32])
        top_all = sbuf(st0, "top_all", [128, NCH, 8])
        gate_all = sbuf(st0, "gate_all", [128, NCH, 4])
        pos_all = sbuf(st0, "pos_all", [128, NCH, 32])
        carry = sbuf(st0, "carry", [128, 32])
        gt2_b = sbuf(st0, "gt2_b", [128, D])
        woff = sbuf(st0, "woff", [128, NB], I32)
        dest = sbuf(st0, "dest", [128, NCH, 4], I32)
        wofc = [sbuf(st0, "wofc%d" % i, [128, 1], I32) for i in range(2)]
        dcur = [sbuf(st0, "dcur%d" % i, [128, 4], I32) for i in range(2)]
        P.op("dve", lambda e: e.memset(carry[:], 0.0), [], _bl([carry]))

        with ExitStack() as st1:
            modfm = sbuf(st1, "modfm", [128, 32])
            s1 = sbuf(st1, "s1", [128, 8])
            s2_b = sbuf(st1, "s2_b", [128, D]); sh2_b = sbuf(st1, "sh2_b", [128, D])
            lnx_b = sbuf(st1, "lnx_b", [128, 1024])
            chp = sbuf(st1, "chp", [128, 5, 4]); mu = sbuf(st1, "mu", [128, 14]); g1 = sbuf(st1, "g1", [128, 8])
            qkg = sbuf(st1, "qkg", [128, 2]); esink = sbuf(st1, "esink", [128, 4])
            brt = sbuf(st1, "brt", [128, 32])
            omka = sbuf(st1, "omka", [128, 4])
            w_in = sbuf(st1, "w_in", [128, 8, 2560], BF16)
            w_out = sbuf(st1, "w_out", [128, 8, D], BF16)
            lora_up = sbuf(st1, "lora_up", [128, 512], BF16); g_up = sbuf(st1, "g_up", [128, 512], BF16)
            wr = sbuf(st1, "wr", [128, 8, 32], BF16)

            dma("sp", chp[:], ch_d, [], [chp], "p0"); dma("sp", mu[:], mu_d, [], [mu], "p1")
            dma("sp", g1[:], g1_d, [], [g1], "p2"); dma("sp", qkg[:], qk_d, [], [qkg], "p3")
            dma("sp", esink[:], sink_d, [], [esink], "p4")
            dma("sp", brt[:], br_d.partition_broadcast(128), [], [brt], "p5")
            dma("sp", lnx_b[:], lnx_d.partition_broadcast(128), [], [lnx_b], "p6")
            dma("pool", lora_up[:], lora_d, [], [lora_up], "p7"); dma("pool", g_up[:], gup_d, [], [g_up], "p8")
            dma("pool", wr[:], wr_d.rearrange("(k p) e -> p k e", p=128), [], [wr], "p9")
            for kc in range(8):
                dma("pool", w_in[:, kc, :], win_d[kc * 128:(kc + 1) * 128, :], [], [w_in], "wi")
                dma("pool", w_out[:, kc, :], wout_d[kc * 128:(kc + 1) * 128, :], [], [w_out], "wo")
            act(esink[:], esink[:], AF.Exp, [esink], [esink])
            ts("dve", omka[:], chp[:, 3, :], -1.0, 1.0, ALU.mult, ALU.add, [chp], [omka])
            ts("dve", qkg[:, 0:1], qkg[:, 0:1], 0.125, None, ALU.mult, None, [qkg], [qkg])

            with ExitStack() as st2:
                cfm = sbuf(st2, "cfm", [128, 8]); sc32 = sbuf(st2, "sc32", [128, 8])
                screp = sbuf(st2, "screp", [128, 8, 128])
                badafm = sbuf(st2, "badafm", [128, 48])
                wblk = [sbuf(st2, "wblk%d" % i, [128, 8, 512]) for i in range(2)]
                brow = [sbuf(st2, "brow%d" % i, [128, 512]) for i in range(2)]
                g2b = sbuf(st2, "g2b", [128, D]); gt1_b = sbuf(st2, "gt1_b", [128, D])
                dma("sp", cfm[:], cfm_d, [], [cfm], "a0"); dma("sp", badafm[:], bada_fm_d, [], [badafm], "a1")
                dma("sp", g2b[:], g2_d.partition_broadcast(128), [], [g2b], "a2")
                act(sc32[:], cfm[:], AF.Silu, [cfm], [sc32])
                for kc in range(8):
                    cp("dve", screp[:, kc, :], sc32[:, kc:kc + 1].to_broadcast([128, 128]), [sc32], [screp])
                wv = wada_d.rearrange("(k p) n -> p k n", p=128)
                bdst = {4: gt1_b[:, 0:512], 5: gt1_b[:, 512:1024], 6: sh2_b[:, 0:512], 7: sh2_b[:, 512:1024],
                        8: s2_b[:, 0:512], 9: s2_b[:, 512:1024], 10: gt2_b[:, 0:512], 11: gt2_b[:, 512:1024]}
                btl = {4: gt1_b, 5: gt1_b, 6: sh2_b, 7: sh2_b, 8: s2_b, 9: s2_b, 10: gt2_b, 11: gt2_b}
                bi_fm, bk_fm = bank()
                for blk in range(12):
                    wb = wblk[blk % 2]
                    dma("sp", wb[:], wv[:, :, blk * 512:(blk + 1) * 512], [], [wb], "wa%d" % (blk % 2))
                    if blk < 4:
                        for j in range(4):
                            col = blk * 4 + j
                            for kc in range(8):
                                mm(pv(bi_fm)[:, col:col + 1], wb[:, kc, j * 128:(j + 1) * 128], sc32[:, kc:kc + 1],
                                   [wb, sc32], [bk_fm], start=(kc == 0), stop=(kc == 7), inc=(kc == 7))
                    else:
                        br_ = brow[blk % 2]
                        dma("sp", br_[:], bada_row_d[:, blk * 512:(blk + 1) * 512].partition_broadcast(128), [], [br_], "wr%d" % (blk % 2))
                        bi, bk = bank()
                        for kc in range(8):
                            mm(pv(bi), screp[:, kc, :], wb[:, kc, :], [wb, screp], [bk], start=(kc == 0), stop=(kc == 7), inc=(kc == 7))
                        tt("dve", bdst[blk], pv(bi), br_[:], ALU.add, [bk, br_], [btl[blk]])
                    if blk == 3:
                        tt("dve", modfm[:, 0:16], pv(bi_fm)[:, 0:16], badafm[:, 0:16], ALU.add, [bk_fm, badafm], [modfm])
                stt("dve", s1[:], modfm[:, 8:16], 1.0, g1[:], ALU.add, ALU.mult, [modfm, g1], [s1])
                stt("dve", s2_b[:], s2_b[:], 1.0, g2b[:], ALU.add, ALU.mult, [s2_b, g2b], [s2_b])
                for kc in range(8):
                    tt("dve", w_out[:, kc, :], w_out[:, kc, :], gt1_b[:], ALU.mult, [w_out, gt1_b], [w_out])
                P.barrier()
            if dbg_chunk is None:
                for q in range(0, NSLOT, 1024):
                    r_ = min(1024, NSLOT - q)
                    dma("act", xs_d[q:q + r_, :], zer_d[0:r_, :], [], [], "zf")
            sh1 = modfm[:, 0:8]

            xt = [sbuf(st1, "xt%d" % i, [128, D]) for i in range(2)]
            xn = sbuf(st1, "xn", [128, D], BF16)
            sm = sbuf(st1, "sm", [128, 16]); smB = sbuf(st1, "smB", [128, 16])
            hT = sbuf(st1, "hT", [128, 8, 128], BF16)
            pbuf = sbuf(st1, "pbuf", [128, 14, 129])
            psh = sbuf(st1, "psh", [128, 14, 128])
            lo_bf = sbuf(st1, "lo_bf", [128, 128], BF16); sg_bf = sbuf(st1, "sg_bf", [128, 128], BF16)
            sw = sbuf(st1, "sw", [128, 4, 128]); aa = sbuf(st1, "aa", [128, 4, 128])
            cs = sbuf(st1, "cs", [128, 4, 128])
            csC = sbuf(st1, "csC", [128, 4])
            gC = [sbuf(st1, "gC%d" % i, [128, 4]) for i in range(2)]
            kk = sbuf(st1, "kk", [128, 4, 128])
            k2 = sbuf(st1, "k2", [128, 4, 128]); beta = sbuf(st1, "beta", [128, 4, 128])
            e1 = sbuf(st1, "e1", [128, 4, 128]); e2 = sbuf(st1, "e2", [128, 4, 128])
            abp = sbuf(st1, "abp", [128, 4, 2, 128], BF16)
            kbar = sbuf(st1, "kbar", [128, 4, 128], BF16); bbar = sbuf(st1, "bbar", [128, 4, 128], BF16)
            fm3 = sbuf(st1, "fm3", [128, 2, 4, 128], BF16)
            wrk = sbuf(st1, "wrk", [128, 4, 128], BF16)
            tm3 = sbuf(st1, "tm3", [128, 3, 512], BF16)
            v32 = sbuf(st1, "v32", [128, 512]); vtm = sbuf(st1, "vtm", [128, 512], BF16)
            gtm = sbuf(st1, "gtm", [128, 512])
            atb = sbuf(st1, "atb", [128, 8, 256], BF16); atk = sbuf(st1, "atk", [128, 8, 256], BF16)
            Lf = [sbuf(st1, "Lf%d" % g, [128, 4, 2, 128], BF16) for g in range(2)]
            Pq = [[sbuf(st1, "Pq%d_%d" % (g, i), [128, 4, 2, 128], BF16) for i in range(2)] for g in range(2)]
            Mq = [[sbuf(st1, "Mq%d_%d" % (g, i), [128, 4, 2, 128], BF16) for i in range(2)] for g in range(2)]
            Xb0 = sbuf(st1, "Xb0", [128, 8, 128], BF16); Xbf = sbuf(st1, "Xbf", [128, 8, 128], BF16)
            RpT = sbuf(st1, "RpT", [128, 4, 128], BF16)
            Y0 = sbuf(st1, "Y0", [128, 512]); TpT = sbuf(st1, "TpT", [128, 4, 64], BF16); D32 = sbuf(st1, "D32", [128, 4, 64])
            H32 = sbuf(st1, "H32", [128, 4, 64]); Hbf = [sbuf(st1, "Hbf%d" % i, [128, 4, 64], BF16) for i in range(2)]
            yy = sbuf(st1, "yy", [128, 8, 64]); ysq = sbuf(st1, "ysq", [128, 8, 64])
            gn = sbuf(st1, "gn", [128, 6, 8]); bon = sbuf(st1, "bon", [128, 8])
            obf = sbuf(st1, "obf", [128, 512], BF16)
            mixR = sbuf(st1, "mixR", [128, 4, 128], BF16); matt = [sbuf(st1, "matt%d" % i, [128, 4, 128], BF16) for i in range(2)]
            qsq = sbuf(st1, "qsq", [128, 5, 128], BF16); qrs = sbuf(st1, "qrs", [128, 5, 128])
            qhat = sbuf(st1, "qhat", [128, 4, 128], BF16)
            khat = [sbuf(st1, "khat%d" % i, [128, 128], BF16) for i in range(2)]
            vat = [sbuf(st1, "vat%d" % i, [128, 128], BF16) for i in range(2)]
            ptm = [sbuf(st1, "ptm%d" % i, [128, 4, 128], BF16) for i in range(4)]
            h2 = [sbuf(st1, "h2_0", [128, D], BF16)] * 2
            msk = sbuf(st1, "msk", [128, 32], BF16); ex4 = sbuf(st1, "ex4", [128, 4])

            P.op("dve", lambda e: e.memset(pbuf[:], 0.0), [], _bl([pbuf]))
            if os.environ.get("KMARK"):
                print("SBUF remaining after mixer alloc", nc.sbuf_bytes_remaining)
            P.op("dve", lambda e: e.memset(H32[:], 0.0), [], _bl([H32]))
            P.op("dve", lambda e: e.memset(Hbf[0][:], 0.0), [], _bl([Hbf[0]]))

            nchunks = nch if dbg_chunk is None else dbg_chunk + 1
            def Fe(n):
                dg = (n == dbg_chunk)
                xc = xt[n % 2]
                dma("sp", xc[:], x_d[n * 128:(n + 1) * 128, :], [], [xc], "x%d" % (n % 2))
                mark("norm1")
                act(xn[:], xc[:], AF.Square, [xc], [xn, sm], accum=sm[:, 0:1])
                ts("dve", sm[:, 1:2], sm[:, 0:1], 1.0 / D, NORM_EPS, ALU.mult, ALU.add, [sm], [sm])
                act(sm[:, 1:2], sm[:, 1:2], AF.Sqrt, [sm], [sm])
                P.op("dve", lambda e: e.reciprocal(out=sm[:, 2:3], in_=sm[:, 1:2]), _bl([sm]), _bl([sm]))
                act(xn[:], xc[:], AF.Copy, [xc, sm], [xn], scale=sm[:, 2:3])
                bi, bk = bank()
                for kc in range(8):
                    P.op("pe", lambda e, kc=kc, bi=bi: e.transpose(pvb(bi)[:, kc * 128:(kc + 1) * 128], xn[:, kc * 128:(kc + 1) * 128], ident),
                         _bl([xn, cst]), _bl([bk]), inc=(kc == 7))
                for kc in range(8):
                    act(hT[:, kc, :], pvb(bi)[:, kc * 128:(kc + 1) * 128], AF.Identity, [bk, s1, modfm], [hT],
                        bias=sh1[:, kc:kc + 1], scale=s1[:, kc:kc + 1])
                if dg:
                    dbg("hT", hT[:].rearrange("p a n -> p (a n)"), [128, 1024], BF16, [hT])
                mark("inproj")
                pb = []
                for grp in range(5):
                    bi, bk = bank()
                    pb.append((bi, bk))
                    for j in range(4):
                        fc = grp * 4 + j
                        if fc == 19:
                            for kc in range(8):
                                mm(pv(bi)[:, j * 128:(j + 1) * 128], hT[:, kc, :], w_in[:, kc, 2432:2560], [hT, w_in], [bk],
                                   start=(kc == 0), stop=(kc == 7), inc=(kc == 7))
                            continue
                        col = fc * 128
                        for kc in range(8):
                            mm(pv(bi)[:, j * 128:(j + 1) * 128], w_in[:, kc, col:col + 128], hT[:, kc, :], [hT, w_in], [bk],
                               start=(kc == 0), stop=(kc == 7), inc=(kc == 7))
                    if grp < 4:
                        nj = 4 if grp < 3 else 2
                        cp("act" if grp % 2 == 0 else "dve", pbuf[:, grp * 4:grp * 4 + nj, 1:129], pv(bi)[:, 0:nj * 128].rearrange("p (a n) -> p a n", a=nj), [bk], [pbuf])
                mark("tokshift")
                tt("dve", psh[:], pbuf[:, :, 0:128], pbuf[:, :, 1:129], ALU.subtract, [pbuf], [psh])
                tt("pool", psh[:], psh[:], mu[:].unsqueeze(2).to_broadcast([128, 14, 128]), ALU.mult, [psh, mu], [psh])
                tt("dve", psh[:], psh[:], pbuf[:, :, 1:129], ALU.add, [psh, pbuf], [psh])
                cp("pool", pbuf[:, :, 0:1], pbuf[:, :, 128:129], [pbuf], [pbuf])
                if dg:
                    dbg("psh", psh[:].rearrange("p a n -> p (a n)"), [128, 14 * 128], F32, [psh])
                rr_ = psh[:, 0:4, :]; kr_ = psh[:, 4:8, :]; vr_ = psh[:, 8:12, :]
                mark("qknorm")
                qbi, qbk = pb[3][0], pb[3][1]
                q2bi, q2bk = pb[4]
                qv = [pv(qbi)[:, 256:384], pv(qbi)[:, 384:512], pv(q2bi)[:, 0:128], pv(q2bi)[:, 128:256], pv(q2bi)[:, 256:384]]
                qb_ = [qbk, qbk, q2bk, q2bk, q2bk]
                for j in range(5):
                    act(qsq[:, j, :], qv[j], AF.Square, [qb_[j]], [qsq])
                bi, bk = bank(); bi2, bk2 = bank()
                for j in range(5):
                    bb_i, bb_k = (bi, bk) if j < 4 else (bi2, bk2)
                    mm(pv(bb_i)[:, (j % 4) * 128:(j % 4 + 1) * 128], bdiag, qsq[:, j, :], [qsq, cst], [bb_k], inc=(j >= 3))
                ts("dve", qrs[:, 0:4, :], pv(bi).rearrange("p (a n) -> p a n", a=4), 1.0 / 64, NORM_EPS, ALU.mult, ALU.add, [bk], [qrs])
                ts("dve", qrs[:, 4, :], pv(bi2)[:, 0:128], 1.0 / 64, NORM_EPS, ALU.mult, ALU.add, [bk2], [qrs])
                act(qrs[:], qrs[:], AF.Sqrt, [qrs], [qrs])
                P.op("dve", lambda e: e.reciprocal(out=qrs[:], in_=qrs[:]), _bl([qrs]), _bl([qrs]))
                for j in range(4):
                    stt("dve", qhat[:, j, :], qv[j], qkg[:, 0:1], qrs[:, j, :], ALU.mult, ALU.mult, [qb_[j], qkg, qrs], [qhat])
                kc_ = khat[n % 2]; kp_ = khat[(n + 1) % 2]
                vc_ = vat[n % 2]; vp_ = vat[(n + 1) % 2]
                stt("dve", kc_[:], qv[4], qkg[:, 1:2], qrs[:, 4, :], ALU.mult, ALU.mult, [q2bk, qkg, qrs], [kc_])
                cp("act", vc_[:], pv(q2bi)[:, 384:512], [q2bk], [vc_])
                if dg:
                    dbg("qhat", qhat[:].rearrange("p a n -> p (a n)"), [128, 512], BF16, [qhat])
                    dbg("khat", kc_[:], [128, 128], BF16, [kc_])
                kcur[0] = (kc_, kp_, vc_, vp_)
            def Fl(n):
                dg = (n == dbg_chunk)
                xc = xt[n % 2]
                kc_, kp_, vc_, vp_ = khat[n % 2], khat[(n + 1) % 2], vat[n % 2], vat[(n + 1) % 2]
                rr_ = psh[:, 0:4, :]; kr_ = psh[:, 4:8, :]; vr_ = psh[:, 8:12, :]
                mark("attn")
                kbs = ([(kp_, vp_, tri_gt)] if n > 0 else []) + [(kc_, vc_, tri_le)]
                pts = []
                for kv in range(2):
                    Pp = slice(64 * kv, 64 * kv + 64)
                    for ib, (kt_, vt_, mk_) in enumerate(kbs):
                        bi, bk = bank()
                        mm(pv(bi).rearrange("p (a n) -> p a n", a=4), kt_[Pp, :], qhat[Pp, :, :], [kt_, qhat], [bk])
                        pm_ = ptm[kv * 2 + ib]
                        act(pm_[:], pv(bi).rearrange("p (a n) -> p a n", a=4), AF.Exp, [bk], [pm_])
                        tt("pool", pm_[:], pm_[:], mk_.unsqueeze(1).to_broadcast([128, 4, 128]), ALU.mult, [pm_, cst], [pm_])
                        pts.append((kv, pm_, vt_))
                obi, obk = bank(); dbi, dbk = bank()
                for kv in range(2):
                    Pp = slice(64 * kv, 64 * kv + 64)
                    lst = [p_ for p_ in pts if p_[0] == kv]
                    for ii, (_, pm_, vt_) in enumerate(lst):
                        mm(pv(obi)[Pp, :], vt_[:, Pp], pm_[:].rearrange("p a n -> p (a n)"), [vt_, pm_], [obk],
                           start=(ii == 0), stop=(ii == len(lst) - 1), inc=False)
                    for ii, (_, pm_, vt_) in enumerate(lst):
                        mm(pv(dbi)[Pp, :], ones_bf[:, 0:64], pm_[:].rearrange("p a n -> p (a n)"), [pm_, cst], [dbk],
                           start=(ii == 0), stop=(ii == len(lst) - 1), inc=(kv == 1 and ii == len(lst) - 1))
                den = e1[:]
                tt("dve", den, pv(dbi).rearrange("p (a n) -> p a n", a=4), esink[:].unsqueeze(2).to_broadcast([128, 4, 128]), ALU.add, [dbk, esink], [e1])
                P.op("dve", lambda e: e.reciprocal(out=den, in_=den), _bl([e1]), _bl([e1]))
                tt("dve", matt[n % 2][:], pv(obi).rearrange("p (a n) -> p a n", a=4), den, ALU.mult, [obk, e1], [matt[n % 2]])

                mark("rwkvprep")
                act(lo_bf[0:64, :], psh[0:64, 12, :], AF.Tanh, [psh], [lo_bf])
                cp("pool", lo_bf[64:128, :], psh[64:128, 12, :], [psh], [lo_bf])
                act(sg_bf[:], psh[:, 13, :], AF.Sigmoid, [psh], [sg_bf])
                zbi, zbk = bank(); abi, abk = bank(); gbi, gbk = bank()
                for c in range(4):
                    mm(pv(zbi)[:, c * 128:(c + 1) * 128], lora_up[0:64, c * 128:(c + 1) * 128], lo_bf[0:64, :], [lora_up, lo_bf], [zbk], inc=(c == 3))
                for c in range(4):
                    mm(pv(abi)[:, c * 128:(c + 1) * 128], lora_up[64:128, c * 128:(c + 1) * 128], lo_bf[64:128, :], [lora_up, lo_bf], [abk], inc=(c == 3))
                mm(pv(gbi), sg_bf[:], g_up[:], [sg_bf, g_up], [gbk])
                for c in range(4):
                    act(sw[:, c, :], pv(zbi)[:, c * 128:(c + 1) * 128], AF.Sigmoid, [zbk, chp], [sw], bias=chp[:, 0, c:c + 1])
                    act(aa[:, c, :], pv(abi)[:, c * 128:(c + 1) * 128], AF.Sigmoid, [abk, chp], [aa], bias=chp[:, 1, c:c + 1])
                cp("act", gtm[:], pv(gbi), [gbk], [gtm])
                for c in range(4):
                    P.op("dve", lambda e, c=c: e.tensor_tensor_scan(out=cs[:, c, :], data0=ones_f, data1=sw[:, c, :], initial=0.0,
                                                                     op0=ALU.mult, op1=ALU.add), _bl([sw, cstf]), _bl([cs]))
                ts("dve", csC[:], cs[:, :, 127], -DECAY_K, None, ALU.mult, None, [cs], [csC])
                gCn = gC[n % 2]
                act(gCn[:], csC[:], AF.Exp, [csC], [gCn])
                tt("dve", kk[:], kr_, chp[:, 2, :].unsqueeze(2).to_broadcast([128, 4, 128]), ALU.mult, [psh, chp], [kk])
                tt("pool", wrk[:], kk[:], kk[:], ALU.mult, [kk], [wrk])
                sbi, sbk = bank()
                for c in range(4):
                    mm(pv(sbi)[:, c * 128:(c + 1) * 128], bdiag, wrk[:, c, :], [wrk, cst], [sbk], inc=(c == 3))
                act(e2[:], pv(sbi).rearrange("p (a n) -> p a n", a=4), AF.Sqrt, [sbk], [e2])
                ts("dve", e2[:], e2[:], 1e-12, None, ALU.max, None, [e2], [e2])
                P.op("dve", lambda e: e.reciprocal(out=e2[:], in_=e2[:]), _bl([e2]), _bl([e2]))
                tt("dve", kk[:], kk[:], e2[:], ALU.mult, [kk, e2], [kk])
                tt("pool", k2[:], aa[:], chp[:, 3, :].unsqueeze(2).to_broadcast([128, 4, 128]), ALU.mult, [aa, chp], [k2])
                tt("pool", k2[:], k2[:], omka[:].unsqueeze(2).to_broadcast([128, 4, 128]), ALU.add, [k2, omka], [k2])
                tt("dve", k2[:], k2[:], kr_, ALU.mult, [k2, psh], [k2])
                tt("pool", beta[:], kk[:], aa[:], ALU.mult, [kk, aa], [beta])
                if dg:
                    dbg("sw", sw[:].rearrange("p a n -> p (a n)"), [128, 512], F32, [sw])
                    dbg("aa", aa[:].rearrange("p a n -> p (a n)"), [128, 512], F32, [aa])
                    dbg("kkn", kk[:].rearrange("p a n -> p (a n)"), [128, 512], F32, [kk])
                    dbg("k2", k2[:].rearrange("p a n -> p (a n)"), [128, 512], F32, [k2])
                mark("scaled")
                act(e1[:], cs[:], AF.Exp, [cs], [e1], scale=-DECAY_K)
                tt("dve", abp[:, :, 1, :], rr_, e1[:], ALU.mult, [psh, e1], [abp])
                act(e2[:], cs[:], AF.Exp, [cs], [e2], scale=DECAY_K)
                tt("pool", kbar[:], k2[:], e2[:], ALU.mult, [k2, e2], [kbar])
                tt("pool", bbar[:], beta[:], e2[:], ALU.mult, [beta, e2], [bbar])
                tt("pool", e1[:], cs[:], sw[:], ALU.subtract, [cs, sw], [e1])
                act(e1[:], e1[:], AF.Exp, [e1], [e1], scale=-DECAY_K)
                stt("dve", abp[:, :, 0, :], kk[:], -1.0, e1[:], ALU.mult, ALU.mult, [kk, e1], [abp])
                for c in range(4):
                    act(e2[:, c, :], cs[:, c, :], AF.Exp, [cs, csC], [e2], bias=csC[:, c:c + 1], scale=DECAY_K)
                tt("dve", fm3[:, 0, :, :], k2[:], e2[:], ALU.mult, [k2, e2], [fm3])
                tt("pool", fm3[:, 1, :, :], beta[:], e2[:], ALU.mult, [beta, e2], [fm3])
                ysq4 = ysq[:].rearrange("p h n -> p (h n)").rearrange("p (a n) -> p a n", a=4)
                tt("pool", ysq4, rr_, k2[:], ALU.mult, [psh, k2], [ysq])
                tt("pool", wrk[:], ysq4, chp[:, 4, :].unsqueeze(2).to_broadcast([128, 4, 128]), ALU.mult, [ysq, chp], [wrk])
                mark("transp")
                tb0, tk0 = bank(); tb1, tk1 = bank()
                for a3 in range(3):
                    for c in range(4):
                        idx = a3 * 4 + c
                        tb_, tk_ = (tb0, tk0) if idx < 8 else (tb1, tk1)
                        off = (idx % 8) * 128
                        src_ = abp[:, c, 0, :] if a3 == 0 else fm3[:, a3 - 1, c, :]
                        P.op("pe", lambda e, src_=src_, tb_=tb_, off=off: e.transpose(pvb(tb_)[:, off:off + 128], src_, ident),
                             _bl([fm3, abp, cst]), _bl([tk_]), inc=(idx == 7 or idx == 11))
                cp("act", tm3[:, 0:2, :], pvb(tb0).rearrange("p (a n) -> p a n", a=2), [tk0], [tm3])
                cp("dve", tm3[:, 2, :], pvb(tb1)[:, 0:512], [tk1], [tm3])
                vb_, vk_ = bank()
                for c in range(4):
                    P.op("pe", lambda e, c=c, vb_=vb_: e.transpose(pv(vb_)[:, c * 128:(c + 1) * 128], psh[:, 8 + c, :], ident_f),
                         _bl([psh, cstf]), _bl([vk_]), inc=(c == 3))
                cp("act", v32[:], pv(vb_), [vk_], [v32])
                cp("dve", vtm[:], pv(vb_), [vk_], [vtm])
                A_TM = tm3[:, 0, :]; Kt_TM = tm3[:, 1, :]; Bt_TM = tm3[:, 2, :]
            def B1(n):
                dg = (n == dbg_chunk)
                xc = xt[n % 2]
                gCn = gC[n % 2]
                A_TM = tm3[:, 0, :]; Kt_TM = tm3[:, 1, :]; Bt_TM = tm3[:, 2, :]
                mark("chunkmat")
                mkAT = cst[:, 1:3, :].unsqueeze(1).to_broadcast([128, 2, 2, 128])
                for c in range(4):
                    g, cc = c // 2, c % 2
                    ai2, ak2 = bank2(); li2, lk2 = bank2()
                    for hh in range(2):
                        Pp = slice(64 * hh, 64 * hh + 64)
                        mm(pv(ai2 + hh)[:, 0:256].rearrange("p (a n) -> p a n", a=2), bbar[Pp, c, :], abp[Pp, c, :, :], [bbar, abp], [ak2[hh]], inc=False)
                        mm(pv(ai2 + hh)[:, 256:512].rearrange("p (a n) -> p a n", a=2), kbar[Pp, c, :], abp[Pp, c, :, :], [kbar, abp], [ak2[hh]])
                        mm(pv(li2 + hh)[:, 0:128], abp[Pp, c, 0, :], bbar[Pp, c, :], [bbar, abp], [lk2[hh]])
                    vat_ = psum_t[:, ai2:ai2 + 2, :].rearrange("p h (s n) -> p h s n", s=4)
                    vl_ = psum_t[:, li2:li2 + 2, 0:128]
                    tt("dve", atb[:, 2 * c:2 * c + 2, :].rearrange("p h (a n) -> p h a n", a=2), vat_[:, :, 0:2, :], mkAT, ALU.mult, ak2 + [cst], [atb])
                    tt("dve", atk[:, 2 * c:2 * c + 2, :].rearrange("p h (a n) -> p h a n", a=2), vat_[:, :, 2:4, :], mkAT, ALU.mult, ak2 + [cst], [atk])
                    tt("dve", Pq[g][0][:, 2 * cc:2 * cc + 2, 1, :], vat_[:, :, 0, :], cst[:, 7, :].unsqueeze(1).to_broadcast([128, 2, 128]), ALU.mult, ak2 + [cst], [Pq[g][0]])
                    tt("dve", Lf[g][:, 2 * cc:2 * cc + 2, 0, :], vl_, tri_gt.unsqueeze(1).to_broadcast([128, 2, 128]), ALU.mult, lk2 + [cst], [Lf[g]])
                    tt("dve", Pq[g][0][:, 2 * cc:2 * cc + 2, 0, :], vl_, cst[:, 6, :].unsqueeze(1).to_broadcast([128, 2, 128]), ALU.mult, lk2 + [cst], [Pq[g][0]])
                idb = ident.unsqueeze(1).unsqueeze(1).to_broadcast([128, 4, 2, 128])
                for g in range(2):
                    cp("pool", Lf[g][:, :, 1, :], atb[:, 4 * g:4 * g + 4, 0:128], [atb], [Lf[g]])
                    xbi, xbk = bank()
                    for h4 in range(4):
                        h = 4 * g + h4
                        mm(pv(xbi)[:, h4 * 64:(h4 + 1) * 64], atk[:, h, 0:128], vtm[:, h * 64:(h + 1) * 64], [atk, vtm], [xbk], inc=(h4 == 3))
                    cp("act", Xb0[:, 4 * g:4 * g + 4, 64:128], pv(xbi)[:, 0:256].rearrange("p (h n) -> p h n", h=4), [xbk], [Xb0])
                    cp("pool", Xb0[:, 4 * g:4 * g + 4, 0:64], A_TM[:, 256 * g:256 * g + 256].rearrange("p (h n) -> p h n", h=4), [tm3], [Xb0])
                    tt("pool", Mq[g][0][:], Pq[g][0][:], idb, ALU.add, [Pq[g][0], cst], [Mq[g][0]])
                pc, mc = 0, 0
                for it in range(3):
                    for g in range(2):
                        Pc = Pq[g][pc]; Pn = Pq[g][1 - pc]; Mc = Mq[g][mc]; Mn = Mq[g][1 - mc]
                        si, sk = bank2()
                        for h4 in range(4):
                            mm(pv(si, 2)[:, h4 * 256:h4 * 256 + 128], Pc[:, h4, 1, :], Pc[:, h4, 0, :], [Pc], sk, inc=False)
                            mm(pv(si, 2)[:, h4 * 256 + 128:h4 * 256 + 256], Pc[:, h4, 0, :], Pc[:, h4, 1, :], [Pc], sk, inc=(h4 == 3))
                        cp("act", Pn[:], pv(si, 2).rearrange("p (h a n) -> p h a n", h=4, a=2), sk, [Pn])
                        gi_, gk_ = bank2()
                        for h4 in range(4):
                            mm(pv(gi_, 2)[:, h4 * 256:h4 * 256 + 128], Pn[:, h4, 1, :], Mc[:, h4, 0, :], [Pn, Mc], gk_, start=True, stop=False, inc=False)
                            mm(pv(gi_, 2)[:, h4 * 256:h4 * 256 + 128], ident, Mc[:, h4, 0, :], [Mc, cst], gk_, start=False, stop=True, inc=False)
                            mm(pv(gi_, 2)[:, h4 * 256 + 128:h4 * 256 + 256], Mc[:, h4, 0, :], Pn[:, h4, 1, :], [Pn, Mc], gk_, start=True, stop=False, inc=False)
                            mm(pv(gi_, 2)[:, h4 * 256 + 128:h4 * 256 + 256], ident, Mc[:, h4, 1, :], [Mc, cst], gk_, start=False, stop=True, inc=(h4 == 3))
                        cp("act", Mn[:], pv(gi_, 2).rearrange("p (h a n) -> p h a n", h=4, a=2), gk_, [Mn])
                    pc, mc = 1 - pc, 1 - mc
                for mi in (8, 10, 12):
                    for g in range(2):
                        Mc = Mq[g][mc]; Mn = Mq[g][1 - mc]; Zq = Pq[g][pc]
                        zi, zk = bank2()
                        for h4 in range(4):
                            mm(pv(zi, 2)[:, h4 * 256:h4 * 256 + 128], Lf[g][:, h4, 1, :], Mc[:, h4, 0, :], [Lf[g], Mc], zk, inc=False)
                            mm(pv(zi, 2)[:, h4 * 256 + 128:h4 * 256 + 256], Lf[g][:, h4, 0, :], Mc[:, h4, 1, :], [Lf[g], Mc], zk, inc=(h4 == 3))
                        tt("dve", Zq[:], pv(zi, 2).rearrange("p (h a n) -> p h a n", h=4, a=2), cst[:, mi:mi + 2, :].unsqueeze(1).to_broadcast([128, 4, 2, 128]), ALU.mult, zk + [cst], [Zq])
                        gi_, gk_ = bank2()
                        for h4 in range(4):
                            mm(pv(gi_, 2)[:, h4 * 256:h4 * 256 + 128], Mc[:, h4, 1, :], Zq[:, h4, 0, :], [Zq, Mc], gk_, start=True, stop=False, inc=False)
                            mm(pv(gi_, 2)[:, h4 * 256:h4 * 256 + 128], ident, Mc[:, h4, 0, :], [Mc, cst], gk_, start=False, stop=True, inc=False)
                            mm(pv(gi_, 2)[:, h4 * 256 + 128:h4 * 256 + 256], Mc[:, h4, 0, :], Zq[:, h4, 1, :], [Zq, Mc], gk_, start=True, stop=False, inc=False)
                            mm(pv(gi_, 2)[:, h4 * 256 + 128:h4 * 256 + 256], ident, Mc[:, h4, 1, :], [Mc, cst], gk_, start=False, stop=True, inc=(h4 == 3))
                        cp("act", Mn[:], pv(gi_, 2).rearrange("p (h a n) -> p h a n", h=4, a=2), gk_, [Mn])
                    mc = 1 - mc
                for g in range(2):
                    Mc = Mq[g][mc]
                    fi, fk = bank()
                    for h4 in range(4):
                        mm(pv(fi)[:, h4 * 128:(h4 + 1) * 128], Mc[:, h4, 1, :], Xb0[:, 4 * g + h4, :], [Mc, Xb0], [fk], inc=(h4 == 3))
                    cp("act", Xbf[:, 4 * g:4 * g + 4, :], pv(fi).rearrange("p (h n) -> p h n", h=4), [fk], [Xbf])
                if dg:
                    dbg("atb", atb[:].rearrange("p a n -> p (a n)"), [128, 2048], BF16, [atb])
                    dbg("atk", atk[:].rearrange("p a n -> p (a n)"), [128, 2048], BF16, [atk])
                    dbg("abp", abp[:].rearrange("p c a n -> p (c a n)"), [128, 1024], BF16, [abp])
                    dbg("bbar", bbar[:].rearrange("p c n -> p (c n)"), [128, 512], BF16, [bbar])
                    dbg("kbar", kbar[:].rearrange("p c n -> p (c n)"), [128, 512], BF16, [kbar])
                    dbg("tm3", tm3[:].rearrange("p c n -> p (c n)"), [128, 1536], BF16, [tm3])
                    dbg("vtm", vtm[:], [128, 512], BF16, [vtm])
                    dbg("x0", Xb0[:].rearrange("p h n -> p (h n)"), [128, 1024], BF16, [Xb0])
                    dbg("xf", Xbf[:].rearrange("p h n -> p (h n)"), [128, 1024], BF16, [Xbf])
                mark("rpt")
                rbi, rbk = bank(); ybi, ybk = bank(); tbi, tbk = bank()
                for h in range(8):
                    c, hh = h // 2, h % 2
                    Pp = slice(64 * hh, 64 * hh + 64)
                    mm(pv(rbi)[Pp, c * 128:(c + 1) * 128], Xbf[:, h, 0:64], atb[:, h, 128:256], [Xbf, atb], [rbk], inc=(h == 7))
                tt("dve", RpT[:], pv(rbi).rearrange("p (a n) -> p a n", a=4), abp[:, :, 1, :], ALU.add, [rbk, abp], [RpT])
                for h in range(8):
                    mm(pv(ybi)[:, h * 64:(h + 1) * 64], atb[:, h, 128:256], Xbf[:, h, 64:128], [Xbf, atb], [ybk], start=True, stop=False, inc=False)
                    mm(pv(ybi)[:, h * 64:(h + 1) * 64], atk[:, h, 128:256], vtm[:, h * 64:(h + 1) * 64], [atk, vtm], [ybk], start=False, stop=True, inc=(h == 7))
                cp("act", Y0[:], pv(ybi), [ybk], [Y0])
                if dg:
                    dbg("y0", Y0[:], [128, 512], F32, [Y0])
                for h in range(8):
                    c, hh = h // 2, h % 2
                    Pp = slice(64 * hh, 64 * hh + 64)
                    mm(pv(tbi)[Pp, c * 64:(c + 1) * 64], Xbf[:, h, 0:64], Bt_TM[:, h * 64:(h + 1) * 64], [Xbf, tm3], [tbk], inc=False)
                    mm(pv(tbi)[Pp, 256 + c * 64:256 + (c + 1) * 64], Kt_TM[:, h * 64:(h + 1) * 64], vtm[:, h * 64:(h + 1) * 64], [tm3, vtm], [tbk], start=True, stop=False, inc=False)
                    mm(pv(tbi)[Pp, 256 + c * 64:256 + (c + 1) * 64], Bt_TM[:, h * 64:(h + 1) * 64], Xbf[:, h, 64:128], [tm3, Xbf], [tbk], start=False, stop=True, inc=(h == 7))
                cp("act", TpT[:], pv(tbi)[:, 0:256].rearrange("p (a n) -> p a n", a=4), [tbk], [TpT])
                cp("dve", D32[:], pv(tbi)[:, 256:512].rearrange("p (a n) -> p a n", a=4), [tbk], [D32])
                mark("serial")
                Hc = Hbf[n % 2]; Hn = Hbf[(n + 1) % 2]
                ob2 = [bank(), bank()]
                for h in range(8):
                    c, hh = h // 2, h % 2
                    Pp = slice(64 * hh, 64 * hh + 64)
                    mm(pv(ob2[hh][0])[:, c * 64:(c + 1) * 64], RpT[Pp, c, :], Hc[Pp, c, :], [RpT, Hc], [ob2[hh][1]], inc=(h >= 6))
                yyv = yy[:].rearrange("p (c q) n -> p c q n", q=2); y0v = Y0[:].rearrange("p (c q n) -> p c q n", q=2, n=64)
                for hh in range(2):
                    tt("dve", yyv[:, :, hh, :], pv(ob2[hh][0])[:, 0:256].rearrange("p (a n) -> p a n", a=4), y0v[:, :, hh, :], ALU.add, [ob2[hh][1], Y0], [yy])
                hb2 = [bank(), bank()]
                for h in range(8):
                    c, hh = h // 2, h % 2
                    Pp = slice(64 * hh, 64 * hh + 64)
                    mm(pv(hb2[hh][0])[Pp, c * 64:(c + 1) * 64], TpT[Pp, c, :], Hc[Pp, c, :], [TpT, Hc], [hb2[hh][1]], inc=(h >= 6))
                for hh in range(2):
                    Pp = slice(64 * hh, 64 * hh + 64)
                    tt("dve", D32[Pp, :, :], D32[Pp, :, :], pv(hb2[hh][0])[Pp, 0:256].rearrange("p (a n) -> p a n", a=4), ALU.add, [D32, hb2[hh][1]], [D32])
                for c in range(4):
                    stt("dve", H32[:, c, :], H32[:, c, :], gCn[:, c:c + 1], D32[:, c, :], ALU.mult, ALU.add, [H32, gCn, D32], [H32])
                cp("dve", Hn[:], H32[:], [H32], [Hn])
                if dg:
                    dbg("yy", yy[:].rearrange("p a n -> p (a n)"), [128, 512], F32, [yy])
                mark("gnorm")
                red("dve", gn[:, 0, :], yy[:], [yy], [gn])
                tt("pool", ysq[:], yy[:], yy[:], ALU.mult, [yy], [ysq])
                red("dve", gn[:, 1, :], ysq[:], [ysq], [gn])
                ts("dve", gn[:, 2, :], gn[:, 0, :], 1.0 / 64, None, ALU.mult, None, [gn], [gn])
                tt("dve", gn[:, 3, :], gn[:, 2, :], gn[:, 2, :], ALU.mult, [gn], [gn])
                stt("dve", gn[:, 4, :], gn[:, 1, :], 1.0 / 64, gn[:, 3, :], ALU.mult, ALU.subtract, [gn], [gn])
                ts("dve", gn[:, 4, :], gn[:, 4, :], GN_EPS, None, ALU.add, None, [gn], [gn])
                act(gn[:, 4, :], gn[:, 4, :], AF.Sqrt, [gn], [gn])
                P.op("dve", lambda e: e.reciprocal(out=gn[:, 5, :], in_=gn[:, 4, :]), _bl([gn]), _bl([gn]))
                tt("dve", yy[:], yy[:], gn[:, 2, :].unsqueeze(2).to_broadcast([128, 8, 64]), ALU.subtract, [yy, gn], [yy])
                tt("dve", yy[:], yy[:], gn[:, 5, :].unsqueeze(2).to_broadcast([128, 8, 64]), ALU.mult, [yy, gn], [yy])
                yf = yy[:].rearrange("p h n -> p (h n)")
                tt("pool", yf, yf, lnx_b[:, 0:512], ALU.mult, [yy, lnx_b], [yy])
                tt("pool", yf, yf, lnx_b[:, 512:1024], ALU.add, [yy, lnx_b], [yy])
                bbi, bbk = bank()
                for c in range(4):
                    mm(pv(bbi)[:, c * 2:(c + 1) * 2], wrk[:, c, :], bsel[:], [wrk, bsel], [bbk], inc=(c == 3))
                cp("act", bon[:], pv(bbi)[:, 0:8], [bbk], [bon])
                tt("dve", ysq[:], v32[:].rearrange("p (h n) -> p h n", h=8), bon[:].unsqueeze(2).to_broadcast([128, 8, 64]), ALU.mult, [v32, bon], [ysq])
                tt("dve", yy[:], yy[:], ysq[:], ALU.add, [yy, ysq], [yy])
                tt("dve", obf[:], yf, gtm[:], ALU.mult, [yy, gtm], [obf])
                if dg:
                    dbg("orwkv", obf[:], [128, 512], BF16, [obf])
            def B2(n):
                dg = (n == dbg_chunk)
                xc = xt[n % 2]
                tbi2, tbk2 = bank()
                for c in range(4):
                    P.op("pe", lambda e, c=c, tbi2=tbi2: e.transpose(pvb(tbi2)[:, c * 128:(c + 1) * 128], obf[:, c * 128:(c + 1) * 128], ident),
                         _bl([obf, cst]), _bl([tbk2]), inc=(c == 3))
                cp("act", mixR[:], pvb(tbi2)[:, 0:512].rearrange("p (a n) -> p a n", a=4), [tbk2], [mixR])
                if dg:
                    dbg("mixR", mixR[:].rearrange("p a n -> p (a n)"), [128, 512], BF16, [mixR])
                    dbg("mixA", matt[n % 2][:].rearrange("p a n -> p (a n)"), [128, 512], BF16, [matt[n % 2]])
                mark("outproj")
                x1c = xc; h2c = h2[n % 2]
                oi, ok = bank2()
                for nh in range(2):
                    for kc in range(8):
                        mt_ = mixR[:, kc, :] if kc < 4 else matt[n % 2][:, kc - 4, :]
                        mm(pv(oi, 2)[:, nh * 512:(nh + 1) * 512], mt_, w_out[:, kc, nh * 512:(nh + 1) * 512], [mixR, matt[n % 2], w_out], ok,
                           start=(kc == 0), stop=(kc == 7), inc=(kc == 7 and nh == 1))
                tt("dve", xc[:], pv(oi, 2), xc[:], ALU.add, ok + [xc], [xc])
                dma("sp", out_d[n * 128:(n + 1) * 128, :], x1c[:], [x1c], [], "x1o")
                if dg:
                    dbg("x1", x1c[:], [128, 1024], F32, [x1c])
                mark("norm2")
                act(h2c[:], x1c[:], AF.Square, [x1c], [h2c, smB], accum=smB[:, 4:5])
                ts("dve", smB[:, 5:6], smB[:, 4:5], 1.0 / D, NORM_EPS, ALU.mult, ALU.add, [smB], [smB])
                act(smB[:, 5:6], smB[:, 5:6], AF.Sqrt, [smB], [smB])
                P.op("dve", lambda e: e.reciprocal(out=smB[:, 6:7], in_=smB[:, 5:6]), _bl([smB]), _bl([smB]))
                h2f = atb[:].rearrange("p a n -> p (a n)").bitcast(F32)[:, 0:D]
                stt("dve", h2f, x1c[:], smB[:, 6:7], s2_b[:], ALU.mult, ALU.mult, [x1c, smB, s2_b], [atb])
                tt("pool", h2c[:], h2f, sh2_b[:], ALU.add, [atb, sh2_b], [h2c])
                dma("sp", hbuf_d[n * 128:(n + 1) * 128, :], h2c[:], [h2c], [], "h2o")
                mark("router")
                ti, tk_ = bank()
                for kc in range(8):
                    P.op("pe", lambda e, kc=kc, ti=ti: e.transpose(pvb(ti)[:, kc * 128:(kc + 1) * 128], h2c[:, kc * 128:(kc + 1) * 128], ident),
                         _bl([h2c, cst]), _bl([tk_]), inc=(kc == 7))
                h2T = atk[:].rearrange("p a n -> p (a n)")[:, 0:1024].rearrange("p (a n) -> p a n", a=8)
                cp("act", h2T, pvb(ti).rearrange("p (a n) -> p a n", a=8), [tk_], [atk])
                li, lk = bank()
                for kc in range(8):
                    mm(pv(li)[:, 0:32], h2T[:, kc, :], wr[:, kc, :], [atk, wr], [lk], start=(kc == 0), stop=(kc == 7), inc=(kc == 7))
                tt("dve", lg_all[:, n, :], pv(li)[:, 0:32], brt[:], ALU.add, [lk, brt], [lg_all])
                P.op("dve", lambda e, n=n: e.max(out=top_all[:, n, :], in_=lg_all[:, n, :]), _bl([lg_all]), _bl([top_all]))
                ts("dve", smB[:, 8:9], top_all[:, n, 0:1], -1.0, None, ALU.mult, None, [top_all], [smB])
                act(ex4[:], top_all[:, n, 0:4], AF.Exp, [top_all, smB], [ex4, smB], bias=smB[:, 8:9], accum=smB[:, 9:10])
                P.op("dve", lambda e: e.reciprocal(out=smB[:, 10:11], in_=smB[:, 9:10]), _bl([smB]), _bl([smB]))
                ts("dve", gate_all[:, n, :], ex4[:], smB[:, 10:11], None, ALU.mult, None, [ex4, smB], [gate_all])
                ts("dve", msk[:], lg_all[:, n, :], top_all[:, n, 3:4], None, ALU.is_ge, None, [lg_all, top_all], [msk])
                ci, ck = bank()
                mm(pv(ci)[:, 0:32], tri_lt, msk[:], [msk, cst], [ck], inc=False)
                mm(pv(ci)[:, 32:64], ones_bf, msk[:], [msk, cst], [ck])
                tt("dve", pos_all[:, n, :], pv(ci)[:, 0:32], carry[:], ALU.add, [ck, carry], [pos_all])
                tt("dve", carry[:], carry[:], pv(ci)[:, 32:64], ALU.add, [ck, carry], [carry])
            kcur = [None]
            Fe(0); Fl(0)
            for n in range(nchunks):
                if n + 1 < nchunks and not os.environ.get("KNOIL"):
                    interleave(P, lambda n=n: B1(n), lambda n=n: Fe(n + 1))
                    interleave(P, lambda n=n: B2(n), lambda n=n: Fl(n + 1))
                else:
                    B1(n); B2(n)
                    if n + 1 < nchunks:
                        Fe(n + 1); Fl(n + 1)
            if dbg_chunk is not None:
                dbg("lg", lg_all[:, 0:nchunks, :].rearrange("p a n -> p (a n)"), [128, nchunks * 32], F32, [lg_all])
            P.barrier()

        if dbg_chunk is None and not os.environ.get("KSKIPMOE"):
            with ExitStack() as st3:
                bst = sbuf(st3, "bst", [128, NB * 32])
                dma("sp", bst[:], bst_d, [], [bst], "bst")
                bstart = bst[:].rearrange("p (b e) -> p b e", b=NB)
                cmp8 = sbuf(st3, "cmp8", [128, 32, MB]); nblk = sbuf(st3, "nblk", [128, 32])
                pend = sbuf(st3, "pend", [128, 32]); pstart = sbuf(st3, "pstart", [128, 32])
                cmpb = sbuf(st3, "cmpb", [128, NB, 32]); eb = sbuf(st3, "eb", [128, NB])
                woff_f = sbuf(st3, "woff_f", [128, NB]); same = sbuf(st3, "same", [128, NB])
                vala = sbuf(st3, "vala", [128, NCH, 32]); oha = sbuf(st3, "oha", [128, NCH, 32])
                dest_f = sbuf(st3, "dest_f", [128, NCH, 4])
                hs = [sbuf(st3, "hs%d" % i, [128, D], BF16) for i in range(4)]
                tt("dve", cmp8[:], carry[:].unsqueeze(2).to_broadcast([128, 32, MB]), thr8.unsqueeze(1).to_broadcast([128, 32, MB]), ALU.is_gt, [carry, cst2], [cmp8])
                red("dve", nblk[:], cmp8[:], [cmp8], [nblk])
                ts("dve", nblk[:], nblk[:], float(BLK), None, ALU.mult, None, [nblk], [nblk])
                P.op("dve", lambda e: e.tensor_tensor_scan(out=pend[:], data0=ones_f[:, 0:32], data1=nblk[:], initial=0.0, op0=ALU.mult, op1=ALU.add),
                     _bl([nblk, cstf]), _bl([pend]))
                tt("dve", pstart[:], pend[:], nblk[:], ALU.subtract, [pend, nblk], [pstart])
                tt("dve", cmpb[:], bstart, pend[:].unsqueeze(1).to_broadcast([128, NB, 32]), ALU.is_ge, [pend, bst], [cmpb])
                red("dve", eb[:], cmpb[:], [cmpb], [eb])
                ts("dve", eb[:], eb[:], 31.0, None, ALU.min, None, [eb], [eb])
                ts("dve", woff_f[:], eb[:], 128.0, iota_p, ALU.mult, ALU.add, [eb, cst2], [woff_f])
                P.op("dve", lambda e: e.memset(same[:], 0.0), [], _bl([same]))
                tt("dve", same[:, 2:NB], eb[:, 2:NB], eb[:, 0:NB - 2], ALU.is_equal, [eb, same], [same])
                stt("dve", woff_f[:], same[:], 8192.0, woff_f[:], ALU.mult, ALU.add, [same, woff_f], [woff_f])
                cp("dve", woff[:], woff_f[:], [woff_f], [woff])
                tt("dve", vala[:, 0:nch, :], pos_all[:, 0:nch, :], pstart[:].unsqueeze(1).to_broadcast([128, nch, 32]), ALU.add, [pos_all, pstart], [vala])
                for k in range(4):
                    tt("dve", oha[:, 0:nch, :], lg_all[:, 0:nch, :], top_all[:, 0:nch, k:k + 1].to_broadcast([128, nch, 32]), ALU.is_equal, [lg_all, top_all], [oha])
                    tt("dve", oha[:, 0:nch, :], oha[:, 0:nch, :], vala[:, 0:nch, :], ALU.mult, [oha, vala], [oha])
                    red("dve", dest_f[:, 0:nch, k], oha[:, 0:nch, :], [oha], [dest_f])
                cp("dve", dest[:, 0:nch, :], dest_f[:, 0:nch, :], [dest_f], [dest])
                P._wait("pool", ("zf", P.dsem["zf"][1]))
                for n in range(nch):
                    hc = hs[n % 4]; dc = dcur[n % 2]
                    dma("sp", hc[:], hbuf_d[n * 128:(n + 1) * 128, :], [], [hc], "hs%d" % (n % 4))
                    cp("dve", dc[:], dest[:, n, :], [dest], [dc])
                    for k in range(4):
                        P.dma("pool", lambda e, k=k, hc=hc, dc=dc: e.indirect_dma_start(
                            out=xs_d, out_offset=bass.IndirectOffsetOnAxis(ap=dc[:, k:k + 1], axis=0),
                            in_=hc[:, :], in_offset=None, bounds_check=bc(e, NSLOT - 1), oob_is_err=False),
                            _bl([hc, dc]), [], key="sc%d" % (n % 4))
                P.barrier()

            with ExitStack() as st4:
                w1s = [sbuf(st4, "w1s%d" % i, [128, 8, 2048], BF16) for i in range(2)]
                w2s = [sbuf(st4, "w2s%d" % i, [128, 8, 1024], BF16) for i in range(2)]
                b1s = [sbuf(st4, "b1s%d" % i, [128, 16 + 1024]) for i in range(2)]
                b1l1 = [sbuf(st4, "b1l1_%d" % i, [128, 8]) for i in range(2)]
                xg = [sbuf(st4, "xg%d" % i, [128, JB, D], BF16) for i in range(2)]
                xT = sbuf(st4, "xT", [128, 8, BLK], BF16)
                aT = sbuf(st4, "aT", [128, 8, BLK], BF16)
                t1 = [sbuf(st4, "t1_%d" % i, [128, BLK]) for i in range(2)]
                t2 = [sbuf(st4, "t2_%d" % i, [128, BLK]) for i in range(2)]
                sgm = [sbuf(st4, "sgm%d" % i, [128, BLK]) for i in range(2)]
                yo = [sbuf(st4, "yo%d" % i, [128, D]) for i in range(2)]
                nblocks = min(NB, nch * 128 * 4 // BLK + 32)

                def load_w(b):
                    i = b % 2
                    cp("dve", wofc[i][:], woff[:, b:b + 1], [woff], [wofc[i]])
                    P.dma("pool", lambda e: e.indirect_dma_start(out=w1s[i][:].rearrange("p a n -> p (a n)"), out_offset=None, in_=w1_d,
                          in_offset=bass.IndirectOffsetOnAxis(ap=wofc[i][:, 0:1], axis=0), bounds_check=bc(e, 32 * 128 - 1), oob_is_err=False),
                          _bl([wofc[i]]), _bl([w1s[i]]), key="w1_%d" % i)
                    P.dma("pool", lambda e: e.indirect_dma_start(out=w2s[i][:].rearrange("p a n -> p (a n)"), out_offset=None, in_=w2_d,
                          in_offset=bass.IndirectOffsetOnAxis(ap=wofc[i][:, 0:1], axis=0), bounds_check=bc(e, 32 * 128 - 1), oob_is_err=False),
                          _bl([wofc[i]]), _bl([w2s[i]]), key="w2_%d" % i)
                    P.dma("pool", lambda e: e.indirect_dma_start(out=b1s[i][:, :], out_offset=None, in_=b1_d,
                          in_offset=bass.IndirectOffsetOnAxis(ap=wofc[i][:, 0:1], axis=0), bounds_check=bc(e, 32 * 128 - 1), oob_is_err=False),
                          _bl([wofc[i]]), _bl([b1s[i]]), key="b1_%d" % i)

                def load_x(b):
                    i = b % 2
                    dma("sp", xg[i][:], xs_d[b * BLK:(b + 1) * BLK, :].rearrange("(j p) d -> p j d", p=128), [], [xg[i]], "xg%d" % i)

                load_w(0); load_x(0)
                for b in range(nblocks):
                    i = b % 2
                    if b + 1 < nblocks:
                        load_w(b + 1); load_x(b + 1)
                    for kc in range(8):
                        ti, tk_ = bank()
                        for j in range(JB):
                            P.op("pe", lambda e, kc=kc, j=j, ti=ti, i=i: e.transpose(pvb(ti)[:, j * 128:(j + 1) * 128], xg[i][:, j, kc * 128:(kc + 1) * 128], ident),
                                 _bl([xg[i], cst]), _bl([tk_]), inc=(j == JB - 1))
                        cp("act", xT[:, kc, :], pvb(ti)[:, 0:BLK], [tk_], [xT])
                    ts("dve", b1l1[i][:], b1s[i][:, 8:16], 1.0, None, ALU.add, None, [b1s[i]], [b1l1[i]])
                    for fc in range(8):
                        gi, gk = bank(); li, lk = bank()
                        for kc in range(8):
                            mm(pv(gi)[:, 0:BLK], w1s[i][:, kc, fc * 128:(fc + 1) * 128], xT[:, kc, :], [w1s[i], xT], [gk], start=(kc == 0), stop=(kc == 7), inc=(kc == 7))
                        for kc in range(8):
                            mm(pv(li)[:, 0:BLK], w1s[i][:, kc, 1024 + fc * 128:1024 + (fc + 1) * 128], xT[:, kc, :], [w1s[i], xT], [lk], start=(kc == 0), stop=(kc == 7), inc=(kc == 7))
                        a1 = t1[fc % 2]; a2 = t2[fc % 2]; sg_ = sgm[fc % 2]
                        ts("dve", a1[:], pv(gi)[:, 0:BLK], b1s[i][:, fc:fc + 1], 7.0, ALU.add, ALU.min, [gk, b1s[i]], [a1])
                        act(sg_[:], a1[:], AF.Sigmoid, [a1], [sg_], scale=1.702)
                        ts("dve", a2[:], pv(li)[:, 0:BLK], b1l1[i][:, fc:fc + 1], 8.0, ALU.add, ALU.min, [lk, b1l1[i]], [a2])
                        stt("dve", a2[:], a2[:], -6.0, a1[:], ALU.max, ALU.mult, [a2, a1], [a2])
                        tt("dve", aT[:, fc, :], a2[:], sg_[:], ALU.mult, [a2, sg_], [aT])
                    for j in range(JB):
                        oi, ok = bank2()
                        for nh in range(2):
                            for kc in range(8):
                                mm(pv(oi, 2)[:, nh * 512:(nh + 1) * 512], aT[:, kc, j * 128:(j + 1) * 128], w2s[i][:, kc, nh * 512:(nh + 1) * 512],
                                   [aT, w2s[i]], ok, start=(kc == 0), stop=(kc == 7), inc=(kc == 7 and nh == 1))
                        yb = yo[j % 2]
                        tt("dve", yb[:], pv(oi, 2), b1s[i][:, 16:1040], ALU.add, ok + [b1s[i]], [yb])
                        dma("sp", ys_d[b * BLK + j * 128:b * BLK + (j + 1) * 128, :], yb[:], [yb], [], "yo%d" % (j % 2))
                P.barrier()

            with ExitStack() as st5:
                yg = [[sbuf(st5, "yg%d_%d" % (i, k), [128, D]) for k in range(4)] for i in range(3)]
                xr = [sbuf(st5, "xr%d" % i, [128, D]) for i in range(3)]
                acc = [sbuf(st5, "acc%d" % i, [128, D]) for i in range(3)]
                nfin = nch
                for n in range(nfin):
                    i = n % 3
                    dma("sp", xr[i][:], out_d[n * 128:(n + 1) * 128, :], [], [xr[i]], "xr%d" % i)
                    dc = dcur[n % 2]
                    cp("dve", dc[:], dest[:, n, :], [dest], [dc])
                    for k in range(4):
                        P.dma("pool", lambda e, k=k, i=i, dc=dc: e.indirect_dma_start(out=yg[i][k][:, :], out_offset=None, in_=ys_d,
                              in_offset=bass.IndirectOffsetOnAxis(ap=dc[:, k:k + 1], axis=0), bounds_check=bc(e, NSLOT - 1), oob_is_err=False),
                              _bl([dc]), _bl([yg[i][k]]), key="yg%d_%d" % (i, k))
                    a_ = acc[i]
                    ts("dve", a_[:], yg[i][0][:], gate_all[:, n, 0:1], None, ALU.mult, None, [yg[i][0], gate_all], [a_])
                    for k in range(1, 4):
                        stt("dve", a_[:], yg[i][k][:], gate_all[:, n, k:k + 1], a_[:], ALU.mult, ALU.add, [yg[i][k], gate_all, a_], [a_])
                    tt("dve", a_[:], a_[:], gt2_b[:], ALU.mult, [a_, gt2_b], [a_])
                    tt("dve", a_[:], a_[:], xr[i][:], ALU.add, [a_, xr[i]], [a_])
                    dma("sp", out_d[n * 128:(n + 1) * 128, :], a_[:], [a_], [], "fo%d" % i)
        for k, v in P.dsem.items():
            P._wait("sp", (k, v[1]))
        P.emit()
    return nc, dbg_out, P


def _consts():
    p = np.arange(128)[:, None]; j = np.arange(128)[None, :]
    c = np.zeros((128, 14, 128), np.float32)
    c[:, 0] = (p == j); c[:, 1] = (p < j); c[:, 2] = (p <= j); c[:, 3] = (p > j)
    c[:, 4] = ((p // 64) == (j // 64)); c[:, 5] = 1.0
    c[:, 6] = (p > j) & ((p // 16) == (j // 16)); c[:, 7] = (p < j) & ((p // 16) == (j // 16))
    for ii, bsz in enumerate((32, 64, 128)):
        hb = bsz // 2
        c[:, 8 + 2 * ii] = ((p // bsz) == (j // bsz)) & ((p // hb) > (j // hb))
        c[:, 9 + 2 * ii] = ((p // bsz) == (j // bsz)) & ((p // hb) < (j // hb))
    c2 = np.zeros((128, MB + 3), np.float32)
    c2[:, 0:MB] = np.arange(MB)[None, :] * BLK
    c2[:, MB] = (np.arange(128) < 64); c2[:, MB + 1] = (np.arange(128) >= 64)
    c2[:, MB + 2] = np.arange(128)
    bst = np.ascontiguousarray(np.broadcast_to(np.repeat(np.arange(NB) * BLK, 32)[None, :], (128, NB * 32))).astype(np.float32)
    return c, c2, bst


def _prep_shared(inp):
    f = lambda a: np.ascontiguousarray(np.asarray(a, dtype=np.float32))
    L = 0
    fm = lambda v, k: f(np.asarray(v).reshape(k, 128).T)
    qperm = np.concatenate([np.concatenate([np.arange(64) + 64 * jj, np.arange(64) + 64 * (4 + jj)]) for jj in range(4)])
    w_in = np.asarray(inp["w_in"][L])
    cols = np.concatenate([np.arange(1792), 1792 + qperm, np.arange(2304, 2560)])
    w_out = np.asarray(inp["w_out"][L])
    rows = np.concatenate([np.arange(512), 512 + qperm])
    c, c2, bst = _consts()
    sh = {
        "w_ada": f(inp["w_ada"][L]), "b_ada_fm": fm(inp["b_ada"][L], 48), "b_ada_row": f(inp["b_ada"][L]).reshape(1, -1),
        "g1_fm": fm(inp["norm1_g"][L], 8), "g2_row": f(inp["norm2_g"][L]).reshape(1, -1),
        "w_in": f(w_in[:, cols]), "mu_fm": fm(inp["mu_shift"][L], 14),
        "chp": f(np.stack([np.asarray(inp[k][L]).reshape(-1).reshape(4, 128).T for k in ("w0", "a0", "k_k", "k_a", "r_k")], axis=1)),
        "lnx_row": f(np.concatenate([inp["lnx_g"][L], inp["lnx_b"][L]])).reshape(1, -1),
        "lora_up": f(np.concatenate([inp["w_up"][L], inp["a_up"][L]], axis=0)), "g_up": f(inp["g_up"][L]),
        "qkg": f(np.stack([np.tile(inp["q_norm_g"][L], 2), np.tile(inp["k_norm_g"][L], 2)], axis=1)),
        "sink_fm": f(np.concatenate([np.tile(np.asarray(inp["sinks"][L])[0:4][None, :], (64, 1)),
                                     np.tile(np.asarray(inp["sinks"][L])[4:8][None, :], (64, 1))], axis=0)),
        "w_out": f(w_out[rows, :]), "w_router": f(inp["w_router"][L]), "b_router": f(inp["b_router"][L]).reshape(1, -1),
        "w1p": f(np.asarray(inp["w1"][L]).reshape(32, 8, 128, 2048).transpose(0, 2, 1, 3).reshape(32 * 128, 8 * 2048)),
        "w2p": f(np.asarray(inp["w2"][L]).reshape(32, 8, 128, 1024).transpose(0, 2, 1, 3).reshape(32 * 128, 8 * 1024)),
        "b12p": f(np.concatenate([np.asarray(inp["b1"][L]).reshape(32, 16, 128).transpose(0, 2, 1).reshape(32 * 128, 16),
                                  np.repeat(np.asarray(inp["b2"][L]), 128, axis=0)], axis=1)),
        "consts": c, "consts2": c2, "bstart": bst, "zeros": np.zeros((1024, D), dtype=ml_dtypes.bfloat16),
    }
    return sh


def kernel(**inputs):
    sh = _prep_shared(inputs)
    x = np.asarray(inputs["x"], dtype=np.float32)
    cc = np.asarray(inputs["c"], dtype=np.float32)
    nc, _, _ = build_program()
    in_maps = []
    for b in range(8):
        m = dict(sh)
        m["x"] = np.ascontiguousarray(x[b])
        m["c_fm"] = np.ascontiguousarray(cc[b].reshape(8, 128).T)
        in_maps.append(m)
    res = run_bass_kernel_spmd(nc, in_maps, core_ids=list(range(8)))
    return np.stack([np.asarray(res.results[b]["out"], dtype=np.float32) for b in range(8)], axis=0)
```
